# Optimizing a Trainium2 kernel written in Bass

```python
import jax, jax.numpy as jnp
from jax import lax
import numpy as np


D_MODEL = 1024
BATCH = 4
SEQ = 8192
DEPTH = 4

GRID_W = 64
CTX_LEN = 256
FOURIER_GROUPS = 4
FOURIER_GROUP_DIM = D_MODEL // 8
FOURIER_DIM = FOURIER_GROUPS * FOURIER_GROUP_DIM
HEAD_DIM = 64
N_Q_HEADS = D_MODEL // 128
N_KV_HEADS = N_Q_HEADS // 4
Q_PER_KV = N_Q_HEADS // N_KV_HEADS
ATTN_DIM = N_Q_HEADS * HEAD_DIM
KV_DIM = N_KV_HEADS * HEAD_DIM
WINDOW = 128
BLOCK = 128
ROPE_THETA = 10000.0
ROPE_FREQS = HEAD_DIM // 4
IN_DIM = FOURIER_DIM + ATTN_DIM + 2 * KV_DIM + 2 * D_MODEL
SPLITS = (FOURIER_DIM, FOURIER_DIM + ATTN_DIM, FOURIER_DIM + ATTN_DIM + KV_DIM,
          FOURIER_DIM + ATTN_DIM + 2 * KV_DIM, FOURIER_DIM + ATTN_DIM + 2 * KV_DIM + D_MODEL)
KV_LO = FOURIER_DIM + ATTN_DIM
KV_HI = FOURIER_DIM + ATTN_DIM + 2 * KV_DIM
N_EXPERTS = 32
TOP_K = 4
D_EXPERT = D_MODEL
SWIGLU_LIMIT = 7.0
SWIGLU_ALPHA = 1.702
MOE_BLOCK = 128
N_MOD = 6
EPS = 1e-5
NEG_INF = -1e30

kernel_name = 'hybrid_fourier_window_moe_dit_block'


def rmsnorm(x, g):
    xf = x.astype(jnp.float32)
    y = xf * lax.rsqrt(jnp.mean(xf * xf, axis=-1, keepdims=True) + EPS)
    return (y * g.astype(jnp.float32)).astype(x.dtype)


def modulate(h, shift, scale):
    return h * (1 + scale) + shift


def axial_rope_tables(n):
    rows = n // GRID_W
    row = jnp.broadcast_to(jnp.arange(rows)[:, None], (rows, GRID_W)).reshape(-1)
    col = jnp.broadcast_to(jnp.arange(GRID_W)[None, :], (rows, GRID_W)).reshape(-1)
    inv_freq = ROPE_THETA ** (-jnp.arange(ROPE_FREQS, dtype=jnp.float32) / ROPE_FREQS)
    ang = jnp.stack([row.astype(jnp.float32)[:, None] * inv_freq,
                     col.astype(jnp.float32)[:, None] * inv_freq], axis=1)
    return jnp.cos(ang), jnp.sin(ang)


def apply_axial_rope(x, cos, sin):
    b, n, h, _ = x.shape
    xs = x.reshape(b, n, h, 2, 2, ROPE_FREQS)
    c = cos.reshape(1, n, 1, 2, ROPE_FREQS).astype(x.dtype)
    s = sin.reshape(1, n, 1, 2, ROPE_FREQS).astype(x.dtype)
    x1, x2 = xs[..., 0, :], xs[..., 1, :]
    out = jnp.stack([x1 * c - x2 * s, x2 * c + x1 * s], axis=-2)
    return out.reshape(x.shape)


def split_projection(p):
    b, n = p.shape[:2]
    f, q, k, v, gf, ga = jnp.split(p, SPLITS, axis=-1)
    return (f, q.reshape(b, n, N_Q_HEADS, HEAD_DIM), k.reshape(b, n, N_KV_HEADS, HEAD_DIM),
            v.reshape(b, n, N_KV_HEADS, HEAD_DIM), gf, ga)


def fourier_mix(f):
    b, n, _ = f.shape
    fg = f.astype(jnp.float32).reshape(b, n, FOURIER_GROUPS, FOURIER_GROUP_DIM)
    out = jnp.fft.fft2(fg, axes=(1, 3), norm='ortho').real
    return out.reshape(b, n, FOURIER_DIM).astype(f.dtype)


def sink_column(sink, shape):
    return jnp.broadcast_to(sink.astype(jnp.float32).reshape((1,) * (len(shape) - 4) + (N_KV_HEADS, Q_PER_KV, 1, 1)), shape)


def latent_attention(q, k, v, kc, vc, sink):
    b, n = q.shape[:2]
    L = kc.shape[1]
    nb = n // BLOCK
    qb = (q * HEAD_DIM ** -0.5).reshape(b, nb, BLOCK, N_KV_HEADS, Q_PER_KV, HEAD_DIM)

    def windows(t):
        tp = jnp.pad(t, ((0, 0), (BLOCK, BLOCK), (0, 0), (0, 0))).reshape(b, nb + 2, BLOCK, N_KV_HEADS, HEAD_DIM)
        return jnp.concatenate([tp[:, :-2], tp[:, 1:-1], tp[:, 2:]], axis=2)

    kw, vw = windows(k), windows(v)
    s_loc = jnp.einsum('bnqhgd,bnkhd->bnhgqk', qb, kw).astype(jnp.float32)
    r = jnp.arange(BLOCK)[:, None]
    s = jnp.arange(3 * BLOCK)[None, :]
    rel = s - BLOCK - r
    key_pos = (jnp.arange(nb)[:, None, None] - 1) * BLOCK + s[None]
    mask = (jnp.abs(rel) <= WINDOW)[None] & (key_pos >= 0) & (key_pos < n)
    s_loc = jnp.where(mask[None, :, None, None], s_loc, NEG_INF)
    s_ctx = jnp.einsum('bnqhgd,bkhd->bnhgqk', qb, kc).astype(jnp.float32)
    sk = sink_column(sink, s_ctx.shape[:-1] + (1,))
    p = jax.nn.softmax(jnp.concatenate([s_loc, s_ctx, sk], axis=-1), axis=-1)
    p_loc = p[..., :3 * BLOCK].astype(v.dtype)
    p_ctx = p[..., 3 * BLOCK:3 * BLOCK + L].astype(v.dtype)
    o = (jnp.einsum('bnhgqk,bnkhd->bnqhgd', p_loc, vw)
         + jnp.einsum('bnhgqk,bkhd->bnqhgd', p_ctx, vc))
    return o.reshape(b, n, ATTN_DIM)


def context_attention(qc, kc, vc, sink):
    b, L = qc.shape[:2]
    qg = (qc * HEAD_DIM ** -0.5).reshape(b, L, N_KV_HEADS, Q_PER_KV, HEAD_DIM)
    s = jnp.einsum('bqhgd,bkhd->bhgqk', qg, kc).astype(jnp.float32)
    sk = sink_column(sink, s.shape[:-1] + (1,))
    p = jax.nn.softmax(jnp.concatenate([s, sk], axis=-1), axis=-1)[..., :L].astype(vc.dtype)
    o = jnp.einsum('bhgqk,bkhd->bqhgd', p, vc)
    return o.reshape(b, L, ATTN_DIM)


def merge_branches(f_mixed, attn, gf, ga, w_fo, w_ao, w_out):
    y = jax.nn.sigmoid(gf) * (f_mixed @ w_fo) + jax.nn.sigmoid(ga) * (attn @ w_ao)
    return y @ w_out


def token_mixer(hx, hc, w_in, w_fo, w_ao, w_out, sink, cos, sin, with_ctx_out):
    fx, qx, kx, vx, gfx, gax = split_projection(hx @ w_in)
    qx, kx = apply_axial_rope(qx, cos, sin), apply_axial_rope(kx, cos, sin)
    if with_ctx_out:
        fc, qc, kc, vc, gfc, gac = split_projection(hc @ w_in)
    else:
        b, L = hc.shape[:2]
        kc, vc = jnp.split(hc @ w_in[:, KV_LO:KV_HI], 2, axis=-1)
        kc, vc = kc.reshape(b, L, N_KV_HEADS, HEAD_DIM), vc.reshape(b, L, N_KV_HEADS, HEAD_DIM)
    ax = latent_attention(qx, kx, vx, kc, vc, sink)
    out_x = merge_branches(fourier_mix(fx), ax, gfx, gax, w_fo, w_ao, w_out)
    if not with_ctx_out:
        return out_x, None
    ac = context_attention(qc, kc, vc, sink)
    out_c = merge_branches(fourier_mix(fc), ac, gfc, gac, w_fo, w_ao, w_out)
    return out_x, out_c


def moe_ffn(h, router_w, router_b, w_gu, b_gu, w_down, b_down):
    t, d = h.shape
    logits = (h @ router_w + router_b).astype(jnp.float32)
    top_v, top_i = lax.top_k(logits, TOP_K)
    gates = jax.nn.softmax(top_v, axis=-1).astype(h.dtype)
    tk = t * TOP_K
    flat_e = top_i.reshape(tk)
    flat_tok = jnp.arange(tk) // TOP_K
    order = jnp.argsort(flat_e)
    e_sorted = flat_e[order]
    tok_sorted = flat_tok[order]
    gate_sorted = gates.reshape(tk)[order]
    counts = jnp.zeros((N_EXPERTS,), jnp.int32).at[flat_e].add(1)
    padded = (counts + MOE_BLOCK - 1) // MOE_BLOCK * MOE_BLOCK
    pad_end = jnp.cumsum(padded)
    pad_start = pad_end - padded
    start = jnp.cumsum(counts) - counts
    dest = pad_start[e_sorted] + jnp.arange(tk) - start[e_sorted]
    n_blocks = -(-(tk + N_EXPERTS * (MOE_BLOCK - 1)) // MOE_BLOCK)
    rows = n_blocks * MOE_BLOCK
    slot_tok = jnp.full((rows,), t, jnp.int32).at[dest].set(tok_sorted)
    h_pad = jnp.concatenate([h, jnp.zeros((1, d), h.dtype)], axis=0)
    xb = h_pad[slot_tok].reshape(n_blocks, MOE_BLOCK, d)
    block_e = jnp.minimum(jnp.searchsorted(pad_end, jnp.arange(n_blocks) * MOE_BLOCK, side='right'), N_EXPERTS - 1)

    def expert_block(args):
        xe, e = args
        gu = xe @ w_gu[e] + b_gu[e]
        a, u = jnp.split(gu, 2, axis=-1)
        a = jnp.minimum(a, SWIGLU_LIMIT)
        u = jnp.clip(u, -SWIGLU_LIMIT, SWIGLU_LIMIT)
        y = a * jax.nn.sigmoid(SWIGLU_ALPHA * a) * (u + 1)
        return y @ w_down[e] + b_down[e]

    yb = lax.map(expert_block, (xb, block_e)).reshape(rows, d)
    contrib = yb[dest] * gate_sorted[:, None]
    return jax.ops.segment_sum(contrib, tok_sorted, num_segments=t)


def setup_inputs(seed: int = 0) -> dict:
    key = jax.random.key(seed)
    ks = jax.random.split(key, 20)

    def nrm(k, shape, scale):
        return jax.random.normal(k, shape, jnp.float32) * scale

    return {
        'x': nrm(ks[0], (BATCH, SEQ, D_MODEL), 1.0),
        'c': nrm(ks[1], (BATCH, D_MODEL), 1.0),
        'ctx': nrm(ks[2], (BATCH, CTX_LEN, D_MODEL), 1.0),
        'c_ctx': nrm(ks[3], (D_MODEL,), 1.0),
        'ada_w': nrm(ks[4], (DEPTH, D_MODEL, N_MOD * D_MODEL), 0.5 * D_MODEL ** -0.5),
        'ada_b': nrm(ks[5], (DEPTH, N_MOD * D_MODEL), 0.02),
        'norm1_g': 1.0 + nrm(ks[6], (DEPTH, D_MODEL), 0.02),
        'norm2_g': 1.0 + nrm(ks[7], (DEPTH, D_MODEL), 0.02),
        'w_in': nrm(ks[8], (DEPTH, D_MODEL, IN_DIM), D_MODEL ** -0.5),
        'attn_sink': nrm(ks[9], (DEPTH, N_Q_HEADS), 0.5),
        'w_fourier_out': nrm(ks[10], (DEPTH, FOURIER_DIM, D_MODEL), FOURIER_DIM ** -0.5),
        'w_attn_out': nrm(ks[11], (DEPTH, ATTN_DIM, D_MODEL), ATTN_DIM ** -0.5),
        'w_out': nrm(ks[12], (DEPTH, D_MODEL, D_MODEL), D_MODEL ** -0.5),
        'router_w': nrm(ks[13], (DEPTH, D_MODEL, N_EXPERTS), D_MODEL ** -0.5),
        'router_b': nrm(ks[14], (DEPTH, N_EXPERTS), 0.01),
        'expert_w_gu': nrm(ks[15], (DEPTH, N_EXPERTS, D_MODEL, 2 * D_EXPERT), D_MODEL ** -0.5),
        'expert_b_gu': nrm(ks[16], (DEPTH, N_EXPERTS, 2 * D_EXPERT), 0.02),
        'expert_w_down': nrm(ks[17], (DEPTH, N_EXPERTS, D_EXPERT, D_MODEL), D_EXPERT ** -0.5),
        'expert_b_down': nrm(ks[18], (DEPTH, N_EXPERTS, D_MODEL), 0.02),
        'final_norm_g': 1.0 + nrm(ks[19], (D_MODEL,), 0.02),
    }


def reference(x, c, ctx, c_ctx, ada_w, ada_b, norm1_g, norm2_g, w_in, attn_sink, w_fourier_out,
              w_attn_out, w_out, router_w, router_b, expert_w_gu, expert_b_gu, expert_w_down,
              expert_b_down, final_norm_g):
    b, n, d = x.shape
    L = ctx.shape[1]
    cos, sin = axial_rope_tables(n)
    silu_c = jax.nn.silu(c)
    silu_cc = jax.nn.silu(c_ctx)
    for l in range(DEPTH):
        last = l == DEPTH - 1
        mod_x = (silu_c @ ada_w[l] + ada_b[l])[:, None, :]
        mod_c = silu_cc @ ada_w[l] + ada_b[l]
        sh1x, sc1x, g1x, sh2x, sc2x, g2x = jnp.split(mod_x, N_MOD, axis=-1)
        sh1c, sc1c, g1c, sh2c, sc2c, g2c = jnp.split(mod_c, N_MOD, axis=-1)
        hx = modulate(rmsnorm(x, norm1_g[l]), sh1x, sc1x)
        hc = modulate(rmsnorm(ctx, norm1_g[l]), sh1c, sc1c)
        out_x, out_c = token_mixer(hx, hc, w_in[l], w_fourier_out[l], w_attn_out[l], w_out[l],
                                   attn_sink[l], cos, sin, not last)
        x = x + g1x * out_x
        hx2 = modulate(rmsnorm(x, norm2_g[l]), sh2x, sc2x).reshape(b * n, d)
        if last:
            m = moe_ffn(hx2, router_w[l], router_b[l], expert_w_gu[l], expert_b_gu[l],
                        expert_w_down[l], expert_b_down[l])
            x = x + g2x * m.reshape(b, n, d)
        else:
            ctx = ctx + g1c * out_c
            hc2 = modulate(rmsnorm(ctx, norm2_g[l]), sh2c, sc2c).reshape(b * L, d)
            m = moe_ffn(jnp.concatenate([hx2, hc2], axis=0), router_w[l], router_b[l], expert_w_gu[l],
                        expert_b_gu[l], expert_w_down[l], expert_b_down[l])
            x = x + g2x * m[:b * n].reshape(b, n, d)
            ctx = ctx + g2c * m[b * n:].reshape(b, L, d)
    return rmsnorm(x, final_norm_g)
```

```python
import numpy as np
import ml_dtypes
import concourse.bass as bass
import concourse.mybir as mybir
from concourse.bass_utils import run_bass_kernel_spmd

F32 = mybir.dt.float32
BF16 = mybir.dt.bfloat16
I32 = mybir.dt.int32
ALU = mybir.AluOpType
AF = mybir.ActivationFunctionType
AX = mybir.AxisListType

D = 1024
TX = 8192
TC = 256
T = TX + TC
NT = T // 128
NTX = TX // 128
DEPTH = 4
NE = 32
NB = (T * 4 + NE * 127) // 128 + 1
NSLOT = NB * 128
EPS = 1e-5
OOB = 1 << 28


class Sched:
    ENGS = ['pe', 'act', 'dve', 'pool', 'sp']
    EPOCH = 12000
    NPOOL = 6

    def __init__(self, nc):
        self.nc = nc
        self.ops = []
        self.last_w = {}
        self.readers = {}
        self.sig = []
        self.nsem = 0
        self.esem = {e: self._new_sem('e_' + e) for e in self.ENGS}
        self.ecnt = {e: 0 for e in self.ENGS}
        self.dpool = {e: [[self._new_sem('d_%s%d' % (e, i)), 0] for i in range(self.NPOOL)]
                      for e in self.ENGS}
        self.dnext = {e: 0 for e in self.ENGS}
        self.known = {e: {} for e in self.ENGS}

    def _new_sem(self, name):
        self.nsem += 1
        return self.nc.alloc_semaphore(name='%s_%d' % (name, self.nsem))

    def _prune(self, eng, waits):
        kn = self.known[eng]
        wl = []
        for key, (s, v) in waits.items():
            if kn.get(key, 0) >= v:
                continue
            kn[key] = v
            wl.append((s, v))
        return wl

    def add(self, eng, fn, reads=(), writes=(), ndma=0):
        opid = len(self.ops)
        deps = set()
        for k in list(reads) + list(writes):
            if k in self.last_w:
                deps.add(self.last_w[k])
        for k in writes:
            for r in self.readers.get(k, ()):
                deps.add(r)
        waits = {}
        for d in deps:
            deng = self.ops[d]['eng']
            if deng == 'pe' and eng == 'pe' and not self.ops[d]['ndma'] and not ndma:
                continue
            s, v = self.sig[d]
            key = id(s)
            if key not in waits or waits[key][1] < v:
                waits[key] = (s, v)
        if ndma:
            pool = self.dpool[eng]
            slot = pool[self.dnext[eng] % self.NPOOL]
            self.dnext[eng] += 1
            if slot[1] > 0:
                key = id(slot[0])
                if key not in waits or waits[key][1] < slot[1]:
                    waits[key] = (slot[0], slot[1])
            if slot[1] + 16 * ndma > self.EPOCH * 2:
                slot[0] = self._new_sem('d_' + eng)
                slot[1] = 0
            slot[1] += 16 * ndma
            sig = (slot[0], slot[1])
            inc = (slot[0], 16)
        else:
            if self.ecnt[eng] >= self.EPOCH:
                self.esem[eng] = self._new_sem('e_' + eng)
                self.ecnt[eng] = 0
            self.ecnt[eng] += 1
            sig = (self.esem[eng], self.ecnt[eng])
            inc = (self.esem[eng], 1)
        self.ops.append(dict(eng=eng, fn=fn, waits=self._prune(eng, waits), inc=inc, ndma=ndma))
        self.sig.append(sig)
        for k in reads:
            self.readers.setdefault(k, []).append(opid)
        for k in writes:
            self.last_w[k] = opid
            self.readers[k] = []
        return opid

    def barrier(self):
        sigs = {}
        for e in self.ENGS:
            if self.ecnt[e] > 0:
                sigs[id(self.esem[e])] = (self.esem[e], self.ecnt[e])
            for slot in self.dpool[e]:
                if slot[1] > 0:
                    sigs[id(slot[0])] = (slot[0], slot[1])
        for e in self.ENGS:
            self.ops.append(dict(eng=e, fn=None, waits=self._prune(e, dict(sigs)), inc=None, ndma=0))
            self.sig.append(None)
        self.last_w = {}
        self.readers = {}

    def emit(self):
        nc = self.nc
        with nc.Block() as block:
            def run(engname):
                def body(e):
                    for op in self.ops:
                        if op['eng'] != engname:
                            continue
                        for s, v in op['waits']:
                            e.wait_ge(s, v)
                        if op['fn'] is None:
                            continue
                        try:
                            r = op['fn'](e)
                        except Exception:
                            print('EMIT FAIL', engname, 'op#', self.ops.index(op), 'engine-op-count', self.ecnt, flush=True)
                            raise
                        if not isinstance(r, (list, tuple)):
                            r = [r]
                        if op['ndma']:
                            assert len(r) == op['ndma'], (len(r), op['ndma'])
                        else:
                            assert len(r) == 1
                        for ins in r:
                            ins.then_inc(op['inc'][0], op['inc'][1])
                return body
            block.tensor(run('pe'))
            block.scalar(run('act'))
            block.vector(run('dve'))
            block.gpsimd(run('pool'))
            block.sync(run('sp'))


_DTS = {F32: 4, BF16: 2, I32: 4}


class Arena:
    def __init__(self, nc, limit=229376):
        self.nc = nc
        self.off = 20480
        self.n = 0
        self.limit = limit

    def tile(self, shape, dtype, name='t'):
        nb = _DTS[dtype]
        for s in shape[1:]:
            nb *= s
        nb = (nb + 63) // 64 * 64
        self.n += 1
        h = self.nc.alloc_sbuf_tensor_at('%s_%d' % (name, self.n), list(shape), dtype, offset=self.off)
        self.off += nb
        assert self.off <= self.limit, ('SBUF overflow', name, self.off)
        return h.ap()

    def mark(self):
        return self.off

    def reset(self, m):
        self.off = m


def host_consts():
    c = {}
    c['ident_bf'] = np.eye(128, dtype=np.float32).astype(ml_dtypes.bfloat16)
    c['ident_f'] = np.eye(128, dtype=np.float32)
    k = np.arange(128)[:, None]
    q = np.arange(128)[None, :]
    c['mask_prev'] = (k >= q).astype(np.float32).astype(ml_dtypes.bfloat16)
    c['mask_next'] = (k <= q).astype(np.float32).astype(ml_dtypes.bfloat16)
    c['ustrict'] = (k < q).astype(np.float32).astype(ml_dtypes.bfloat16)
    n = np.arange(TX)
    row = (n // 64).astype(np.float64)
    col = (n % 64).astype(np.float64)
    inv = 10000.0 ** (-np.arange(16, dtype=np.float64) / 16)
    cosT = np.zeros((64, TX), np.float64)
    sinT = np.zeros((64, TX), np.float64)
    for ax, pos in enumerate((row, col)):
        ang = (pos[None, :].astype(np.float32) * inv[:, None].astype(np.float32)).astype(np.float32)
        for pr in range(2):
            cosT[ax * 32 + pr * 16: ax * 32 + pr * 16 + 16] = np.cos(ang)
            sinT[ax * 32 + pr * 16: ax * 32 + pr * 16 + 16] = np.sin(ang)
    c['cosT'] = cosT.astype(np.float32)
    c['sinT'] = sinT.astype(np.float32)
    rm = np.zeros((128, 128), np.float32)
    for ax in range(2):
        for f in range(16):
            d0 = ax * 32 + f
            d1 = ax * 32 + 16 + f
            rm[d1, d0] = -1.0
            rm[d0, d1] = 1.0
    c['rotm'] = rm.astype(ml_dtypes.bfloat16)
    a = np.arange(128, dtype=np.float64)
    th = 2 * np.pi * np.outer(a, a) / 128
    c['c128'] = np.cos(th).astype(np.float32).astype(ml_dtypes.bfloat16)
    c['s128'] = np.sin(th).astype(np.float32).astype(ml_dtypes.bfloat16)
    c['cc'] = np.cos(th).astype(np.float32).astype(ml_dtypes.bfloat16)
    c['nsc'] = (-np.sin(th)).astype(np.float32).astype(ml_dtypes.bfloat16)
    n2 = np.arange(64, dtype=np.float64)
    tw = 2 * np.pi * np.outer(a, n2) / 8192
    c['twr'] = np.cos(tw).astype(np.float32)
    c['twi'] = np.sin(tw).astype(np.float32)
    c['ntwi'] = (-np.sin(tw)).astype(np.float32)
    th64 = 2 * np.pi * np.outer(n2, n2) / 64
    c64 = np.cos(th64) / 1024.0
    s64 = np.sin(th64) / 1024.0
    r2 = np.zeros((128, 128), np.float64)
    r2[0:64, 0:64] = c64
    r2[64:128, 0:64] = -s64
    r2[0:64, 64:128] = s64
    r2[64:128, 64:128] = c64
    c['r2'] = r2.astype(np.float32).astype(ml_dtypes.bfloat16)
    b = np.arange(256, dtype=np.float64)
    th256 = 2 * np.pi * np.outer(b, b) / 256
    sc = 1.0 / np.sqrt(256.0 * 128.0)
    c['c256'] = (np.cos(th256) * sc).astype(np.float32).astype(ml_dtypes.bfloat16).reshape(2, 128, 256).transpose(1, 0, 2).copy()
    c['s256'] = (np.sin(th256) * sc).astype(np.float32).astype(ml_dtypes.bfloat16).reshape(2, 128, 256).transpose(1, 0, 2).copy()
    c['blk128'] = np.broadcast_to((np.arange(NB, dtype=np.float32) * 128.0)[None, :], (32, NB)).copy()
    c['pcol'] = np.arange(128, dtype=np.float32).reshape(128, 1)
    c['u32'] = (np.arange(32)[:, None] < np.arange(32)[None, :]).astype(np.float32)
    return c


CONST_SPECS = None


def build(nl=DEPTH, final_norm=True, dbg=()):
    nc = bass.Bass("TRN2", target_bir_lowering=False)
    consts = host_consts()

    def din(name, shape, dt=F32):
        return nc.dram_tensor(name, list(shape), dt, kind="ExternalInput").ap()

    def dscr(name, shape, dt):
        kind = "ExternalOutput" if name in dbg else "Internal"
        return nc.dram_tensor(name, list(shape), dt, kind=kind).ap()

    xin = din('xin', [TX, D])
    ctxin = din('ctxin', [TC, D])
    ccol = din('ccol', [128, 8, 2])
    ada_w = din('ada_w', [DEPTH, D, 6 * D])
    ada_bT = din('ada_bT', [DEPTH, 128, 48])
    n1g = din('n1g', [DEPTH, 128, 8])
    n2g = din('n2g', [DEPTH, 128, 8])
    fng = din('fng', [128, 8])
    w_in = din('w_in', [DEPTH, D, 3328])
    sink = din('sink', [DEPTH, 8])
    w_fo = din('w_fo', [DEPTH, 512, D])
    w_ao = din('w_ao', [DEPTH, 512, D])
    w_out = din('w_out', [DEPTH, D, D])
    r_w = din('r_w', [DEPTH, 128, 8, NE])
    r_b = din('r_b', [DEPTH, 1, NE])
    wgu = din('wgu', [DEPTH, NE * 128, 8 * 2048])
    wdn = din('wdn', [DEPTH, NE * 128, 8 * 1024])
    bgu = din('bgu', [DEPTH, NE, 2048])
    bdn = din('bdn', [DEPTH, NE, 1024])
    cd = {}
    for k, v in consts.items():
        dt = BF16 if v.dtype == ml_dtypes.bfloat16 else F32
        cd[k] = din('c_' + k, v.shape, dt)
    yout = nc.dram_tensor('yout', [TX, D], F32, kind="ExternalOutput").ap()

    xres = dscr('xres', [T, D], F32)
    f_tm = dscr('f_tm', [T, 512], BF16)
    q_blk = dscr('q_blk', [NT, 64, 8, 128], BF16)
    kT_all = dscr('kT_all', [64, 2, T], BF16)
    v_blk = dscr('v_blk', [128, NT, 128], BF16)
    g_blk = dscr('g_blk', [NT, 128, 16, 128], BF16)
    t2_blk = dscr('t2_blk', [NT, 128, 8, 128], BF16)
    Zd = dscr('Zd', [2, 128, 64, 512], BF16)
    h2_tm = dscr('h2_tm', [T, D], BF16)
    Xs = dscr('Xs', [NSLOT, D], BF16)
    Ys = dscr('Ys', [NSLOT, D], F32)

    S = Sched(nc)
    A = Arena(nc)
    BCREG = {}
    pq = [nc.alloc_psum_tensor('pq%d' % i, [128, 1024], F32).ap() for i in range(3)]
    pbf = [nc.alloc_psum_tensor('pbf%d' % i, [128, 1024], BF16).ap() for i in range(2)]

    def bank(i):
        return pq[i // 2][:, (i % 2) * 512:(i % 2) * 512 + 512]

    C = {}
    qi = [0]

    def dmaq():
        qi[0] += 1
        return 'sp' if qi[0] % 2 else 'act'

    def load_const(name, shape, dt):
        t = A.tile(shape, dt, name)
        S.add('sp', lambda e: [e.dma_start(out=t, in_=cd[name])], writes=[name], ndma=1)
        C[name] = t
        return t

    for nm in ('ident_bf', 'mask_prev', 'mask_next', 'ustrict', 'c128', 's128', 'cc', 'nsc', 'r2'):
        load_const(nm, [128, 128], BF16)
    load_const('ident_f', [128, 128], F32)
    load_const('rotm', [128, 128], BF16)
    load_const('twr', [128, 64], F32)
    load_const('twi', [128, 64], F32)
    load_const('ntwi', [128, 64], F32)
    load_const('c256', [128, 2, 256], BF16)
    load_const('s256', [128, 2, 256], BF16)
    load_const('blk128', [32, NB], F32)
    load_const('pcol', [128, 1], F32)
    load_const('u32', [32, 32], F32)
    ones_bf = A.tile([128, 128], BF16, 'ones_bf')
    S.add('pool', lambda e: e.memset(ones_bf, 1.0), writes=['ones_bf'])
    ones_f = A.tile([128, 128], F32, 'ones_f')
    S.add('pool', lambda e: e.memset(ones_f, 1.0), writes=['ones_f'])
    onec = A.tile([128, 1], F32, 'onec')
    S.add('pool', lambda e: e.memset(onec, 1.0), writes=['onec'])
    epsc = A.tile([128, 1], F32, 'epsc')
    S.add('pool', lambda e: e.memset(epsc, EPS), writes=['epsc'])
    scc = A.tile([128, 8, 2], F32, 'scc')
    S.add('sp', lambda e: [e.dma_start(out=scc, in_=ccol)], writes=['scc'], ndma=1)
    S.add('act', lambda e: e.activation(out=scc, in_=scc, func=AF.Silu), reads=['scc'], writes=['scc'])
    modT = A.tile([128, 48, 2], F32, 'modT')
    s1 = A.tile([128, 8, 2], F32, 's1')
    s2 = A.tile([128, 8, 2], F32, 's2')
    gcol = A.tile([128, 8], F32, 'gcol')
    BT = {k: A.tile([128, D], F32, 'bt_' + k) for k in
          ('g1x', 'g1c', 'g2x', 'g2c', 's2x', 's2c', 'h2x', 'h2c')}
    rwp = A.tile([128, 8, NE], F32, 'rwp')
    rwx = A.tile([128, 8, 2, NE], F32, 'rwx')
    rcst = A.tile([1, 2, NE], F32, 'rcst')
    rbt = A.tile([1, NE], F32, 'rbt')
    PERSIST = A.mark()

    for ci in range(16):
        S.add(dmaq(), lambda e, ci=ci: [e.dma_start(out=xres[ci * 512:(ci + 1) * 512, :], in_=xin[ci * 512:(ci + 1) * 512, :])],
              writes=[('xres0', ci)], ndma=1)
    S.add('act', lambda e: [e.dma_start(out=xres[TX:T, :], in_=ctxin)], writes=['xres2'], ndma=1)
    _mz = A.mark()
    zt = A.tile([128, 8 * D], BF16, 'zt')
    A.reset(_mz)
    S.add('pool', lambda e: e.memset(zt, 0.0), writes=['zt'])
    Xz = Xs.rearrange("(p r) d -> p (r d)", p=128)
    for c0 in range(0, NB * D, 8 * D):
        w_ = min(8 * D, NB * D - c0)
        S.add(dmaq(), lambda e, c0=c0, w_=w_: [e.dma_start(out=Xz[:, c0:c0 + w_], in_=zt[:, 0:w_])], reads=['zt'], writes=[('Xz', c0)], ndma=1)
    S.barrier()

    def bcast_tile(dst, colsrc, j, key):
        for kc in range(8):
            src = colsrc[:, kc, j:j + 1] if j is not None else colsrc[:, kc:kc + 1]
            tmp = BCT[kc % 2]
            S.add('dve', lambda e, src=src, tmp=tmp: e.tensor_copy(out=tmp, in_=src.to_broadcast([128, 128])),
                  reads=[key], writes=[('bct', kc % 2)])
            pb = bank(kc % 2)
            S.add('pe', lambda e, tmp=tmp, pb=pb: e.matmul(pb[:, 0:128], lhsT=tmp, rhs=C['ident_f'], start=True, stop=True),
                  reads=[('bct', kc % 2)], writes=[('bk', kc % 2)])
            S.add('act', lambda e, pb=pb, kc=kc: e.copy(out=dst[:, kc * 128:(kc + 1) * 128], in_=pb[:, 0:128]),
                  reads=[('bk', kc % 2)], writes=[('btile', id(dst))])

    BCT = [A.tile([128, 128], F32, 'bct') for _ in range(2)]
    PERSIST = A.mark()

    def _layer(l):
        A.reset(PERSIST)
        aw = [A.tile([128, 8, 768], F32, 'aw') for _ in range(2)]
        adb = A.tile([128, 48], F32, 'adb')
        S.add('act', lambda e: [e.dma_start(out=adb, in_=ada_bT[l])], writes=['adb'], ndma=1)
        for cch in range(8):
            buf = aw[cch % 2]
            S.add(dmaq(), lambda e, buf=buf, cch=cch: [e.dma_start(
                out=buf, in_=ada_w[l, :, cch * 768:(cch + 1) * 768].rearrange("(kc p) n -> p kc n", p=128))],
                writes=[('aw', cch % 2)], ndma=1)
            for mi in range(6):
                mm = cch * 6 + mi
                for kc in range(8):
                    S.add('pe', lambda e, buf=buf, kc=kc, mm=mm, mi=mi: e.matmul(
                        bank(0)[:, mm * 2:mm * 2 + 2], lhsT=buf[:, kc, mi * 128:(mi + 1) * 128], rhs=scc[:, kc, :],
                        start=(kc == 0), stop=(kc == 7)), reads=[('aw', cch % 2), 'scc'], writes=['bk0'])
        S.add('dve', lambda e: e.tensor_tensor(out=modT, in0=bank(0)[:, 0:96].rearrange("p (m j) -> p m j", j=2),
                                               in1=adb.unsqueeze(2).to_broadcast([128, 48, 2]), op=ALU.add),
              reads=['bk0', 'adb'], writes=['modT'])
        for (sdst, gsrc, moff, nm) in ((s1, n1g, 8, 's1'), (s2, n2g, 32, 's2')):
            S.add('sp', lambda e, gsrc=gsrc: [e.dma_start(out=gcol, in_=gsrc[l])], writes=['gcol'], ndma=1)
            S.add('dve', lambda e, sdst=sdst, moff=moff: e.tensor_scalar(
                out=sdst, in0=modT[:, moff:moff + 8, :], scalar1=1.0, scalar2=None, op0=ALU.add),
                reads=['modT'], writes=[nm])
            S.add('dve', lambda e, sdst=sdst: e.tensor_tensor(
                out=sdst, in0=sdst, in1=gcol.unsqueeze(2).to_broadcast([128, 8, 2]), op=ALU.mult),
                reads=[nm, 'gcol'], writes=[nm])
        bcast_tile(BT['g1x'], modT[:, 16:24, :], 0, 'modT')
        bcast_tile(BT['g1c'], modT[:, 16:24, :], 1, 'modT')
        bcast_tile(BT['g2x'], modT[:, 40:48, :], 0, 'modT')
        bcast_tile(BT['g2c'], modT[:, 40:48, :], 1, 'modT')
        bcast_tile(BT['s2x'], s2, 0, 's2')
        bcast_tile(BT['s2c'], s2, 1, 's2')
        bcast_tile(BT['h2x'], modT[:, 24:32, :], 0, 'modT')
        bcast_tile(BT['h2c'], modT[:, 24:32, :], 1, 'modT')
        S.add('sp', lambda e: [e.dma_start(out=rwp, in_=r_w[l])], writes=['rwp'], ndma=1)
        S.add('sp', lambda e: [e.dma_start(out=rbt, in_=r_b[l])], writes=['rbt'], ndma=1)
        for j in range(2):
            S.add('dve', lambda e, j=j: e.tensor_tensor(out=rwx[:, :, j, :], in0=rwp,
                                                        in1=s2[:, :, j:j + 1].to_broadcast([128, 8, NE]), op=ALU.mult),
                  reads=['rwp', 's2'], writes=[('rwx', j)])
            for kc in range(8):
                S.add('pe', lambda e, j=j, kc=kc: e.matmul(bank(2)[0:1, j * NE:(j + 1) * NE], lhsT=modT[:, 24 + kc, j:j + 1],
                                                          rhs=rwp[:, kc, :], start=(kc == 0), stop=(kc == 7)),
                      reads=['modT', 'rwp'], writes=['bk2'])
            S.add('dve', lambda e, j=j: e.tensor_tensor(out=rcst[:, j, :], in0=bank(2)[0:1, j * NE:(j + 1) * NE], in1=rbt, op=ALU.add),
                  reads=['bk2', 'rbt'], writes=[('rcst', j)])
        S.barrier()

        if 'stop_P0' in dbg:
            return
        A.reset(PERSIST)
        win = A.tile([128, 8, 3328], BF16, 'win')
        for kc in range(8):
            S.add('pool', lambda e, kc=kc: [e.dma_start(out=win[:, kc, :], in_=w_in[l, kc * 128:(kc + 1) * 128, :])],
                  writes=[('win', kc)], ndma=1)
        WIN = [('win', kc) for kc in range(8)]
        xt_p1 = [A.tile([128, D], F32, 'xt') for _ in range(2)]
        xn = [A.tile([128, D], BF16, 'xn') for _ in range(2)]
        junk_p1 = A.tile([128, D], BF16, 'junk')
        ss_p1 = [A.tile([128, 1], F32, 'ss') for _ in range(2)]
        hT = [A.tile([128, 8, 512], BF16, 'hT') for _ in range(2)]
        fsb = [A.tile([128, 512], BF16, 'fsb') for _ in range(2)]
        vst = [A.tile([128, 4, 128], BF16, 'vst') for _ in range(2)]
        qst = [A.tile([64, 4, 8, 128], BF16, 'qst') for _ in range(2)]
        gst = [A.tile([128, 4, 16, 128], BF16, 'gst') for _ in range(2)]
        qraw = [A.tile([128, 512], BF16, 'qraw') for _ in range(2)]
        for b_ in range(2):
            S.add('pool', lambda e, b_=b_: e.memset(qraw[b_], 0.0), writes=[('qraw', b_)])
        qro = [A.tile([64, 512], BF16, 'qro') for _ in range(2)]
        rt1 = [A.tile([64, 512], F32, 'rt1') for _ in range(2)]
        rt2 = [A.tile([64, 512], F32, 'rt2') for _ in range(2)]
        cst = A.tile([64, 512], F32, 'cst')
        snt = A.tile([64, 512], F32, 'snt')
        nst = (T + 511) // 512
        it = 0
        ih = 0
        for st in range(nst):
            if 'one_st' in dbg and st > 0:
                break
            t0 = st * 512
            ntok = min(512, T - t0)
            ntile = ntok // 128
            isx = t0 < TX
            j = 0 if isx else 1
            hb = hT[st % 2]
            HK = ('hT', st % 2)
            for ti in range(ntile):
                b = it % 2
                it += 1
                r0 = t0 + ti * 128
                S.add(dmaq(), lambda e, b=b, r0=r0: [e.dma_start(out=xt_p1[b], in_=xres[r0:r0 + 128, :])],
                      writes=[('xt', b)], ndma=1)
                S.add('act', lambda e, b=b: e.activation(out=junk_p1, in_=xt_p1[b], func=AF.Square, accum_out=ss_p1[b]),
                      reads=[('xt', b)], writes=[('ss', b), 'junk'])
                S.add('dve', lambda e, b=b: e.tensor_scalar(out=ss_p1[b], in0=ss_p1[b], scalar1=1.0 / D, scalar2=EPS,
                                                            op0=ALU.mult, op1=ALU.add), reads=[('ss', b)], writes=[('ss', b)])
                S.add('act', lambda e, b=b: e.sqrt(out=ss_p1[b], in_=ss_p1[b]), reads=[('ss', b)], writes=[('ss', b)])
                S.add('dve', lambda e, b=b: e.reciprocal(out=ss_p1[b], in_=ss_p1[b]), reads=[('ss', b)], writes=[('ss', b)])
                S.add('act', lambda e, b=b: e.activation(out=xn[b], in_=xt_p1[b], func=AF.Copy, scale=ss_p1[b][:, 0:1]),
                      reads=[('xt', b), ('ss', b)], writes=[('xn', b)])
                pb = pbf[b]
                for kc in range(8):
                    S.add('pe', lambda e, b=b, kc=kc, pb=pb: e.transpose(out=pb[:, kc * 128:(kc + 1) * 128],
                                                                        in_=xn[b][:, kc * 128:(kc + 1) * 128], identity=C['ident_bf']),
                          reads=[('xn', b), 'ident_bf'], writes=[('pbf', b)])
                S.add('dve', lambda e, pb=pb, j=j, hb=hb, ti=ti: e.tensor_tensor(
                    out=hb[:, :, ti * 128:(ti + 1) * 128], in0=pb.rearrange("p (k t) -> p k t", t=128),
                    in1=s1[:, :, j:j + 1].to_broadcast([128, 8, 128]), op=ALU.mult),
                    reads=[('pbf', b), 's1'], writes=[HK])
                S.add('pool', lambda e, j=j, hb=hb, ti=ti: e.tensor_tensor(
                    out=hb[:, :, ti * 128:(ti + 1) * 128], in0=hb[:, :, ti * 128:(ti + 1) * 128],
                    in1=modT[:, 0:8, j:j + 1].to_broadcast([128, 8, 128]), op=ALU.add),
                    reads=[HK, 'modT'], writes=[HK])
                if 'p1_norm' in dbg:
                    continue
                bk = bank(ti % 2)
                for kc in range(8):
                    S.add('pe', lambda e, hb=hb, ti=ti, kc=kc, bk=bk: e.matmul(
                        bk, lhsT=hb[:, kc, ti * 128:(ti + 1) * 128], rhs=win[:, kc, 0:512], start=(kc == 0), stop=(kc == 7)),
                        reads=[HK] + WIN, writes=[('bk', ti % 2)])
                fb = fsb[ti % 2]
                S.add('act', lambda e, fb=fb, bk=bk: e.copy(out=fb, in_=bk), reads=[('bk', ti % 2)], writes=[('fsb', ti % 2)])
                S.add('sp', lambda e, fb=fb, r0=r0: [e.dma_start(out=f_tm[r0:r0 + 128, :], in_=fb)],
                      reads=[('fsb', ti % 2)], writes=[('f_tm', r0)], ndma=1)
                bk = bank(2 + ti % 2)
                for kc in range(8):
                    S.add('pe', lambda e, hb=hb, ti=ti, kc=kc, bk=bk: e.matmul(
                        bk[:, 0:128], lhsT=hb[:, kc, ti * 128:(ti + 1) * 128], rhs=win[:, kc, 1152:1280], start=(kc == 0), stop=(kc == 7)),
                        reads=[HK] + WIN, writes=[('bk', 2 + ti % 2)])
                S.add('dve', lambda e, bk=bk, ti=ti, st=st: e.tensor_copy(out=vst[st % 2][:, ti, :], in_=bk[:, 0:128]),
                      reads=[('bk', 2 + ti % 2)], writes=[('vst', st % 2)])
            if 'p1_norm' not in dbg:
                S.add('sp', lambda e, st=st, ntile=ntile: [e.dma_start(out=v_blk[:, st * 4:st * 4 + ntile, :], in_=vst[st % 2][:, 0:ntile, :])],
                      reads=[('vst', st % 2)], writes=[('v_blk', st)], ndma=1)
            if 'p1_norm' in dbg or 'p1_fv' in dbg:
                continue
            if isx:
                S.add('sp', lambda e, t0=t0: [e.dma_start(out=cst, in_=cd['cosT'][:, t0:t0 + 512])], writes=['cst'], ndma=1)
                S.add('act', lambda e, t0=t0: [e.dma_start(out=snt, in_=cd['sinT'][:, t0:t0 + 512])], writes=['snt'], ndma=1)
            for hh in range(10):
                col0 = 512 + hh * 64
                b = ih % 2
                ih += 1
                bk = bank(4)
                for kc in range(8):
                    S.add('pe', lambda e, hb=hb, kc=kc, col0=col0, bk=bk, ntok=ntok: e.matmul(
                        bk[0:64, 0:ntok], lhsT=win[:, kc, col0:col0 + 64], rhs=hb[:, kc, 0:ntok], start=(kc == 0), stop=(kc == 7)),
                        reads=[HK] + WIN, writes=['bk4'])
                QSK = ('qst', st % 2)
                if hh < 8:
                    fin = qst[st % 2][:, 0:ntile, hh, :]
                    fink = [QSK]
                else:
                    fin = qro[b][:, 0:ntok].rearrange("p (a t) -> p a t", t=128)
                    fink = [('qro', b)]
                if isx:
                    S.add('act', lambda e, b=b, bk=bk: e.copy(out=qraw[b][0:64, :], in_=bk[0:64, :]), reads=['bk4'], writes=[('qraw', b)])
                    bk5 = bank(5)
                    S.add('pe', lambda e, b=b, bk5=bk5: e.matmul(bk5, lhsT=C['rotm'], rhs=qraw[b], start=True, stop=True),
                          reads=[('qraw', b), 'rotm'], writes=['bk5'])
                    S.add('act', lambda e, b=b, bk=bk: e.copy(out=rt1[b], in_=bk[0:64, :]), reads=['bk4'], writes=[('rt1', b)])
                    S.add('act', lambda e, b=b, bk5=bk5: e.copy(out=rt2[b], in_=bk5[0:64, :]), reads=['bk5'], writes=[('rt2', b)])
                    S.add('pool', lambda e, b=b: e.tensor_tensor(out=rt1[b], in0=rt1[b], in1=cst, op=ALU.mult),
                          reads=[('rt1', b), 'cst'], writes=[('rt1', b)])
                    S.add('pool', lambda e, b=b: e.tensor_tensor(out=rt2[b], in0=rt2[b], in1=snt, op=ALU.mult),
                          reads=[('rt2', b), 'snt'], writes=[('rt2', b)])
                    S.add('pool', lambda e, b=b, fin=fin: e.tensor_tensor(
                        out=fin, in0=rt1[b].rearrange("p (a t) -> p a t", t=128), in1=rt2[b].rearrange("p (a t) -> p a t", t=128), op=ALU.add),
                        reads=[('rt1', b), ('rt2', b)], writes=fink)
                else:
                    S.add('act', lambda e, bk=bk, ntok=ntok, fin=fin: e.copy(out=fin, in_=bk[0:64, 0:ntok].rearrange("p (a t) -> p a t", t=128)),
                          reads=['bk4'], writes=fink)
                if hh >= 8:
                    S.add(dmaq(), lambda e, b=b, hh=hh, t0=t0, ntok=ntok: [e.dma_start(out=kT_all[:, hh - 8, t0:t0 + ntok], in_=qro[b][:, 0:ntok])],
                          reads=[('qro', b)], writes=[('kT_all', hh, st)], ndma=1)
                elif hh == 7:
                    S.add(dmaq(), lambda e, st=st, ntile=ntile: [e.dma_start(
                        out=q_blk[st * 4:st * 4 + ntile].rearrange("b d h t -> d b (h t)"),
                        in_=qst[st % 2][:, 0:ntile, :, :].rearrange("p a h t -> p a (h t)"))],
                        reads=[QSK], writes=[('q_blk', st)], ndma=1)
            if 'p1_qk' in dbg:
                continue
            for m in range(16):
                col0 = 1280 + m * 128
                bk = bank(m % 2)
                for kc in range(8):
                    S.add('pe', lambda e, hb=hb, kc=kc, col0=col0, bk=bk, ntok=ntok: e.matmul(
                        bk[:, 0:ntok], lhsT=win[:, kc, col0:col0 + 128], rhs=hb[:, kc, 0:ntok], start=(kc == 0), stop=(kc == 7)),
                        reads=[HK] + WIN, writes=[('bk', m % 2)])
                S.add('act', lambda e, bk=bk, ntok=ntok, ntile=ntile, m=m, st=st: e.activation(
                    out=gst[st % 2][:, 0:ntile, m, :], in_=bk[:, 0:ntok].rearrange("p (a t) -> p a t", t=128), func=AF.Sigmoid),
                    reads=[('bk', m % 2)], writes=[('gst', st % 2)])
            S.add(dmaq(), lambda e, st=st, ntile=ntile: [e.dma_start(
                out=g_blk[st * 4:st * 4 + ntile].rearrange("b p m t -> p b (m t)"),
                in_=gst[st % 2][:, 0:ntile, :, :].rearrange("p a m t -> p a (m t)"))],
                reads=[('gst', st % 2)], writes=[('g_blk', st)], ndma=1)
        S.barrier()

        if 'stop_P1' in dbg:
            return
        A.reset(PERSIST)
        wao = A.tile([64, 8, D], BF16, 'wao')
        S.add('pool', lambda e: [e.dma_start(out=wao, in_=w_ao[l].rearrange("(h d) n -> d h n", d=64))], writes=['wao'], ndma=1)
        kall = A.tile([64, 2, T], BF16, 'kall')
        S.add('sp', lambda e: [e.dma_start(out=kall, in_=kT_all)], writes=['kall'], ndma=1)
        vall = A.tile([128, NT, 128], BF16, 'vall')
        S.add('act', lambda e: [e.dma_start(out=vall, in_=v_blk)], writes=['vall'], ndma=1)
        skr = A.tile([64, 8], F32, 'skr')
        S.add('sp', lambda e: [e.dma_start(out=skr, in_=sink[l:l + 1, :].to_broadcast([64, 8]))], writes=['skr'], ndma=1)
        S.add('act', lambda e: e.activation(out=skr, in_=skr, func=AF.Exp), reads=['skr'], writes=['skr'])
        esk = A.tile([64, 8, 128], F32, 'esk')
        S.add('dve', lambda e: e.tensor_copy(out=esk, in_=skr.unsqueeze(2).to_broadcast([64, 8, 128])), reads=['skr'], writes=['esk'])
        qb_sb = [A.tile([64, 8, 128], BF16, 'qb') for _ in range(2)]
        pT = [A.tile([128, 512], BF16, 'pT') for _ in range(3)]
        rec = A.tile([64, 512], F32, 'rec')
        pos_ = A.tile([64, 512], F32, 'pos_')
        atT = [A.tile([64, 8, 128], BF16, 'atT') for _ in range(2)]
        gab = [A.tile([128, 8, 128], BF16, 'gab') for _ in range(2)]
        t2b = [A.tile([128, 8, 128], BF16, 't2b') for _ in range(2)]
        ip = 0
        for qb in range(NT):
            b = qb % 2
            r0 = qb * 128
            isx = qb < NTX
            S.add('sp', lambda e, b=b, qb=qb: [e.dma_start(out=qb_sb[b], in_=q_blk[qb])], writes=[('qb', b)], ndma=1)
            S.add('act', lambda e, b=b, qb=qb: [e.dma_start(out=gab[b], in_=g_blk[qb][:, 8:16, :])], writes=[('gab', b)], ndma=1)
            chunks = []
            if isx:
                lo = max(qb - 1, 0)
                hi = min(qb + 1, NTX - 1)
                for kbk in range(lo, hi + 1):
                    ci = kbk
                    mk = None if kbk == qb else ('mask_prev' if kbk < qb else 'mask_next')
                    chunks.append(('loc', ci, mk))
            chunks.append(('loc', NTX, None))
            chunks.append(('loc', NTX + 1, None))
            for kv in range(2):
                po = bank(2 + kv * 2)
                pd = bank(3 + kv * 2)
                POK = ('bk', 2 + kv * 2)
                PDK = ('bk', 3 + kv * 2)
                for cidx, (kind, ci, mk) in enumerate(chunks):
                    sb = bank(cidx % 2)
                    SBK = ('bk', cidx % 2)
                    kl = kall[:, kv, ci * 128:(ci + 1) * 128]
                    vl = vall[:, ci, kv * 64:(kv + 1) * 64]
                    rk = ['kall']
                    rv = ['vall']
                    S.add('pe', lambda e, sb=sb, kl=kl, b=b, kv=kv: e.matmul(
                        sb.rearrange("p (h t) -> p h t", t=128), lhsT=kl, rhs=qb_sb[b][:, kv * 4:(kv + 1) * 4, :], start=True, stop=True),
                        reads=rk + [('qb', b)], writes=[SBK])
                    pt = pT[ip % 3]
                    PTK = ('pT', ip % 3)
                    ip += 1
                    S.add('act', lambda e, pt=pt, sb=sb: e.activation(out=pt, in_=sb, func=AF.Exp, scale=0.125),
                          reads=[SBK], writes=[PTK])
                    if mk is not None:
                        S.add('pool', lambda e, pt=pt, mk=mk: e.tensor_tensor(
                            out=pt.rearrange("p (h t) -> p h t", t=128), in0=pt.rearrange("p (h t) -> p h t", t=128),
                            in1=C[mk].unsqueeze(1).to_broadcast([128, 4, 128]), op=ALU.mult),
                            reads=[PTK, mk], writes=[PTK])
                    first = cidx == 0
                    last = cidx == len(chunks) - 1
                    S.add('pe', lambda e, po=po, vl=vl, pt=pt, first=first, last=last: e.matmul(
                        po[0:64, :], lhsT=vl, rhs=pt, start=first, stop=last), reads=rv + [PTK], writes=[POK])
                    S.add('pe', lambda e, pd=pd, pt=pt, first=first, last=last: e.matmul(
                        pd[0:64, :], lhsT=ones_bf[:, 0:64], rhs=pt, start=first, stop=last), reads=['ones_bf', PTK], writes=[PDK])
                S.add('act', lambda e, pd=pd: e.copy(out=rec, in_=pd[0:64, :]), reads=[PDK], writes=['rec'])
                S.add('act', lambda e, po=po: e.copy(out=pos_, in_=po[0:64, :]), reads=[POK], writes=['pos_'])
                S.add('dve', lambda e, kv=kv: e.tensor_tensor(
                    out=rec, in0=rec, in1=esk[:, kv * 4:(kv + 1) * 4, :].rearrange("p h t -> p (h t)"), op=ALU.add),
                    reads=['rec', 'esk'], writes=['rec'])
                S.add('dve', lambda e: e.reciprocal(out=rec, in_=rec), reads=['rec'], writes=['rec'])
                S.add('pool', lambda e, kv=kv, b=b: e.tensor_tensor(
                    out=atT[b][:, kv * 4:(kv + 1) * 4, :].rearrange("p h t -> p (h t)"), in0=pos_, in1=rec, op=ALU.mult),
                    reads=['pos_', 'rec'], writes=[('atT', b)])
            for m in range(8):
                ob = bank(m % 2)
                OBK = ('bk', m % 2)
                for h in range(8):
                    S.add('pe', lambda e, ob=ob, h=h, m=m, b=b: e.matmul(
                        ob[:, 0:128], lhsT=wao[:, h, m * 128:(m + 1) * 128], rhs=atT[b][:, h, :], start=(h == 0), stop=(h == 7)),
                        reads=['wao', ('atT', b)], writes=[OBK])
                S.add('dve', lambda e, ob=ob, m=m, b=b: e.tensor_tensor(
                    out=t2b[b][:, m, :], in0=ob[:, 0:128], in1=gab[b][:, m, :], op=ALU.mult),
                    reads=[OBK, ('gab', b)], writes=[('t2b', b)])
            S.add('sp', lambda e, b=b, qb=qb: [e.dma_start(out=t2_blk[qb], in_=t2b[b])],
                  reads=[('t2b', b)], writes=[('t2_blk', qb)], ndma=1)
        S.barrier()

        if 'stop_P2' in dbg:
            return
        A.reset(PERSIST)
        fmT = [A.tile([128, T], BF16, 'fmT') for _ in range(4)]
        MK3 = A.mark()
        Dt = [A.tile([128, 8, 512], BF16, 'Dt') for _ in range(2)]
        Zc = [A.tile([128, 2, 8, 512], BF16, 'Zc') for _ in range(2)]
        tt1 = [A.tile([128, 512], F32, 'tt1') for _ in range(2)]
        tt2 = [A.tile([128, 512], F32, 'tt2') for _ in range(2)]
        tt3 = [A.tile([128, 512], F32, 'tt3') for _ in range(2)]
        tt4 = [A.tile([128, 512], F32, 'tt4') for _ in range(2)]
        fview = f_tm[0:TX, :].rearrange("(n1 n2) c -> n1 n2 c", n2=64)
        for n2c in range(8):
            b = n2c % 2
            S.add(dmaq(), lambda e, b=b, n2c=n2c: [e.dma_start(out=Dt[b], in_=fview[:, n2c * 8:(n2c + 1) * 8, :])],
                  writes=[('Dt', b)], ndma=1)
            for jj in range(8):
                n2 = n2c * 8 + jj
                bb = jj % 2
                pr = bank(bb * 2)
                ps = bank(bb * 2 + 1)
                S.add('pe', lambda e, pr=pr, b=b, jj=jj: e.matmul(pr, lhsT=C['c128'], rhs=Dt[b][:, jj, :], start=True, stop=True),
                      reads=[('Dt', b), 'c128'], writes=[('bk', bb * 2)])
                S.add('pe', lambda e, ps=ps, b=b, jj=jj: e.matmul(ps, lhsT=C['s128'], rhs=Dt[b][:, jj, :], start=True, stop=True),
                      reads=[('Dt', b), 's128'], writes=[('bk', bb * 2 + 1)])
                S.add('act', lambda e, pr=pr, bb=bb, n2=n2: e.activation(out=tt1[bb], in_=pr, func=AF.Copy, scale=C['twr'][:, n2:n2 + 1]),
                      reads=[('bk', bb * 2), 'twr'], writes=[('tt1', bb)])
                S.add('act', lambda e, pr=pr, bb=bb, n2=n2: e.activation(out=tt2[bb], in_=pr, func=AF.Copy, scale=C['twi'][:, n2:n2 + 1]),
                      reads=[('bk', bb * 2), 'twi'], writes=[('tt2', bb)])
                S.add('act', lambda e, ps=ps, bb=bb, n2=n2: e.activation(out=tt3[bb], in_=ps, func=AF.Copy, scale=C['ntwi'][:, n2:n2 + 1]),
                      reads=[('bk', bb * 2 + 1), 'ntwi'], writes=[('tt3', bb)])
                S.add('act', lambda e, ps=ps, bb=bb, n2=n2: e.activation(out=tt4[bb], in_=ps, func=AF.Copy, scale=C['twr'][:, n2:n2 + 1]),
                      reads=[('bk', bb * 2 + 1), 'twr'], writes=[('tt4', bb)])
                S.add('dve', lambda e, bb=bb, b=b, jj=jj: e.tensor_tensor(out=Zc[b][:, 0, jj, :], in0=tt1[bb], in1=tt3[bb], op=ALU.add),
                      reads=[('tt1', bb), ('tt3', bb)], writes=[('Zc', b)])
                S.add('pool', lambda e, bb=bb, b=b, jj=jj: e.tensor_tensor(out=Zc[b][:, 1, jj, :], in0=tt2[bb], in1=tt4[bb], op=ALU.add),
                      reads=[('tt2', bb), ('tt4', bb)], writes=[('Zc', b)])
            for ri in range(2):
                S.add(dmaq(), lambda e, b=b, ri=ri, n2c=n2c: [e.dma_start(out=Zd[ri, :, n2c * 8:(n2c + 1) * 8, :], in_=Zc[b][:, ri, :, :])],
                      reads=[('Zc', b)], writes=[('Zd', ri, n2c)], ndma=1)
        S.barrier()
        if 'p3_s1' in dbg:
            return
        A.reset(MK3)
        Pg = A.tile([128, 2, 2, TX], BF16, 'Pg')
        Zs = [A.tile([128, 8, 256], BF16, 'Zs') for _ in range(2)]
        iz = 0
        for gp in range(2):
            for k1c in range(16):
                b = iz % 2
                iz += 1
                for ri in range(2):
                    S.add(dmaq(), lambda e, b=b, ri=ri, k1c=k1c, gp=gp: [e.dma_start(
                        out=Zs[b][ri * 64:(ri + 1) * 64, :, :],
                        in_=Zd[ri, k1c * 8:(k1c + 1) * 8, :, gp * 256:(gp + 1) * 256].rearrange("k n c -> n k c"))],
                        writes=[('Zs', b, ri)], ndma=1)
                for gi in range(2):
                    pz = pq[gi]
                    PZK = ('pq', gi)
                    for kj in range(8):
                        S.add('pe', lambda e, pz=pz, b=b, kj=kj, gi=gi: e.matmul(pz[:, kj * 128:(kj + 1) * 128], lhsT=Zs[b][:, kj, gi * 128:(gi + 1) * 128],
                                                                                rhs=C['r2'], start=True, stop=True),
                              reads=[('Zs', b, 0), ('Zs', b, 1), 'r2'], writes=[PZK])
                    for ri in range(2):
                        for hf in range(2):
                            outv = Pg[:, gi, ri, :].rearrange("c (k1 k2) -> c k1 k2", k2=64)[:, k1c * 8 + hf * 4:k1c * 8 + hf * 4 + 4, :]
                            inv = pz[:, hf * 512:(hf + 1) * 512].rearrange("c (k1 r k2) -> c k1 r k2", r=2, k2=64)[:, :, ri, :]
                            if (ri + hf) % 2 == 0:
                                S.add('act', lambda e, outv=outv, inv=inv: e.copy(out=outv, in_=inv), reads=[PZK], writes=[('Pg', gi, ri)])
                            else:
                                S.add('dve', lambda e, outv=outv, inv=inv: e.tensor_copy(out=outv, in_=inv), reads=[PZK], writes=[('Pg', gi, ri)])
            for gi in range(2):
                g = gp * 2 + gi
                for tc in range(16):
                    bk = bank(4 + tc % 2)
                    BKK = ('bk', 4 + tc % 2)
                    S.add('pe', lambda e, bk=bk, tc=tc, gi=gi: e.matmul(bk, lhsT=C['cc'], rhs=Pg[:, gi, 0, tc * 512:(tc + 1) * 512], start=True, stop=False),
                          reads=[('Pg', gi, 0), 'cc'], writes=[BKK])
                    S.add('pe', lambda e, bk=bk, tc=tc, gi=gi: e.matmul(bk, lhsT=C['nsc'], rhs=Pg[:, gi, 1, tc * 512:(tc + 1) * 512], start=False, stop=True),
                          reads=[('Pg', gi, 1), 'nsc'], writes=[BKK])
                    if tc % 2 == 0:
                        S.add('act', lambda e, bk=bk, tc=tc, g=g: e.copy(out=fmT[g][:, tc * 512:(tc + 1) * 512], in_=bk), reads=[BKK], writes=[('fmT', g)])
                    else:
                        S.add('dve', lambda e, bk=bk, tc=tc, g=g: e.tensor_copy(out=fmT[g][:, tc * 512:(tc + 1) * 512], in_=bk), reads=[BKK], writes=[('fmT', g)])
        if 'p3_s2' in dbg:
            S.barrier()
            return
        Dc = A.tile([128, 2, 512], BF16, 'Dc')
        Pc = A.tile([128, 2, TC], BF16, 'Pc')
        S.add('sp', lambda e: [e.dma_start(out=Dc, in_=f_tm[TX:T, :].rearrange("(c p) f -> p c f", p=128))], writes=['Dc'], ndma=1)
        for g in range(4):
            for ri, cn in enumerate(('c256', 's256')):
                bk = bank(ri)
                for ch in range(2):
                    S.add('pe', lambda e, bk=bk, ch=ch, g=g, cn=cn: e.matmul(bk[:, 0:TC], lhsT=Dc[:, ch, g * 128:(g + 1) * 128], rhs=C[cn][:, ch, :],
                                                                            start=(ch == 0), stop=(ch == 1)),
                          reads=['Dc', cn], writes=[('bk', ri)])
                S.add('act', lambda e, bk=bk, ri=ri: e.copy(out=Pc[:, ri, :], in_=bk[:, 0:TC]), reads=[('bk', ri)], writes=[('Pc', ri)])
            bk = bank(2)
            S.add('pe', lambda e, bk=bk: e.matmul(bk[:, 0:TC], lhsT=C['cc'], rhs=Pc[:, 0, :], start=True, stop=False), reads=[('Pc', 0), 'cc'], writes=['bk2'])
            S.add('pe', lambda e, bk=bk: e.matmul(bk[:, 0:TC], lhsT=C['nsc'], rhs=Pc[:, 1, :], start=False, stop=True), reads=[('Pc', 1), 'nsc'], writes=['bk2'])
            S.add('dve', lambda e, bk=bk, g=g: e.tensor_copy(out=fmT[g][:, TX:T], in_=bk[:, 0:TC]), reads=['bk2'], writes=[('fmT', g)])
        S.barrier()

        if 'stop_P3' in dbg:
            return
        A.reset(MK3)
        wfo = A.tile([128, 4, D], BF16, 'wfo')
        S.add('pool', lambda e: [e.dma_start(out=wfo, in_=w_fo[l].rearrange("(g p) n -> p g n", p=128))], writes=['wfo'], ndma=1)
        wou = A.tile([128, 8, D], BF16, 'wou')
        S.add('pool', lambda e: [e.dma_start(out=wou, in_=w_out[l].rearrange("(k p) n -> p k n", p=128))], writes=['wou'], ndma=1)
        sgf = [A.tile([128, 4, 8, 128], BF16, 'sgf') for _ in range(2)]
        t2s = [A.tile([128, 4, 8, 128], BF16, 't2s')] * 2
        yT = [A.tile([128, 8, 512], BF16, 'yT')] * 2
        ut = A.tile([128, 512], F32, 'ut')
        xt_p4 = [A.tile([128, D], F32, 'xt4') for _ in range(2)]
        xo_p4 = [A.tile([128, D], F32, 'xo4')] * 2
        it = 0
        for st in range(nst):
            t0 = st * 512
            ntok = min(512, T - t0)
            ntile = ntok // 128
            isx = t0 < TX
            b = st % 2
            S.add('sp', lambda e, b=b, st=st, ntile=ntile: [e.dma_start(
                out=sgf[b][:, 0:ntile, :, :].rearrange("p a m t -> p a (m t)"),
                in_=g_blk[st * 4:st * 4 + ntile, :, 0:8, :].rearrange("b p m t -> p b (m t)"))], writes=[('sgf', b)], ndma=1)
            S.add('act', lambda e, b=b, st=st, ntile=ntile: [e.dma_start(
                out=t2s[b][:, 0:ntile, :, :].rearrange("p a m t -> p a (m t)"),
                in_=t2_blk[st * 4:st * 4 + ntile].rearrange("b p m t -> p b (m t)"))], writes=[('t2s', 0)], ndma=1)
            for m in range(8):
                bk = bank(m % 2)
                BKK = ('bk', m % 2)
                for g in range(4):
                    if isx:
                        rhs = fmT[g][:, 0:TX].rearrange("c (k1 k2) -> c k2 k1", k2=64)[:, st * 4:st * 4 + 4, :]
                    else:
                        rhs = fmT[g][:, TX:T].rearrange("c (a t) -> c a t", t=128)
                    S.add('pe', lambda e, bk=bk, g=g, m=m, rhs=rhs, ntok=ntok: e.matmul(
                        bk[:, 0:ntok].rearrange("p (a t) -> p a t", t=128), lhsT=wfo[:, g, m * 128:(m + 1) * 128], rhs=rhs, start=(g == 0), stop=(g == 3)),
                        reads=['wfo', ('fmT', g)], writes=[BKK])
                S.add('dve', lambda e, bk=bk, b=b, m=m, ntok=ntok, ntile=ntile: e.tensor_tensor(
                    out=ut[:, 0:ntok].rearrange("p (a t) -> p a t", t=128), in0=bk[:, 0:ntok].rearrange("p (a t) -> p a t", t=128),
                    in1=sgf[b][:, 0:ntile, m, :], op=ALU.mult), reads=[BKK, ('sgf', b)], writes=['ut'])
                S.add('pool', lambda e, b=b, m=m, ntok=ntok, ntile=ntile: e.tensor_tensor(
                    out=yT[b][:, m, 0:ntok].rearrange("p (a t) -> p a t", t=128), in0=ut[:, 0:ntok].rearrange("p (a t) -> p a t", t=128),
                    in1=t2s[b][:, 0:ntile, m, :], op=ALU.add), reads=['ut', ('t2s', 0)], writes=[('yT', 0)])
            for ti in range(ntile):
                xb = it % 2
                it += 1
                r0 = t0 + ti * 128
                S.add(dmaq(), lambda e, xb=xb, r0=r0: [e.dma_start(out=xt_p4[xb], in_=xres[r0:r0 + 128, :])], writes=[('xt4', xb)], ndma=1)
                gb = BT['g1x'] if isx else BT['g1c']
                for n in range(2):
                    bk = bank(2 + n)
                    BKK = ('bk', 2 + n)
                    for kc in range(8):
                        S.add('pe', lambda e, bk=bk, b=b, kc=kc, ti=ti, n=n: e.matmul(
                            bk, lhsT=yT[b][:, kc, ti * 128:(ti + 1) * 128], rhs=wou[:, kc, n * 512:(n + 1) * 512], start=(kc == 0), stop=(kc == 7)),
                            reads=[('yT', 0), 'wou'], writes=[BKK])
                    S.add('act', lambda e, bk=bk, xb=xb, n=n: e.copy(out=xo_p4[xb][:, n * 512:(n + 1) * 512], in_=bk),
                          reads=[BKK], writes=[('xo4', 0, n)])
                    S.add('dve', lambda e, xb=xb, n=n, gb=gb: e.tensor_tensor(
                        out=xo_p4[xb][:, n * 512:(n + 1) * 512], in0=xo_p4[xb][:, n * 512:(n + 1) * 512], in1=gb[:, n * 512:(n + 1) * 512], op=ALU.mult),
                        reads=[('xo4', 0, n), ('btile', id(gb))], writes=[('xo4', 0, n)])
                    S.add('pool', lambda e, xb=xb, n=n: e.tensor_tensor(
                        out=xo_p4[xb][:, n * 512:(n + 1) * 512], in0=xo_p4[xb][:, n * 512:(n + 1) * 512], in1=xt_p4[xb][:, n * 512:(n + 1) * 512], op=ALU.add),
                        reads=[('xo4', 0, n), ('xt4', xb)], writes=[('xo4', 0, n)])
                S.add(dmaq(), lambda e, xb=xb, r0=r0: [e.dma_start(out=xres[r0:r0 + 128, :], in_=xo_p4[xb])],
                      reads=[('xo4', 0, 0), ('xo4', 0, 1)], writes=[('xres', r0)], ndma=1)
        S.barrier()

        if 'stop_P4' in dbg:
            return
        if 'skip_moe' in dbg:
            return
        A.reset(PERSIST)
        LG = A.tile([128, NT, NE], F32, 'LG')
        V8 = A.tile([128, NT, 8], F32, 'V8')
        MK = A.tile([128, NT, NE], BF16, 'MK')
        G4 = A.tile([128, NT, 4], F32, 'G4')
        DESTi = A.tile([128, NT, 4], I32, 'DESTi')
        IDXW = A.tile([128, NB], I32, 'IDXW')
        IDXE = A.tile([2, NB], I32, 'IDXE')
        MK5 = A.mark()
        xt_p5 = [A.tile([128, D], F32, 'xt5') for _ in range(2)]
        xnf = [A.tile([128, D], F32, 'xnf') for _ in range(2)]
        xTf = [A.tile([128, 8, 128], F32, 'xTf') for _ in range(2)]
        h2a = [A.tile([128, D], F32, 'h2a') for _ in range(2)]
        h2b = [A.tile([128, D], BF16, 'h2b') for _ in range(2)]
        junk_p5 = A.tile([128, D], BF16, 'junk5')
        ss_p5 = [A.tile([128, 1], F32, 'ss5') for _ in range(2)]
        for tj in range(NT):
            b = tj % 2
            r0 = tj * 128
            isx = tj < NTX
            j = 0 if isx else 1
            S.add(dmaq(), lambda e, b=b, r0=r0: [e.dma_start(out=xt_p5[b], in_=xres[r0:r0 + 128, :])], writes=[('xt5', b)], ndma=1)
            S.add('act', lambda e, b=b: e.activation(out=junk_p5, in_=xt_p5[b], func=AF.Square, accum_out=ss_p5[b]),
                  reads=[('xt5', b)], writes=[('ss5', b), 'junk5'])
            S.add('dve', lambda e, b=b: e.tensor_scalar(out=ss_p5[b], in0=ss_p5[b], scalar1=1.0 / D, scalar2=EPS, op0=ALU.mult, op1=ALU.add),
                  reads=[('ss5', b)], writes=[('ss5', b)])
            S.add('act', lambda e, b=b: e.sqrt(out=ss_p5[b], in_=ss_p5[b]), reads=[('ss5', b)], writes=[('ss5', b)])
            S.add('dve', lambda e, b=b: e.reciprocal(out=ss_p5[b], in_=ss_p5[b]), reads=[('ss5', b)], writes=[('ss5', b)])
            S.add('act', lambda e, b=b: e.activation(out=xnf[b], in_=xt_p5[b], func=AF.Copy, scale=ss_p5[b][:, 0:1]),
                  reads=[('xt5', b), ('ss5', b)], writes=[('xnf', b)])
            sb_ = BT['s2x'] if isx else BT['s2c']
            hb_ = BT['h2x'] if isx else BT['h2c']
            S.add('dve', lambda e, b=b, sb_=sb_: e.tensor_tensor(out=h2a[b], in0=xnf[b], in1=sb_, op=ALU.mult),
                  reads=[('xnf', b), ('btile', id(sb_))], writes=[('h2a', b)])
            S.add('pool', lambda e, b=b, hb_=hb_: e.tensor_tensor(out=h2b[b], in0=h2a[b], in1=hb_, op=ALU.add),
                  reads=[('h2a', b), ('btile', id(hb_))], writes=[('h2b', b)])
            S.add(dmaq(), lambda e, b=b, r0=r0: [e.dma_start(out=h2_tm[r0:r0 + 128, :], in_=h2b[b])], reads=[('h2b', b)],
                  writes=[('h2_tm', tj)], ndma=1)
            pz = pq[b]
            PZK = ('pq', b)
            for kc in range(8):
                S.add('pe', lambda e, pz=pz, b=b, kc=kc: e.transpose(out=pz[:, kc * 128:(kc + 1) * 128], in_=xnf[b][:, kc * 128:(kc + 1) * 128],
                                                                    identity=C['ident_f']), reads=[('xnf', b), 'ident_f'], writes=[PZK])
            S.add('act', lambda e, pz=pz, b=b: e.copy(out=xTf[b].rearrange("p k t -> p (k t)")[:, 0:512], in_=pz[:, 0:512]), reads=[PZK], writes=[('xTf', b, 0)])
            S.add('dve', lambda e, pz=pz, b=b: e.tensor_copy(out=xTf[b].rearrange("p k t -> p (k t)")[:, 512:1024], in_=pz[:, 512:1024]), reads=[PZK], writes=[('xTf', b, 1)])
            lb = bank(4)
            for kc in range(8):
                S.add('pe', lambda e, lb=lb, b=b, kc=kc, j=j: e.matmul(lb[:, 0:NE], lhsT=xTf[b][:, kc, :], rhs=rwx[:, kc, j, :], start=(kc == 0), stop=False),
                      reads=[('xTf', b, 0), ('xTf', b, 1), ('rwx', j)], writes=['bk4'])
            S.add('pe', lambda e, lb=lb, j=j: e.matmul(lb[:, 0:NE], lhsT=ones_f[0:1, :], rhs=rcst[:, j, :], start=False, stop=True),
                  reads=['ones_f', ('rcst', j)], writes=['bk4'])
            S.add('dve', lambda e, lb=lb, tj=tj: e.tensor_copy(out=LG[:, tj, :], in_=lb[:, 0:NE]), reads=['bk4'], writes=[('LG', tj)])
            S.add('dve', lambda e, tj=tj: e.max(out=V8[:, tj, :], in_=LG[:, tj, :]), reads=[('LG', tj)], writes=[('V8', tj)])
            S.add('dve', lambda e, tj=tj: e.tensor_scalar(out=MK[:, tj, :], in0=LG[:, tj, :], scalar1=V8[:, tj, 3:4], scalar2=None, op0=ALU.is_ge),
                  reads=[('LG', tj), ('V8', tj)], writes=[('MK', tj)])
        S.barrier()
        A.reset(MK5)
        NC_ = NT * NE
        POS = A.tile([128, NT, NE], F32, 'POS')
        CNT = A.tile([128, NT, NE], F32, 'CNT')
        TB = A.tile([128, NT, NE], F32, 'TB')
        EQ = A.tile([128, NT, NE], F32, 'EQ')
        TOT = A.tile([128, NE], F32, 'TOT')
        PAD = A.tile([128, NE], F32, 'PAD')
        PADi = A.tile([128, NE], I32, 'PADi')
        PS_ = A.tile([128, NE], F32, 'PS')
        PEND = A.tile([128, NE], F32, 'PEND')
        pendc = A.tile([32, 1], F32, 'pendc')
        cmpt = A.tile([32, NB], BF16, 'cmpt')
        EB = A.tile([128, NB], F32, 'EB')
        EB2 = A.tile([128, NB], F32, 'EB2')
        DESTf = A.tile([128, NT, 4], F32, 'DESTf')
        gs = A.tile([128, NT], F32, 'gs')
        MKf = MK.rearrange("p t e -> p (t e)")
        for c0 in range(0, NC_, 512):
            w = min(512, NC_ - c0)
            S.add('pe', lambda e, c0=c0, w=w: e.matmul(bank(0)[:, 0:w], lhsT=C['ustrict'], rhs=MKf[:, c0:c0 + w], start=True, stop=True),
                  reads=['MKall', 'ustrict'], writes=['bk0'])
            S.add('pe', lambda e, c0=c0, w=w: e.matmul(bank(1)[:, 0:w], lhsT=ones_bf, rhs=MKf[:, c0:c0 + w], start=True, stop=True),
                  reads=['MKall', 'ones_bf'], writes=['bk1'])
            S.add('act', lambda e, c0=c0, w=w: e.copy(out=POS.rearrange("p t e -> p (t e)")[:, c0:c0 + w], in_=bank(0)[:, 0:w]), reads=['bk0'], writes=['POS'])
            S.add('dve', lambda e, c0=c0, w=w: e.tensor_copy(out=CNT.rearrange("p t e -> p (t e)")[:, c0:c0 + w], in_=bank(1)[:, 0:w]), reads=['bk1'], writes=['CNT'])
        S.add('pool', lambda e: e.memset(TB[:, 0, :], 0.0), writes=['TB'])
        for tj in range(1, NT):
            S.add('dve', lambda e, tj=tj: e.tensor_tensor(out=TB[:, tj, :], in0=TB[:, tj - 1, :], in1=CNT[:, tj - 1, :], op=ALU.add),
                  reads=['TB', 'CNT'], writes=['TB'])
        S.add('dve', lambda e: e.tensor_tensor(out=TOT, in0=TB[:, NT - 1, :], in1=CNT[:, NT - 1, :], op=ALU.add), reads=['TB', 'CNT'], writes=['TOT'])
        S.add('dve', lambda e: e.tensor_scalar(out=PADi, in0=TOT, scalar1=127.0, scalar2=None, op0=ALU.add), reads=['TOT'], writes=['PADi'])
        S.add('dve', lambda e: e.tensor_scalar(out=PADi, in0=PADi, scalar1=7, scalar2=7, op0=ALU.arith_shift_right, op1=ALU.logical_shift_left),
              reads=['PADi'], writes=['PADi'])
        S.add('dve', lambda e: e.tensor_copy(out=PAD, in_=PADi), reads=['PADi'], writes=['PAD'])
        S.add('pool', lambda e: e.memset(PS_[:, 0:1], 0.0), writes=['PS'])
        for ex in range(1, NE):
            S.add('dve', lambda e, ex=ex: e.tensor_tensor(out=PS_[:, ex:ex + 1], in0=PS_[:, ex - 1:ex], in1=PAD[:, ex - 1:ex], op=ALU.add),
                  reads=['PS', 'PAD'], writes=['PS'])
        S.add('dve', lambda e: e.tensor_tensor(out=PEND, in0=PS_, in1=PAD, op=ALU.add), reads=['PS', 'PAD'], writes=['PEND'])
        S.add('dve', lambda e: e.tensor_tensor(out=POS, in0=POS, in1=TB, op=ALU.add), reads=['POS', 'TB'], writes=['POS'])
        S.add('dve', lambda e: e.tensor_tensor(out=POS, in0=POS, in1=PS_.unsqueeze(1).to_broadcast([128, NT, NE]), op=ALU.add),
              reads=['POS', 'PS'], writes=['POS'])
        for k in range(4):
            S.add('dve', lambda e, k=k: e.tensor_tensor(out=EQ, in0=LG, in1=V8[:, :, k:k + 1].to_broadcast([128, NT, NE]), op=ALU.is_equal),
                  reads=['LGall', 'V8all'], writes=['EQ'])
            S.add('dve', lambda e: e.tensor_tensor(out=EQ, in0=EQ, in1=POS, op=ALU.mult), reads=['EQ', 'POS'], writes=['EQ'])
            S.add('dve', lambda e, k=k: e.tensor_reduce(out=DESTf[:, :, k], in_=EQ, axis=AX.X, op=ALU.add), reads=['EQ'], writes=['DESTf'])
        S.add('dve', lambda e: e.tensor_copy(out=DESTi, in_=DESTf), reads=['DESTf'], writes=['DESTi'])
        S.add('dve', lambda e: e.tensor_tensor(out=G4, in0=V8[:, :, 0:4], in1=V8[:, :, 0:1].to_broadcast([128, NT, 4]), op=ALU.subtract),
              reads=['V8all'], writes=['G4'])
        S.add('act', lambda e: e.activation(out=G4, in_=G4, func=AF.Exp), reads=['G4'], writes=['G4'])
        S.add('dve', lambda e: e.tensor_reduce(out=gs, in_=G4, axis=AX.X, op=ALU.add), reads=['G4'], writes=['gs'])
        S.add('dve', lambda e: e.reciprocal(out=gs, in_=gs), reads=['gs'], writes=['gs'])
        S.add('dve', lambda e: e.tensor_tensor(out=G4, in0=G4, in1=gs.unsqueeze(2).to_broadcast([128, NT, 4]), op=ALU.mult),
              reads=['G4', 'gs'], writes=['G4'])
        S.add('pe', lambda e: e.transpose(out=bank(2)[0:32, 0:128], in_=PEND, identity=C['ident_f']), reads=['PEND', 'ident_f'], writes=['bk2'])
        S.add('dve', lambda e: e.tensor_copy(out=pendc, in_=bank(2)[0:32, 0:1]), reads=['bk2'], writes=['pendc'])
        S.add('dve', lambda e: e.tensor_scalar(out=cmpt, in0=C['blk128'], scalar1=pendc[:, 0:1], scalar2=None, op0=ALU.is_ge),
              reads=['pendc', 'blk128'], writes=['cmpt'])
        S.add('pe', lambda e: e.matmul(bank(3)[:, 0:NB], lhsT=ones_bf[0:32, :], rhs=cmpt, start=True, stop=True), reads=['cmpt', 'ones_bf'], writes=['bk3'])
        S.add('dve', lambda e: e.tensor_scalar(out=EB, in0=bank(3)[:, 0:NB], scalar1=float(NE - 1), scalar2=None, op0=ALU.min), reads=['bk3'], writes=['EB'])
        S.add('dve', lambda e: e.tensor_scalar(out=IDXE, in0=EB[0:2, :], scalar1=float(l * NE), scalar2=None, op0=ALU.add), reads=['EB'], writes=['IDXE'])
        S.add('dve', lambda e: e.tensor_scalar(out=EB2, in0=EB, scalar1=128.0, scalar2=C['pcol'][:, 0:1], op0=ALU.mult, op1=ALU.add),
              reads=['EB', 'pcol'], writes=['EB2'])
        S.add('dve', lambda e: e.tensor_tensor(out=EQ.rearrange("p t e -> p (t e)")[:, 0:NB - 2], in0=EB[:, 2:NB], in1=EB[:, 0:NB - 2], op=ALU.is_equal),
              reads=['EB', 'EQ'], writes=['EQ'])
        S.add('dve', lambda e: e.scalar_tensor_tensor(out=EB2[:, 2:NB], in0=EQ.rearrange("p t e -> p (t e)")[:, 0:NB - 2], scalar=float(OOB),
                                                      in1=EB2[:, 2:NB], op0=ALU.mult, op1=ALU.add), reads=['EQ', 'EB2'], writes=['EB2'])
        S.add('dve', lambda e: e.tensor_scalar(out=IDXW, in0=EB2, scalar1=float(l * NE * 128), scalar2=None, op0=ALU.add), reads=['EB2'], writes=['IDXW'])
        if 'moe_dbg' in dbg:
            S.barrier()
            d_lg = nc.dram_tensor('d_lg', [128, NT, NE], F32, kind="ExternalOutput").ap()
            d_v8 = nc.dram_tensor('d_v8', [128, NT, 8], F32, kind="ExternalOutput").ap()
            d_dest = nc.dram_tensor('d_dest', [128, NT, 4], I32, kind="ExternalOutput").ap()
            d_g4 = nc.dram_tensor('d_g4', [128, NT, 4], F32, kind="ExternalOutput").ap()
            d_eb = nc.dram_tensor('d_eb', [128, NB], F32, kind="ExternalOutput").ap()
            d_idxw = nc.dram_tensor('d_idxw', [128, NB], I32, kind="ExternalOutput").ap()
            d_pend = nc.dram_tensor('d_pend', [128, NE], F32, kind="ExternalOutput").ap()
            for dd, tt_ in ((d_lg, LG), (d_v8, V8), (d_dest, DESTi), (d_g4, G4), (d_eb, EB), (d_idxw, IDXW), (d_pend, PEND)):
                S.add('sp', lambda e, dd=dd, tt_=tt_: [e.dma_start(out=dd, in_=tt_)], writes=[('dbgout', id(dd))], ndma=1)
        S.barrier()
        A.reset(MK5)
        hs = [A.tile([128, D], BF16, 'hs') for _ in range(3)]
        for tj in range(NT):
            b = tj % 3
            r0 = tj * 128
            S.add('sp', lambda e, b=b, r0=r0: [e.dma_start(out=hs[b], in_=h2_tm[r0:r0 + 128, :])], writes=[('hs', b)], ndma=1)
            for k in range(4):
                S.add('pool', lambda e, b=b, tj=tj, k=k: [e.indirect_dma_start(
                    out=Xs, out_offset=bass.IndirectOffsetOnAxis(ap=DESTi[:, tj, k:k + 1], axis=0), in_=hs[b], in_offset=None)],
                    reads=[('hs', b)], writes=[('Xs', tj, k)], ndma=1)
        S.barrier()
        A.reset(MK5)
        WG = [A.tile([128, 8, 2048], BF16, 'WG') for _ in range(2)]
        WD = [A.tile([128, 8, 1024], BF16, 'WD') for _ in range(2)]
        BG = [A.tile([2, 2048], BF16, 'BG') for _ in range(2)]
        BD = [A.tile([2, 1024], BF16, 'BD') for _ in range(2)]
        xb_ = [A.tile([128, D], BF16, 'xb') for _ in range(2)]
        xT_ = [A.tile([128, 8, 128], BF16, 'xT') for _ in range(2)]
        am = A.tile([128, 512], F32, 'am')
        sg = A.tile([128, 512], F32, 'sg')
        uc = A.tile([128, 512], F32, 'uc')
        yb = A.tile([128, D], BF16, 'yb')
        yT_ = A.tile([128, 8, 128], BF16, 'yT8')
        ob_ = [A.tile([128, D], F32, 'ob') for _ in range(2)]
        wgu_rows = wgu.rearrange("l r c -> (l r) c")
        wdn_rows = wdn.rearrange("l r c -> (l r) c")
        bgu_rows = bgu.rearrange("l r c -> (l r) c")
        bdn_rows = bdn.rearrange("l r c -> (l r) c")

        def bcreg(e):
            if 'r' not in BCREG:
                BCREG['r'] = e.alloc_register('bcreg')
                e.reg_mov(BCREG['r'], DEPTH * NE * 128 - 1)
            return BCREG['r']
        for bi in range(NB):
            b = bi % 2
            S.add('pool', lambda e, b=b, bi=bi: [e.indirect_dma_start(
                out=WG[b].rearrange("p k n -> p (k n)"), out_offset=None, in_=wgu_rows,
                in_offset=bass.IndirectOffsetOnAxis(ap=IDXW[:, bi:bi + 1], axis=0), bounds_check=bcreg(e), oob_is_err=False)],
                writes=[('WG', b)], ndma=1)
            S.add('pool', lambda e, b=b, bi=bi: [e.indirect_dma_start(
                out=WD[b].rearrange("p k n -> p (k n)"), out_offset=None, in_=wdn_rows,
                in_offset=bass.IndirectOffsetOnAxis(ap=IDXW[:, bi:bi + 1], axis=0), bounds_check=bcreg(e), oob_is_err=False)],
                writes=[('WD', b)], ndma=1)
            S.add('pool', lambda e, b=b, bi=bi: [e.indirect_dma_start(
                out=BG[b], out_offset=None, in_=bgu_rows, in_offset=bass.IndirectOffsetOnAxis(ap=IDXE[0:2, bi:bi + 1], axis=0))],
                writes=[('BG', b)], ndma=1)
            S.add('pool', lambda e, b=b, bi=bi: [e.indirect_dma_start(
                out=BD[b], out_offset=None, in_=bdn_rows, in_offset=bass.IndirectOffsetOnAxis(ap=IDXE[0:2, bi:bi + 1], axis=0))],
                writes=[('BD', b)], ndma=1)
            S.add('sp', lambda e, b=b, bi=bi: [e.dma_start(out=xb_[b], in_=Xs[bi * 128:(bi + 1) * 128, :])], writes=[('xb', b)], ndma=1)
            pb = pbf[0]
            for kc in range(8):
                S.add('pe', lambda e, b=b, kc=kc, pb=pb: e.transpose(out=pb[:, kc * 128:(kc + 1) * 128], in_=xb_[b][:, kc * 128:(kc + 1) * 128],
                                                                    identity=C['ident_bf']), reads=[('xb', b), 'ident_bf'], writes=[('pbf', 0)])
            S.add('act', lambda e, b=b, pb=pb: e.copy(out=xT_[b].rearrange("p k t -> p (k t)"), in_=pb), reads=[('pbf', 0)], writes=[('xT', b)])
            for n in range(4):
                bk = bank(n)
                BKK = ('bk', n)
                for kc in range(8):
                    S.add('pe', lambda e, b=b, kc=kc, n=n, bk=bk: e.matmul(bk, lhsT=xT_[b][:, kc, :], rhs=WG[b][:, kc, n * 512:(n + 1) * 512],
                                                                          start=(kc == 0), stop=False), reads=[('xT', b), ('WG', b)], writes=[BKK])
                S.add('pe', lambda e, b=b, n=n, bk=bk: e.matmul(bk, lhsT=ones_bf[0:1, :], rhs=BG[b][0:1, n * 512:(n + 1) * 512], start=False, stop=True),
                      reads=['ones_bf', ('BG', b)], writes=[BKK])
            for hf in range(2):
                ab = bank(hf)
                ub = bank(2 + hf)
                S.add('act', lambda e, ab=ab: e.copy(out=am, in_=ab), reads=[('bk', hf)], writes=['am'])
                S.add('act', lambda e, ub=ub: e.activation(out=uc, in_=ub, func=AF.Identity, bias=onec[:, 0:1]), reads=[('bk', 2 + hf), 'onec'], writes=['uc'])
                S.add('dve', lambda e: e.tensor_scalar(out=am, in0=am, scalar1=7.0, scalar2=None, op0=ALU.min), reads=['am'], writes=['am'])
                S.add('act', lambda e: e.activation(out=sg, in_=am, func=AF.Sigmoid, scale=1.702), reads=['am'], writes=['sg'])
                S.add('dve', lambda e: e.tensor_scalar(out=uc, in0=uc, scalar1=-6.0, scalar2=8.0, op0=ALU.max, op1=ALU.min),
                      reads=['uc'], writes=['uc'])
                S.add('pool', lambda e: e.tensor_tensor(out=am, in0=am, in1=sg, op=ALU.mult), reads=['am', 'sg'], writes=['am'])
                S.add('pool', lambda e, hf=hf: e.tensor_tensor(out=yb[:, hf * 512:(hf + 1) * 512], in0=uc, in1=am, op=ALU.mult),
                      reads=['uc', 'am'], writes=[('yb', hf)])
            pb = pbf[1]
            for kc in range(8):
                S.add('pe', lambda e, kc=kc, pb=pb: e.transpose(out=pb[:, kc * 128:(kc + 1) * 128], in_=yb[:, kc * 128:(kc + 1) * 128], identity=C['ident_bf']),
                      reads=[('yb', kc // 4), 'ident_bf'], writes=[('pbf', 1)])
            S.add('dve', lambda e, pb=pb: e.tensor_copy(out=yT_.rearrange("p k t -> p (k t)"), in_=pb), reads=[('pbf', 1)], writes=['yT8'])
            for n in range(2):
                bk = bank(4 + n)
                BKK = ('bk', 4 + n)
                for kc in range(8):
                    S.add('pe', lambda e, b=b, kc=kc, n=n, bk=bk: e.matmul(bk, lhsT=yT_[:, kc, :], rhs=WD[b][:, kc, n * 512:(n + 1) * 512],
                                                                          start=(kc == 0), stop=False), reads=['yT8', ('WD', b)], writes=[BKK])
                S.add('pe', lambda e, b=b, n=n, bk=bk: e.matmul(bk, lhsT=ones_bf[0:1, :], rhs=BD[b][0:1, n * 512:(n + 1) * 512], start=False, stop=True),
                      reads=['ones_bf', ('BD', b)], writes=[BKK])
                if n == 0:
                    S.add('act', lambda e, b=b, bk=bk: e.copy(out=ob_[b][:, 0:512], in_=bk), reads=[BKK], writes=[('ob', b, 0)])
                else:
                    S.add('dve', lambda e, b=b, bk=bk: e.tensor_copy(out=ob_[b][:, 512:1024], in_=bk), reads=[BKK], writes=[('ob', b, 1)])
            S.add('act', lambda e, b=b, bi=bi: [e.dma_start(out=Ys[bi * 128:(bi + 1) * 128, :], in_=ob_[b])],
                  reads=[('ob', b, 0), ('ob', b, 1)], writes=[('Ys', bi)], ndma=1)
        S.barrier()
        A.reset(MK5)
        Yk = [[A.tile([128, D], F32, 'Yk') for _ in range(4)] for _ in range(2)]
        xt_p9 = [A.tile([128, D], F32, 'xt9') for _ in range(2)]
        acc = [A.tile([128, D], F32, 'acc') for _ in range(2)]
        for tj in range(NT):
            b = tj % 2
            r0 = tj * 128
            isx = tj < NTX
            for k in range(4):
                S.add('pool', lambda e, b=b, tj=tj, k=k: [e.indirect_dma_start(
                    out=Yk[b][k], out_offset=None, in_=Ys, in_offset=bass.IndirectOffsetOnAxis(ap=DESTi[:, tj, k:k + 1], axis=0))],
                    writes=[('Yk', b, k)], ndma=1)
            S.add('sp', lambda e, b=b, r0=r0: [e.dma_start(out=xt_p9[b], in_=xres[r0:r0 + 128, :])], writes=[('xt9', b)], ndma=1)
            S.add('dve', lambda e, b=b, tj=tj: e.tensor_scalar(out=acc[b], in0=Yk[b][0], scalar1=G4[:, tj, 0:1], scalar2=None, op0=ALU.mult),
                  reads=[('Yk', b, 0)], writes=[('acc', b)])
            for k in range(1, 4):
                S.add('dve', lambda e, b=b, tj=tj, k=k: e.scalar_tensor_tensor(out=acc[b], in0=Yk[b][k], scalar=G4[:, tj, k:k + 1], in1=acc[b],
                                                                              op0=ALU.mult, op1=ALU.add), reads=[('Yk', b, k), ('acc', b)], writes=[('acc', b)])
            gb = BT['g2x'] if isx else BT['g2c']
            S.add('pool', lambda e, b=b, gb=gb: e.tensor_tensor(out=acc[b], in0=acc[b], in1=gb, op=ALU.mult), reads=[('acc', b), ('btile', id(gb))], writes=[('acc', b)])
            S.add('pool', lambda e, b=b: e.tensor_tensor(out=acc[b], in0=acc[b], in1=xt_p9[b], op=ALU.add), reads=[('acc', b), ('xt9', b)], writes=[('acc', b)])
            S.add('act', lambda e, b=b, r0=r0: [e.dma_start(out=xres[r0:r0 + 128, :], in_=acc[b])], reads=[('acc', b)], writes=[('xres', r0)], ndma=1)
        S.barrier()

    for _l in range(nl):
        _layer(_l)

    S.barrier()
    A.reset(PERSIST)
    fcol = A.tile([128, 8], F32, 'fcol')
    fb_pf = A.tile([128, D], F32, 'fb')
    S.add('sp', lambda e: [e.dma_start(out=fcol, in_=fng)], writes=['fcol'], ndma=1)
    if final_norm:
        bcast_tile(fb_pf, fcol, None, 'fcol')
    xt_pf = [A.tile([128, D], F32, 'xtf') for _ in range(2)]
    xo_pf = [A.tile([128, D], F32, 'xof') for _ in range(2)]
    junk_pf = A.tile([128, D], BF16, 'junkf')
    ss_pf = [A.tile([128, 1], F32, 'ssf') for _ in range(2)]
    for tj in range(NTX):
        b = tj % 2
        r0 = tj * 128
        S.add(dmaq(), lambda e, b=b, r0=r0: [e.dma_start(out=xt_pf[b], in_=xres[r0:r0 + 128, :])], writes=[('xtf', b)], ndma=1)
        if final_norm:
            S.add('act', lambda e, b=b: e.activation(out=junk_pf, in_=xt_pf[b], func=AF.Square, accum_out=ss_pf[b]), reads=[('xtf', b)], writes=[('ssf', b), 'junkf'])
            S.add('dve', lambda e, b=b: e.tensor_scalar(out=ss_pf[b], in0=ss_pf[b], scalar1=1.0 / D, scalar2=EPS, op0=ALU.mult, op1=ALU.add),
                  reads=[('ssf', b)], writes=[('ssf', b)])
            S.add('act', lambda e, b=b: e.sqrt(out=ss_pf[b], in_=ss_pf[b]), reads=[('ssf', b)], writes=[('ssf', b)])
            S.add('dve', lambda e, b=b: e.reciprocal(out=ss_pf[b], in_=ss_pf[b]), reads=[('ssf', b)], writes=[('ssf', b)])
            S.add('dve', lambda e, b=b: e.scalar_tensor_tensor(out=xo_pf[b], in0=xt_pf[b], scalar=ss_pf[b][:, 0:1], in1=fb_pf, op0=ALU.mult, op1=ALU.mult),
                  reads=[('xtf', b), ('ssf', b), ('btile', id(fb_pf))], writes=[('xof', b)])
        else:
            S.add('dve', lambda e, b=b: e.tensor_copy(out=xo_pf[b], in_=xt_pf[b]), reads=[('xtf', b)], writes=[('xof', b)])
        S.add(dmaq(), lambda e, b=b, r0=r0: [e.dma_start(out=yout[r0:r0 + 128, :], in_=xo_pf[b])], reads=[('xof', b)], writes=[('yout', tj)], ndma=1)
    S.barrier()
    S.emit()
    return nc, consts


def prep_shared(inp):
    f32 = lambda a: np.ascontiguousarray(np.asarray(a, dtype=np.float32))
    sh = {}
    sh['ada_w'] = f32(inp['ada_w'])
    sh['ada_bT'] = f32(np.asarray(inp['ada_b']).reshape(DEPTH, 48, 128).transpose(0, 2, 1))
    sh['n1g'] = f32(np.asarray(inp['norm1_g']).reshape(DEPTH, 8, 128).transpose(0, 2, 1))
    sh['n2g'] = f32(np.asarray(inp['norm2_g']).reshape(DEPTH, 8, 128).transpose(0, 2, 1))
    sh['fng'] = f32(np.asarray(inp['final_norm_g']).reshape(8, 128).T)
    sh['w_in'] = f32(inp['w_in'])
    sh['sink'] = f32(inp['attn_sink'])
    sh['w_fo'] = f32(inp['w_fourier_out'])
    sh['w_ao'] = f32(inp['w_attn_out'])
    sh['w_out'] = f32(inp['w_out'])
    sh['r_w'] = f32(np.asarray(inp['router_w']).reshape(DEPTH, 8, 128, NE).transpose(0, 2, 1, 3))
    sh['r_b'] = f32(np.asarray(inp['router_b']).reshape(DEPTH, 1, NE))
    sh['wgu'] = f32(np.asarray(inp['expert_w_gu']).reshape(DEPTH, NE, 8, 128, 2048).transpose(0, 1, 3, 2, 4).reshape(DEPTH, NE * 128, 8 * 2048))
    sh['wdn'] = f32(np.asarray(inp['expert_w_down']).reshape(DEPTH, NE, 8, 128, 1024).transpose(0, 1, 3, 2, 4).reshape(DEPTH, NE * 128, 8 * 1024))
    sh['bgu'] = f32(inp['expert_b_gu'])
    sh['bdn'] = f32(inp['expert_b_down'])
    return sh


def core_inputs(inp, sh, consts, b):
    m = dict(sh)
    m['xin'] = np.ascontiguousarray(np.asarray(inp['x'][b], dtype=np.float32))
    m['ctxin'] = np.ascontiguousarray(np.asarray(inp['ctx'][b], dtype=np.float32))
    cc = np.zeros((128, 8, 2), np.float32)
    cc[:, :, 0] = np.asarray(inp['c'][b]).reshape(8, 128).T
    cc[:, :, 1] = np.asarray(inp['c_ctx']).reshape(8, 128).T
    m['ccol'] = cc
    for k, v in consts.items():
        m['c_' + k] = v
    return m


def kernel(**inputs):
    nc, consts = build()
    sh = prep_shared(inputs)
    nb = inputs['x'].shape[0]
    in_maps = [core_inputs(inputs, sh, consts, i) for i in range(nb)]
    res = run_bass_kernel_spmd(nc, in_maps, core_ids=list(range(nb)))
    out = np.stack([np.asarray(res.results[i]['yout'], dtype=np.float32) for i in range(nb)], axis=0)
    return out
```

```python
import numpy as np
import ml_dtypes
import concourse.bass as bass
import concourse.mybir as mybir
from concourse.bass_utils import run_bass_kernel_spmd

F32 = mybir.dt.float32
BF16 = mybir.dt.bfloat16
I32 = mybir.dt.int32
ALU = mybir.AluOpType
AF = mybir.ActivationFunctionType
AX = mybir.AxisListType

D = 1024
TX = 8192
TC = 256
T = TX + TC
NT = T // 128
NTX = TX // 128
DEPTH = 4
NE = 32
NB = (T * 4 + NE * 127) // 128 + 1
NSLOT = NB * 128
EPS = 1e-5
OOB = 1 << 28


class Sched:
    ENGS = ['pe', 'act', 'dve', 'pool', 'sp']
    EPOCH = 12000
    NPOOL = 6

    def __init__(self, nc):
        self.nc = nc
        self.ops = []
        self.last_w = {}
        self.readers = {}
        self.sig = []
        self.nsem = 0
        self.esem = {e: self._new_sem('e_' + e) for e in self.ENGS}
        self.ecnt = {e: 0 for e in self.ENGS}
        self.dpool = {e: [[self._new_sem('d_%s%d' % (e, i)), 0] for i in range(self.NPOOL)]
                      for e in self.ENGS}
        self.dnext = {e: 0 for e in self.ENGS}
        self.known = {e: {} for e in self.ENGS}

    def _new_sem(self, name):
        self.nsem += 1
        return self.nc.alloc_semaphore(name='%s_%d' % (name, self.nsem))

    def _prune(self, eng, waits):
        kn = self.known[eng]
        wl = []
        for key, (s, v) in waits.items():
            if kn.get(key, 0) >= v:
                continue
            kn[key] = v
            wl.append((s, v))
        return wl

    def add(self, eng, fn, reads=(), writes=(), ndma=0):
        opid = len(self.ops)
        deps = set()
        for k in list(reads) + list(writes):
            if k in self.last_w:
                deps.add(self.last_w[k])
        for k in writes:
            for r in self.readers.get(k, ()):
                deps.add(r)
        waits = {}
        for d in deps:
            deng = self.ops[d]['eng']
            if deng == 'pe' and eng == 'pe' and not self.ops[d]['ndma'] and not ndma:
                continue
            s, v = self.sig[d]
            key = id(s)
            if key not in waits or waits[key][1] < v:
                waits[key] = (s, v)
        if ndma:
            pool = self.dpool[eng]
            slot = pool[self.dnext[eng] % self.NPOOL]
            self.dnext[eng] += 1
            if slot[1] > 0:
                key = id(slot[0])
                if key not in waits or waits[key][1] < slot[1]:
                    waits[key] = (slot[0], slot[1])
            if slot[1] + 16 * ndma > self.EPOCH * 2:
                slot[0] = self._new_sem('d_' + eng)
                slot[1] = 0
            slot[1] += 16 * ndma
            sig = (slot[0], slot[1])
            inc = (slot[0], 16)
        else:
            if self.ecnt[eng] >= self.EPOCH:
                self.esem[eng] = self._new_sem('e_' + eng)
                self.ecnt[eng] = 0
            self.ecnt[eng] += 1
            sig = (self.esem[eng], self.ecnt[eng])
            inc = (self.esem[eng], 1)
        self.ops.append(dict(eng=eng, fn=fn, waits=self._prune(eng, waits), inc=inc, ndma=ndma))
        self.sig.append(sig)
        for k in reads:
            self.readers.setdefault(k, []).append(opid)
        for k in writes:
            self.last_w[k] = opid
            self.readers[k] = []
        return opid

    def barrier(self):
        sigs = {}
        for e in self.ENGS:
            if self.ecnt[e] > 0:
                sigs[id(self.esem[e])] = (self.esem[e], self.ecnt[e])
            for slot in self.dpool[e]:
                if slot[1] > 0:
                    sigs[id(slot[0])] = (slot[0], slot[1])
        for e in self.ENGS:
            self.ops.append(dict(eng=e, fn=None, waits=self._prune(e, dict(sigs)), inc=None, ndma=0))
            self.sig.append(None)
        self.last_w = {}
        self.readers = {}

    def emit(self):
        nc = self.nc
        with nc.Block() as block:
            def run(engname):
                def body(e):
                    for op in self.ops:
                        if op['eng'] != engname:
                            continue
                        for s, v in op['waits']:
                            e.wait_ge(s, v)
                        if op['fn'] is None:
                            continue
                        try:
                            r = op['fn'](e)
                        except Exception:
                            print('EMIT FAIL', engname, 'op#', self.ops.index(op), 'engine-op-count', self.ecnt, flush=True)
                            raise
                        if not isinstance(r, (list, tuple)):
                            r = [r]
                        if op['ndma']:
                            assert len(r) == op['ndma'], (len(r), op['ndma'])
                        else:
                            assert len(r) == 1
                        for ins in r:
                            ins.then_inc(op['inc'][0], op['inc'][1])
                return body
            block.tensor(run('pe'))
            block.scalar(run('act'))
            block.vector(run('dve'))
            block.gpsimd(run('pool'))
            block.sync(run('sp'))


_DTS = {F32: 4, BF16: 2, I32: 4}


class Arena:
    def __init__(self, nc, limit=229376):
        self.nc = nc
        self.off = 20480
        self.n = 0
        self.limit = limit

    def tile(self, shape, dtype, name='t'):
        nb = _DTS[dtype]
        for s in shape[1:]:
            nb *= s
        nb = (nb + 63) // 64 * 64
        self.n += 1
        h = self.nc.alloc_sbuf_tensor_at('%s_%d' % (name, self.n), list(shape), dtype, offset=self.off)
        self.off += nb
        assert self.off <= self.limit, ('SBUF overflow', name, self.off)
        return h.ap()

    def mark(self):
        return self.off

    def reset(self, m):
        self.off = m


def host_consts():
    c = {}
    c['ident_bf'] = np.eye(128, dtype=np.float32).astype(ml_dtypes.bfloat16)
    c['ident_f'] = np.eye(128, dtype=np.float32)
    k = np.arange(128)[:, None]
    q = np.arange(128)[None, :]
    c['mask_prev'] = (k >= q).astype(np.float32).astype(ml_dtypes.bfloat16)
    c['mask_next'] = (k <= q).astype(np.float32).astype(ml_dtypes.bfloat16)
    c['ustrict'] = (k < q).astype(np.float32).astype(ml_dtypes.bfloat16)
    n = np.arange(TX)
    row = (n // 64).astype(np.float64)
    col = (n % 64).astype(np.float64)
    inv = 10000.0 ** (-np.arange(16, dtype=np.float64) / 16)
    cosT = np.zeros((64, TX), np.float64)
    sinT = np.zeros((64, TX), np.float64)
    for ax, pos in enumerate((row, col)):
        ang = (pos[None, :].astype(np.float32) * inv[:, None].astype(np.float32)).astype(np.float32)
        for pr in range(2):
            cosT[ax * 32 + pr * 16: ax * 32 + pr * 16 + 16] = np.cos(ang)
            sinT[ax * 32 + pr * 16: ax * 32 + pr * 16 + 16] = np.sin(ang)
    c['cosT'] = cosT.astype(np.float32)
    c['sinT'] = sinT.astype(np.float32)
    rm = np.zeros((128, 128), np.float32)
    for ax in range(2):
        for f in range(16):
            d0 = ax * 32 + f
            d1 = ax * 32 + 16 + f
            rm[d1, d0] = -1.0
            rm[d0, d1] = 1.0
    c['rotm'] = rm.astype(ml_dtypes.bfloat16)
    a = np.arange(128, dtype=np.float64)
    th = 2 * np.pi * np.outer(a, a) / 128
    c['c128'] = np.cos(th).astype(np.float32).astype(ml_dtypes.bfloat16)
    c['s128'] = np.sin(th).astype(np.float32).astype(ml_dtypes.bfloat16)
    c['cc'] = np.cos(th).astype(np.float32).astype(ml_dtypes.bfloat16)
    c['nsc'] = (-np.sin(th)).astype(np.float32).astype(ml_dtypes.bfloat16)
    n2 = np.arange(64, dtype=np.float64)
    tw = 2 * np.pi * np.outer(a, n2) / 8192
    c['twr'] = np.cos(tw).astype(np.float32)
    c['twi'] = np.sin(tw).astype(np.float32)
    c['ntwi'] = (-np.sin(tw)).astype(np.float32)
    th64 = 2 * np.pi * np.outer(n2, n2) / 64
    c64 = np.cos(th64) / 1024.0
    s64 = np.sin(th64) / 1024.0
    r2 = np.zeros((128, 128), np.float64)
    r2[0:64, 0:64] = c64
    r2[64:128, 0:64] = -s64
    r2[0:64, 64:128] = s64
    r2[64:128, 64:128] = c64
    c['r2'] = r2.astype(np.float32).astype(ml_dtypes.bfloat16)
    b = np.arange(256, dtype=np.float64)
    th256 = 2 * np.pi * np.outer(b, b) / 256
    sc = 1.0 / np.sqrt(256.0 * 128.0)
    c['c256'] = (np.cos(th256) * sc).astype(np.float32).astype(ml_dtypes.bfloat16).reshape(2, 128, 256).transpose(1, 0, 2).copy()
    c['s256'] = (np.sin(th256) * sc).astype(np.float32).astype(ml_dtypes.bfloat16).reshape(2, 128, 256).transpose(1, 0, 2).copy()
    c['blk128'] = np.broadcast_to((np.arange(NB, dtype=np.float32) * 128.0)[None, :], (32, NB)).copy()
    c['pcol'] = np.arange(128, dtype=np.float32).reshape(128, 1)
    c['u32'] = (np.arange(32)[:, None] < np.arange(32)[None, :]).astype(np.float32)
    return c


CONST_SPECS = None


def build(nl=DEPTH, final_norm=True, dbg=()):
    nc = bass.Bass("TRN2", target_bir_lowering=False)
    consts = host_consts()

    def din(name, shape, dt=F32):
        return nc.dram_tensor(name, list(shape), dt, kind="ExternalInput").ap()

    def dscr(name, shape, dt):
        kind = "ExternalOutput" if name in dbg else "Internal"
        return nc.dram_tensor(name, list(shape), dt, kind=kind).ap()

    xin = din('xin', [TX, D])
    ctxin = din('ctxin', [TC, D])
    ccol = din('ccol', [128, 8, 2])
    ada_w = din('ada_w', [DEPTH, D, 6 * D])
    ada_bT = din('ada_bT', [DEPTH, 128, 48])
    n1g = din('n1g', [DEPTH, 128, 8])
    n2g = din('n2g', [DEPTH, 128, 8])
    fng = din('fng', [128, 8])
    w_in = din('w_in', [DEPTH, D, 3328])
    sink = din('sink', [DEPTH, 8])
    w_fo = din('w_fo', [DEPTH, 512, D])
    w_ao = din('w_ao', [DEPTH, 512, D])
    w_out = din('w_out', [DEPTH, D, D])
    r_w = din('r_w', [DEPTH, 128, 8, NE])
    r_b = din('r_b', [DEPTH, 1, NE])
    wgu = din('wgu', [DEPTH, NE * 128, 8 * 2048])
    wdn = din('wdn', [DEPTH, NE * 128, 8 * 1024])
    bgu = din('bgu', [DEPTH, NE, 2048])
    bdn = din('bdn', [DEPTH, NE, 1024])
    cd = {}
    for k, v in consts.items():
        dt = BF16 if v.dtype == ml_dtypes.bfloat16 else F32
        cd[k] = din('c_' + k, v.shape, dt)
    yout = nc.dram_tensor('yout', [TX, D], F32, kind="ExternalOutput").ap()

    xres = dscr('xres', [T, D], F32)
    f_tm = dscr('f_tm', [T, 512], BF16)
    q_blk = dscr('q_blk', [NT, 64, 8, 128], BF16)
    kT_all = dscr('kT_all', [64, 2, T], BF16)
    v_blk = dscr('v_blk', [128, NT, 128], BF16)
    g_blk = dscr('g_blk', [NT, 128, 16, 128], BF16)
    t2_blk = dscr('t2_blk', [NT, 128, 8, 128], BF16)
    Zd = dscr('Zd', [2, 128, 64, 512], BF16)
    h2_tm = dscr('h2_tm', [T, D], BF16)
    Xs = dscr('Xs', [NSLOT, D], BF16)
    Ys = dscr('Ys', [NSLOT, D], F32)

    S = Sched(nc)
    A = Arena(nc)
    BCREG = {}
    pq = [nc.alloc_psum_tensor('pq%d' % i, [128, 1024], F32).ap() for i in range(3)]
    pbf = [nc.alloc_psum_tensor('pbf%d' % i, [128, 1024], BF16).ap() for i in range(2)]

    def bank(i):
        return pq[i // 2][:, (i % 2) * 512:(i % 2) * 512 + 512]

    C = {}
    qi = [0]

    def dmaq():
        qi[0] += 1
        return 'sp' if qi[0] % 2 else 'act'

    def load_const(name, shape, dt):
        t = A.tile(shape, dt, name)
        S.add('sp', lambda e: [e.dma_start(out=t, in_=cd[name])], writes=[name], ndma=1)
        C[name] = t
        return t

    for nm in ('ident_bf', 'mask_prev', 'mask_next', 'ustrict', 'c128', 's128', 'cc', 'nsc', 'r2'):
        load_const(nm, [128, 128], BF16)
    load_const('ident_f', [128, 128], F32)
    load_const('rotm', [128, 128], BF16)
    load_const('twr', [128, 64], F32)
    load_const('twi', [128, 64], F32)
    load_const('ntwi', [128, 64], F32)
    load_const('c256', [128, 2, 256], BF16)
    load_const('s256', [128, 2, 256], BF16)
    load_const('blk128', [32, NB], F32)
    load_const('pcol', [128, 1], F32)
    load_const('u32', [32, 32], F32)
    ones_bf = A.tile([128, 128], BF16, 'ones_bf')
    S.add('pool', lambda e: e.memset(ones_bf, 1.0), writes=['ones_bf'])
    ones_f = A.tile([128, 128], F32, 'ones_f')
    S.add('pool', lambda e: e.memset(ones_f, 1.0), writes=['ones_f'])
    onec = A.tile([128, 1], F32, 'onec')
    S.add('pool', lambda e: e.memset(onec, 1.0), writes=['onec'])
    epsc = A.tile([128, 1], F32, 'epsc')
    S.add('pool', lambda e: e.memset(epsc, EPS), writes=['epsc'])
    scc = A.tile([128, 8, 2], F32, 'scc')
    S.add('sp', lambda e: [e.dma_start(out=scc, in_=ccol)], writes=['scc'], ndma=1)
    S.add('act', lambda e: e.activation(out=scc, in_=scc, func=AF.Silu), reads=['scc'], writes=['scc'])
    modT = A.tile([128, 48, 2], F32, 'modT')
    s1 = A.tile([128, 8, 2], F32, 's1')
    s2 = A.tile([128, 8, 2], F32, 's2')
    gcol = A.tile([128, 8], F32, 'gcol')
    BT = {k: A.tile([128, D], F32, 'bt_' + k) for k in
          ('g1x', 'g1c', 'g2x', 'g2c', 's2x', 's2c', 'h2x', 'h2c')}
    rwp = A.tile([128, 8, NE], F32, 'rwp')
    rwx = A.tile([128, 8, 2, NE], F32, 'rwx')
    rcst = A.tile([1, 2, NE], F32, 'rcst')
    rbt = A.tile([1, NE], F32, 'rbt')
    PERSIST = A.mark()

    for ci in range(16):
        S.add(dmaq(), lambda e, ci=ci: [e.dma_start(out=xres[ci * 512:(ci + 1) * 512, :], in_=xin[ci * 512:(ci + 1) * 512, :])],
              writes=[('xres0', ci)], ndma=1)
    S.add('act', lambda e: [e.dma_start(out=xres[TX:T, :], in_=ctxin)], writes=['xres2'], ndma=1)
    _mz = A.mark()
    zt = A.tile([128, 8 * D], BF16, 'zt')
    A.reset(_mz)
    S.add('pool', lambda e: e.memset(zt, 0.0), writes=['zt'])
    Xz = Xs.rearrange("(p r) d -> p (r d)", p=128)
    for c0 in range(0, NB * D, 8 * D):
        w_ = min(8 * D, NB * D - c0)
        S.add(dmaq(), lambda e, c0=c0, w_=w_: [e.dma_start(out=Xz[:, c0:c0 + w_], in_=zt[:, 0:w_])], reads=['zt'], writes=[('Xz', c0)], ndma=1)
    S.barrier()

    def bcast_tile(dst, colsrc, j, key):
        for kc in range(8):
            src = colsrc[:, kc, j:j + 1] if j is not None else colsrc[:, kc:kc + 1]
            tmp = BCT[kc % 2]
            S.add('dve', lambda e, src=src, tmp=tmp: e.tensor_copy(out=tmp, in_=src.to_broadcast([128, 128])),
                  reads=[key], writes=[('bct', kc % 2)])
            pb = bank(kc % 2)
            S.add('pe', lambda e, tmp=tmp, pb=pb: e.matmul(pb[:, 0:128], lhsT=tmp, rhs=C['ident_f'], start=True, stop=True),
                  reads=[('bct', kc % 2)], writes=[('bk', kc % 2)])
            S.add('act', lambda e, pb=pb, kc=kc: e.copy(out=dst[:, kc * 128:(kc + 1) * 128], in_=pb[:, 0:128]),
                  reads=[('bk', kc % 2)], writes=[('btile', id(dst))])

    BCT = [A.tile([128, 128], F32, 'bct') for _ in range(2)]
    PERSIST = A.mark()

    def _layer(l):
        A.reset(PERSIST)
        aw = [A.tile([128, 8, 768], F32, 'aw') for _ in range(2)]
        adb = A.tile([128, 48], F32, 'adb')
        S.add('act', lambda e: [e.dma_start(out=adb, in_=ada_bT[l])], writes=['adb'], ndma=1)
        for cch in range(8):
            buf = aw[cch % 2]
            S.add(dmaq(), lambda e, buf=buf, cch=cch: [e.dma_start(
                out=buf, in_=ada_w[l, :, cch * 768:(cch + 1) * 768].rearrange("(kc p) n -> p kc n", p=128))],
                writes=[('aw', cch % 2)], ndma=1)
            for mi in range(6):
                mm = cch * 6 + mi
                for kc in range(8):
                    S.add('pe', lambda e, buf=buf, kc=kc, mm=mm, mi=mi: e.matmul(
                        bank(0)[:, mm * 2:mm * 2 + 2], lhsT=buf[:, kc, mi * 128:(mi + 1) * 128], rhs=scc[:, kc, :],
                        start=(kc == 0), stop=(kc == 7)), reads=[('aw', cch % 2), 'scc'], writes=['bk0'])
        S.add('dve', lambda e: e.tensor_tensor(out=modT, in0=bank(0)[:, 0:96].rearrange("p (m j) -> p m j", j=2),
                                               in1=adb.unsqueeze(2).to_broadcast([128, 48, 2]), op=ALU.add),
              reads=['bk0', 'adb'], writes=['modT'])
        for (sdst, gsrc, moff, nm) in ((s1, n1g, 8, 's1'), (s2, n2g, 32, 's2')):
            S.add('sp', lambda e, gsrc=gsrc: [e.dma_start(out=gcol, in_=gsrc[l])], writes=['gcol'], ndma=1)
            S.add('dve', lambda e, sdst=sdst, moff=moff: e.tensor_scalar(
                out=sdst, in0=modT[:, moff:moff + 8, :], scalar1=1.0, scalar2=None, op0=ALU.add),
                reads=['modT'], writes=[nm])
            S.add('dve', lambda e, sdst=sdst: e.tensor_tensor(
                out=sdst, in0=sdst, in1=gcol.unsqueeze(2).to_broadcast([128, 8, 2]), op=ALU.mult),
                reads=[nm, 'gcol'], writes=[nm])
        bcast_tile(BT['g1x'], modT[:, 16:24, :], 0, 'modT')
        bcast_tile(BT['g1c'], modT[:, 16:24, :], 1, 'modT')
        bcast_tile(BT['g2x'], modT[:, 40:48, :], 0, 'modT')
        bcast_tile(BT['g2c'], modT[:, 40:48, :], 1, 'modT')
        bcast_tile(BT['s2x'], s2, 0, 's2')
        bcast_tile(BT['s2c'], s2, 1, 's2')
        bcast_tile(BT['h2x'], modT[:, 24:32, :], 0, 'modT')
        bcast_tile(BT['h2c'], modT[:, 24:32, :], 1, 'modT')
        S.add('sp', lambda e: [e.dma_start(out=rwp, in_=r_w[l])], writes=['rwp'], ndma=1)
        S.add('sp', lambda e: [e.dma_start(out=rbt, in_=r_b[l])], writes=['rbt'], ndma=1)
        for j in range(2):
            S.add('dve', lambda e, j=j: e.tensor_tensor(out=rwx[:, :, j, :], in0=rwp,
                                                        in1=s2[:, :, j:j + 1].to_broadcast([128, 8, NE]), op=ALU.mult),
                  reads=['rwp', 's2'], writes=[('rwx', j)])
            for kc in range(8):
                S.add('pe', lambda e, j=j, kc=kc: e.matmul(bank(2)[0:1, j * NE:(j + 1) * NE], lhsT=modT[:, 24 + kc, j:j + 1],
                                                          rhs=rwp[:, kc, :], start=(kc == 0), stop=(kc == 7)),
                      reads=['modT', 'rwp'], writes=['bk2'])
            S.add('dve', lambda e, j=j: e.tensor_tensor(out=rcst[:, j, :], in0=bank(2)[0:1, j * NE:(j + 1) * NE], in1=rbt, op=ALU.add),
                  reads=['bk2', 'rbt'], writes=[('rcst', j)])
        S.barrier()

        if 'stop_P0' in dbg:
            return
        A.reset(PERSIST)
        win = A.tile([128, 8, 3328], BF16, 'win')
        for kc in range(8):
            S.add('pool', lambda e, kc=kc: [e.dma_start(out=win[:, kc, :], in_=w_in[l, kc * 128:(kc + 1) * 128, :])],
                  writes=[('win', kc)], ndma=1)
        WIN = [('win', kc) for kc in range(8)]
        xt_p1 = [A.tile([128, D], F32, 'xt') for _ in range(2)]
        xn = [A.tile([128, D], BF16, 'xn') for _ in range(2)]
        junk_p1 = A.tile([128, D], BF16, 'junk')
        ss_p1 = [A.tile([128, 1], F32, 'ss') for _ in range(2)]
        hT = [A.tile([128, 8, 512], BF16, 'hT') for _ in range(2)]
        fsb = [A.tile([128, 512], BF16, 'fsb') for _ in range(2)]
        vst = [A.tile([128, 4, 128], BF16, 'vst') for _ in range(2)]
        qst = [A.tile([64, 4, 8, 128], BF16, 'qst') for _ in range(2)]
        gst = [A.tile([128, 4, 16, 128], BF16, 'gst') for _ in range(2)]
        qraw = [A.tile([128, 512], BF16, 'qraw') for _ in range(2)]
        for b_ in range(2):
            S.add('pool', lambda e, b_=b_: e.memset(qraw[b_], 0.0), writes=[('qraw', b_)])
        qro = [A.tile([64, 512], BF16, 'qro') for _ in range(2)]
        rt1 = [A.tile([64, 512], F32, 'rt1') for _ in range(2)]
        rt2 = [A.tile([64, 512], F32, 'rt2') for _ in range(2)]
        cst = A.tile([64, 512], F32, 'cst')
        snt = A.tile([64, 512], F32, 'snt')
        nst = (T + 511) // 512
        it = 0
        ih = 0
        for st in range(nst):
            if 'one_st' in dbg and st > 0:
                break
            t0 = st * 512
            ntok = min(512, T - t0)
            ntile = ntok // 128
            isx = t0 < TX
            j = 0 if isx else 1
            hb = hT[st % 2]
            HK = ('hT', st % 2)
            for ti in range(ntile):
                b = it % 2
                it += 1
                r0 = t0 + ti * 128
                S.add(dmaq(), lambda e, b=b, r0=r0: [e.dma_start(out=xt_p1[b], in_=xres[r0:r0 + 128, :])],
                      writes=[('xt', b)], ndma=1)
                S.add('act', lambda e, b=b: e.activation(out=junk_p1, in_=xt_p1[b], func=AF.Square, accum_out=ss_p1[b]),
                      reads=[('xt', b)], writes=[('ss', b), 'junk'])
                S.add('dve', lambda e, b=b: e.tensor_scalar(out=ss_p1[b], in0=ss_p1[b], scalar1=1.0 / D, scalar2=EPS,
                                                            op0=ALU.mult, op1=ALU.add), reads=[('ss', b)], writes=[('ss', b)])
                S.add('act', lambda e, b=b: e.sqrt(out=ss_p1[b], in_=ss_p1[b]), reads=[('ss', b)], writes=[('ss', b)])
                S.add('dve', lambda e, b=b: e.reciprocal(out=ss_p1[b], in_=ss_p1[b]), reads=[('ss', b)], writes=[('ss', b)])
                S.add('act', lambda e, b=b: e.activation(out=xn[b], in_=xt_p1[b], func=AF.Copy, scale=ss_p1[b][:, 0:1]),
                      reads=[('xt', b), ('ss', b)], writes=[('xn', b)])
                pb = pbf[b]
                for kc in range(8):
                    S.add('pe', lambda e, b=b, kc=kc, pb=pb: e.transpose(out=pb[:, kc * 128:(kc + 1) * 128],
                                                                        in_=xn[b][:, kc * 128:(kc + 1) * 128], identity=C['ident_bf']),
                          reads=[('xn', b), 'ident_bf'], writes=[('pbf', b)])
                S.add('dve', lambda e, pb=pb, j=j, hb=hb, ti=ti: e.tensor_tensor(
                    out=hb[:, :, ti * 128:(ti + 1) * 128], in0=pb.rearrange("p (k t) -> p k t", t=128),
                    in1=s1[:, :, j:j + 1].to_broadcast([128, 8, 128]), op=ALU.mult),
                    reads=[('pbf', b), 's1'], writes=[HK])
                S.add('pool', lambda e, j=j, hb=hb, ti=ti: e.tensor_tensor(
                    out=hb[:, :, ti * 128:(ti + 1) * 128], in0=hb[:, :, ti * 128:(ti + 1) * 128],
                    in1=modT[:, 0:8, j:j + 1].to_broadcast([128, 8, 128]), op=ALU.add),
                    reads=[HK, 'modT'], writes=[HK])
                if 'p1_norm' in dbg:
                    continue
                bk = bank(ti % 2)
                for kc in range(8):
                    S.add('pe', lambda e, hb=hb, ti=ti, kc=kc, bk=bk: e.matmul(
                        bk, lhsT=hb[:, kc, ti * 128:(ti + 1) * 128], rhs=win[:, kc, 0:512], start=(kc == 0), stop=(kc == 7)),
                        reads=[HK] + WIN, writes=[('bk', ti % 2)])
                fb = fsb[ti % 2]
                S.add('act', lambda e, fb=fb, bk=bk: e.copy(out=fb, in_=bk), reads=[('bk', ti % 2)], writes=[('fsb', ti % 2)])
                S.add('sp', lambda e, fb=fb, r0=r0: [e.dma_start(out=f_tm[r0:r0 + 128, :], in_=fb)],
                      reads=[('fsb', ti % 2)], writes=[('f_tm', r0)], ndma=1)
                bk = bank(2 + ti % 2)
                for kc in range(8):
                    S.add('pe', lambda e, hb=hb, ti=ti, kc=kc, bk=bk: e.matmul(
                        bk[:, 0:128], lhsT=hb[:, kc, ti * 128:(ti + 1) * 128], rhs=win[:, kc, 1152:1280], start=(kc == 0), stop=(kc == 7)),
                        reads=[HK] + WIN, writes=[('bk', 2 + ti % 2)])
                S.add('dve', lambda e, bk=bk, ti=ti, st=st: e.tensor_copy(out=vst[st % 2][:, ti, :], in_=bk[:, 0:128]),
                      reads=[('bk', 2 + ti % 2)], writes=[('vst', st % 2)])
            if 'p1_norm' not in dbg:
                S.add('sp', lambda e, st=st, ntile=ntile: [e.dma_start(out=v_blk[:, st * 4:st * 4 + ntile, :], in_=vst[st % 2][:, 0:ntile, :])],
                      reads=[('vst', st % 2)], writes=[('v_blk', st)], ndma=1)
            if 'p1_norm' in dbg or 'p1_fv' in dbg:
                continue
            if isx:
                S.add('sp', lambda e, t0=t0: [e.dma_start(out=cst, in_=cd['cosT'][:, t0:t0 + 512])], writes=['cst'], ndma=1)
                S.add('act', lambda e, t0=t0: [e.dma_start(out=snt, in_=cd['sinT'][:, t0:t0 + 512])], writes=['snt'], ndma=1)
            for hh in range(10):
                col0 = 512 + hh * 64
                b = ih % 2
                ih += 1
                bk = bank(4)
                for kc in range(8):
                    S.add('pe', lambda e, hb=hb, kc=kc, col0=col0, bk=bk, ntok=ntok: e.matmul(
                        bk[0:64, 0:ntok], lhsT=win[:, kc, col0:col0 + 64], rhs=hb[:, kc, 0:ntok], start=(kc == 0), stop=(kc == 7)),
                        reads=[HK] + WIN, writes=['bk4'])
                QSK = ('qst', st % 2)
                if hh < 8:
                    fin = qst[st % 2][:, 0:ntile, hh, :]
                    fink = [QSK]
                else:
                    fin = qro[b][:, 0:ntok].rearrange("p (a t) -> p a t", t=128)
                    fink = [('qro', b)]
                if isx:
                    S.add('act', lambda e, b=b, bk=bk: e.copy(out=qraw[b][0:64, :], in_=bk[0:64, :]), reads=['bk4'], writes=[('qraw', b)])
                    bk5 = bank(5)
                    S.add('pe', lambda e, b=b, bk5=bk5: e.matmul(bk5, lhsT=C['rotm'], rhs=qraw[b], start=True, stop=True),
                          reads=[('qraw', b), 'rotm'], writes=['bk5'])
                    S.add('act', lambda e, b=b, bk=bk: e.copy(out=rt1[b], in_=bk[0:64, :]), reads=['bk4'], writes=[('rt1', b)])
                    S.add('act', lambda e, b=b, bk5=bk5: e.copy(out=rt2[b], in_=bk5[0:64, :]), reads=['bk5'], writes=[('rt2', b)])
                    S.add('pool', lambda e, b=b: e.tensor_tensor(out=rt1[b], in0=rt1[b], in1=cst, op=ALU.mult),
                          reads=[('rt1', b), 'cst'], writes=[('rt1', b)])
                    S.add('pool', lambda e, b=b: e.tensor_tensor(out=rt2[b], in0=rt2[b], in1=snt, op=ALU.mult),
                          reads=[('rt2', b), 'snt'], writes=[('rt2', b)])
                    S.add('pool', lambda e, b=b, fin=fin: e.tensor_tensor(
                        out=fin, in0=rt1[b].rearrange("p (a t) -> p a t", t=128), in1=rt2[b].rearrange("p (a t) -> p a t", t=128), op=ALU.add),
                        reads=[('rt1', b), ('rt2', b)], writes=fink)
                else:
                    S.add('act', lambda e, bk=bk, ntok=ntok, fin=fin: e.copy(out=fin, in_=bk[0:64, 0:ntok].rearrange("p (a t) -> p a t", t=128)),
                          reads=['bk4'], writes=fink)
                if hh >= 8:
                    S.add(dmaq(), lambda e, b=b, hh=hh, t0=t0, ntok=ntok: [e.dma_start(out=kT_all[:, hh - 8, t0:t0 + ntok], in_=qro[b][:, 0:ntok])],
                          reads=[('qro', b)], writes=[('kT_all', hh, st)], ndma=1)
                elif hh == 7:
                    S.add(dmaq(), lambda e, st=st, ntile=ntile: [e.dma_start(
                        out=q_blk[st * 4:st * 4 + ntile].rearrange("b d h t -> d b (h t)"),
                        in_=qst[st % 2][:, 0:ntile, :, :].rearrange("p a h t -> p a (h t)"))],
                        reads=[QSK], writes=[('q_blk', st)], ndma=1)
            if 'p1_qk' in dbg:
                continue
            for m in range(16):
                col0 = 1280 + m * 128
                bk = bank(m % 2)
                for kc in range(8):
                    S.add('pe', lambda e, hb=hb, kc=kc, col0=col0, bk=bk, ntok=ntok: e.matmul(
                        bk[:, 0:ntok], lhsT=win[:, kc, col0:col0 + 128], rhs=hb[:, kc, 0:ntok], start=(kc == 0), stop=(kc == 7)),
                        reads=[HK] + WIN, writes=[('bk', m % 2)])
                S.add('act', lambda e, bk=bk, ntok=ntok, ntile=ntile, m=m, st=st: e.activation(
                    out=gst[st % 2][:, 0:ntile, m, :], in_=bk[:, 0:ntok].rearrange("p (a t) -> p a t", t=128), func=AF.Sigmoid),
                    reads=[('bk', m % 2)], writes=[('gst', st % 2)])
            S.add(dmaq(), lambda e, st=st, ntile=ntile: [e.dma_start(
                out=g_blk[st * 4:st * 4 + ntile].rearrange("b p m t -> p b (m t)"),
                in_=gst[st % 2][:, 0:ntile, :, :].rearrange("p a m t -> p a (m t)"))],
                reads=[('gst', st % 2)], writes=[('g_blk', st)], ndma=1)
        S.barrier()

        if 'stop_P1' in dbg:
            return
        A.reset(PERSIST)
        wao = A.tile([64, 8, D], BF16, 'wao')
        S.add('pool', lambda e: [e.dma_start(out=wao, in_=w_ao[l].rearrange("(h d) n -> d h n", d=64))], writes=['wao'], ndma=1)
        kall = A.tile([64, 2, T], BF16, 'kall')
        S.add('sp', lambda e: [e.dma_start(out=kall, in_=kT_all)], writes=['kall'], ndma=1)
        vall = A.tile([128, NT, 128], BF16, 'vall')
        S.add('act', lambda e: [e.dma_start(out=vall, in_=v_blk)], writes=['vall'], ndma=1)
        skr = A.tile([64, 8], F32, 'skr')
        S.add('sp', lambda e: [e.dma_start(out=skr, in_=sink[l:l + 1, :].to_broadcast([64, 8]))], writes=['skr'], ndma=1)
        S.add('act', lambda e: e.activation(out=skr, in_=skr, func=AF.Exp), reads=['skr'], writes=['skr'])
        esk = A.tile([64, 8, 128], F32, 'esk')
        S.add('dve', lambda e: e.tensor_copy(out=esk, in_=skr.unsqueeze(2).to_broadcast([64, 8, 128])), reads=['skr'], writes=['esk'])
        qb_sb = [A.tile([64, 8, 128], BF16, 'qb') for _ in range(2)]
        pT = [A.tile([128, 512], BF16, 'pT') for _ in range(3)]
        rec = A.tile([64, 512], F32, 'rec')
        pos_ = A.tile([64, 512], F32, 'pos_')
        atT = [A.tile([64, 8, 128], BF16, 'atT') for _ in range(2)]
        gab = [A.tile([128, 8, 128], BF16, 'gab') for _ in range(2)]
        t2b = [A.tile([128, 8, 128], BF16, 't2b') for _ in range(2)]
        ip = 0
        for qb in range(NT):
            b = qb % 2
            r0 = qb * 128
            isx = qb < NTX
            S.add('sp', lambda e, b=b, qb=qb: [e.dma_start(out=qb_sb[b], in_=q_blk[qb])], writes=[('qb', b)], ndma=1)
            S.add('act', lambda e, b=b, qb=qb: [e.dma_start(out=gab[b], in_=g_blk[qb][:, 8:16, :])], writes=[('gab', b)], ndma=1)
            chunks = []
            if isx:
                lo = max(qb - 1, 0)
                hi = min(qb + 1, NTX - 1)
                for kbk in range(lo, hi + 1):
                    ci = kbk
                    mk = None if kbk == qb else ('mask_prev' if kbk < qb else 'mask_next')
                    chunks.append(('loc', ci, mk))
            chunks.append(('loc', NTX, None))
            chunks.append(('loc', NTX + 1, None))
            for kv in range(2):
                po = bank(2 + kv * 2)
                pd = bank(3 + kv * 2)
                POK = ('bk', 2 + kv * 2)
                PDK = ('bk', 3 + kv * 2)
                for cidx, (kind, ci, mk) in enumerate(chunks):
                    sb = bank(cidx % 2)
                    SBK = ('bk', cidx % 2)
                    kl = kall[:, kv, ci * 128:(ci + 1) * 128]
                    vl = vall[:, ci, kv * 64:(kv + 1) * 64]
                    rk = ['kall']
                    rv = ['vall']
                    S.add('pe', lambda e, sb=sb, kl=kl, b=b, kv=kv: e.matmul(
                        sb.rearrange("p (h t) -> p h t", t=128), lhsT=kl, rhs=qb_sb[b][:, kv * 4:(kv + 1) * 4, :], start=True, stop=True),
                        reads=rk + [('qb', b)], writes=[SBK])
                    pt = pT[ip % 3]
                    PTK = ('pT', ip % 3)
                    ip += 1
                    S.add('act', lambda e, pt=pt, sb=sb: e.activation(out=pt, in_=sb, func=AF.Exp, scale=0.125),
                          reads=[SBK], writes=[PTK])
                    if mk is not None:
                        S.add('pool', lambda e, pt=pt, mk=mk: e.tensor_tensor(
                            out=pt.rearrange("p (h t) -> p h t", t=128), in0=pt.rearrange("p (h t) -> p h t", t=128),
                            in1=C[mk].unsqueeze(1).to_broadcast([128, 4, 128]), op=ALU.mult),
                            reads=[PTK, mk], writes=[PTK])
                    first = cidx == 0
                    last = cidx == len(chunks) - 1
                    S.add('pe', lambda e, po=po, vl=vl, pt=pt, first=first, last=last: e.matmul(
                        po[0:64, :], lhsT=vl, rhs=pt, start=first, stop=last), reads=rv + [PTK], writes=[POK])
                    S.add('pe', lambda e, pd=pd, pt=pt, first=first, last=last: e.matmul(
                        pd[0:64, :], lhsT=ones_bf[:, 0:64], rhs=pt, start=first, stop=last), reads=['ones_bf', PTK], writes=[PDK])
                S.add('act', lambda e, pd=pd: e.copy(out=rec, in_=pd[0:64, :]), reads=[PDK], writes=['rec'])
                S.add('act', lambda e, po=po: e.copy(out=pos_, in_=po[0:64, :]), reads=[POK], writes=['pos_'])
                S.add('dve', lambda e, kv=kv: e.tensor_tensor(
                    out=rec, in0=rec, in1=esk[:, kv * 4:(kv + 1) * 4, :].rearrange("p h t -> p (h t)"), op=ALU.add),
                    reads=['rec', 'esk'], writes=['rec'])
                S.add('dve', lambda e: e.reciprocal(out=rec, in_=rec), reads=['rec'], writes=['rec'])
                S.add('pool', lambda e, kv=kv, b=b: e.tensor_tensor(
                    out=atT[b][:, kv * 4:(kv + 1) * 4, :].rearrange("p h t -> p (h t)"), in0=pos_, in1=rec, op=ALU.mult),
                    reads=['pos_', 'rec'], writes=[('atT', b)])
            for m in range(8):
                ob = bank(m % 2)
                OBK = ('bk', m % 2)
                for h in range(8):
                    S.add('pe', lambda e, ob=ob, h=h, m=m, b=b: e.matmul(
                        ob[:, 0:128], lhsT=wao[:, h, m * 128:(m + 1) * 128], rhs=atT[b][:, h, :], start=(h == 0), stop=(h == 7)),
                        reads=['wao', ('atT', b)], writes=[OBK])
                S.add('dve', lambda e, ob=ob, m=m, b=b: e.tensor_tensor(
                    out=t2b[b][:, m, :], in0=ob[:, 0:128], in1=gab[b][:, m, :], op=ALU.mult),
                    reads=[OBK, ('gab', b)], writes=[('t2b', b)])
            S.add('sp', lambda e, b=b, qb=qb: [e.dma_start(out=t2_blk[qb], in_=t2b[b])],
                  reads=[('t2b', b)], writes=[('t2_blk', qb)], ndma=1)
        S.barrier()

        if 'stop_P2' in dbg:
            return
        A.reset(PERSIST)
        fmT = [A.tile([128, T], BF16, 'fmT') for _ in range(4)]
        MK3 = A.mark()
        Dt = [A.tile([128, 8, 512], BF16, 'Dt') for _ in range(2)]
        Zc = [A.tile([128, 2, 8, 512], BF16, 'Zc') for _ in range(2)]
        tt1 = [A.tile([128, 512], F32, 'tt1') for _ in range(2)]
        tt2 = [A.tile([128, 512], F32, 'tt2') for _ in range(2)]
        tt3 = [A.tile([128, 512], F32, 'tt3') for _ in range(2)]
        tt4 = [A.tile([128, 512], F32, 'tt4') for _ in range(2)]
        fview = f_tm[0:TX, :].rearrange("(n1 n2) c -> n1 n2 c", n2=64)
        for n2c in range(8):
            b = n2c % 2
            S.add(dmaq(), lambda e, b=b, n2c=n2c: [e.dma_start(out=Dt[b], in_=fview[:, n2c * 8:(n2c + 1) * 8, :])],
                  writes=[('Dt', b)], ndma=1)
            for jj in range(8):
                n2 = n2c * 8 + jj
                bb = jj % 2
                pr = bank(bb * 2)
                ps = bank(bb * 2 + 1)
                S.add('pe', lambda e, pr=pr, b=b, jj=jj: e.matmul(pr, lhsT=C['c128'], rhs=Dt[b][:, jj, :], start=True, stop=True),
                      reads=[('Dt', b), 'c128'], writes=[('bk', bb * 2)])
                S.add('pe', lambda e, ps=ps, b=b, jj=jj: e.matmul(ps, lhsT=C['s128'], rhs=Dt[b][:, jj, :], start=True, stop=True),
                      reads=[('Dt', b), 's128'], writes=[('bk', bb * 2 + 1)])
                S.add('act', lambda e, pr=pr, bb=bb, n2=n2: e.activation(out=tt1[bb], in_=pr, func=AF.Copy, scale=C['twr'][:, n2:n2 + 1]),
                      reads=[('bk', bb * 2), 'twr'], writes=[('tt1', bb)])
                S.add('act', lambda e, pr=pr, bb=bb, n2=n2: e.activation(out=tt2[bb], in_=pr, func=AF.Copy, scale=C['twi'][:, n2:n2 + 1]),
                      reads=[('bk', bb * 2), 'twi'], writes=[('tt2', bb)])
                S.add('act', lambda e, ps=ps, bb=bb, n2=n2: e.activation(out=tt3[bb], in_=ps, func=AF.Copy, scale=C['ntwi'][:, n2:n2 + 1]),
                      reads=[('bk', bb * 2 + 1), 'ntwi'], writes=[('tt3', bb)])
                S.add('act', lambda e, ps=ps, bb=bb, n2=n2: e.activation(out=tt4[bb], in_=ps, func=AF.Copy, scale=C['twr'][:, n2:n2 + 1]),
                      reads=[('bk', bb * 2 + 1), 'twr'], writes=[('tt4', bb)])
                S.add('dve', lambda e, bb=bb, b=b, jj=jj: e.tensor_tensor(out=Zc[b][:, 0, jj, :], in0=tt1[bb], in1=tt3[bb], op=ALU.add),
                      reads=[('tt1', bb), ('tt3', bb)], writes=[('Zc', b)])
                S.add('pool', lambda e, bb=bb, b=b, jj=jj: e.tensor_tensor(out=Zc[b][:, 1, jj, :], in0=tt2[bb], in1=tt4[bb], op=ALU.add),
                      reads=[('tt2', bb), ('tt4', bb)], writes=[('Zc', b)])
            for ri in range(2):
                S.add(dmaq(), lambda e, b=b, ri=ri, n2c=n2c: [e.dma_start(out=Zd[ri, :, n2c * 8:(n2c + 1) * 8, :], in_=Zc[b][:, ri, :, :])],
                      reads=[('Zc', b)], writes=[('Zd', ri, n2c)], ndma=1)
        S.barrier()
        if 'p3_s1' in dbg:
            return
        A.reset(MK3)
        Pg = A.tile([128, 2, 2, TX], BF16, 'Pg')
        Zs = [A.tile([128, 8, 256], BF16, 'Zs') for _ in range(2)]
        iz = 0
        for gp in range(2):
            for k1c in range(16):
                b = iz % 2
                iz += 1
                for ri in range(2):
                    S.add(dmaq(), lambda e, b=b, ri=ri, k1c=k1c, gp=gp: [e.dma_start(
                        out=Zs[b][ri * 64:(ri + 1) * 64, :, :],
                        in_=Zd[ri, k1c * 8:(k1c + 1) * 8, :, gp * 256:(gp + 1) * 256].rearrange("k n c -> n k c"))],
                        writes=[('Zs', b, ri)], ndma=1)
                for gi in range(2):
                    pz = pq[gi]
                    PZK = ('pq', gi)
                    for kj in range(8):
                        S.add('pe', lambda e, pz=pz, b=b, kj=kj, gi=gi: e.matmul(pz[:, kj * 128:(kj + 1) * 128], lhsT=Zs[b][:, kj, gi * 128:(gi + 1) * 128],
                                                                                rhs=C['r2'], start=True, stop=True),
                              reads=[('Zs', b, 0), ('Zs', b, 1), 'r2'], writes=[PZK])
                    for ri in range(2):
                        for hf in range(2):
                            outv = Pg[:, gi, ri, :].rearrange("c (k1 k2) -> c k1 k2", k2=64)[:, k1c * 8 + hf * 4:k1c * 8 + hf * 4 + 4, :]
                            inv = pz[:, hf * 512:(hf + 1) * 512].rearrange("c (k1 r k2) -> c k1 r k2", r=2, k2=64)[:, :, ri, :]
                            if (ri + hf) % 2 == 0:
                                S.add('act', lambda e, outv=outv, inv=inv: e.copy(out=outv, in_=inv), reads=[PZK], writes=[('Pg', gi, ri)])
                            else:
                                S.add('dve', lambda e, outv=outv, inv=inv: e.tensor_copy(out=outv, in_=inv), reads=[PZK], writes=[('Pg', gi, ri)])
            for gi in range(2):
                g = gp * 2 + gi
                for tc in range(16):
                    bk = bank(4 + tc % 2)
                    BKK = ('bk', 4 + tc % 2)
                    S.add('pe', lambda e, bk=bk, tc=tc, gi=gi: e.matmul(bk, lhsT=C['cc'], rhs=Pg[:, gi, 0, tc * 512:(tc + 1) * 512], start=True, stop=False),
                          reads=[('Pg', gi, 0), 'cc'], writes=[BKK])
                    S.add('pe', lambda e, bk=bk, tc=tc, gi=gi: e.matmul(bk, lhsT=C['nsc'], rhs=Pg[:, gi, 1, tc * 512:(tc + 1) * 512], start=False, stop=True),
                          reads=[('Pg', gi, 1), 'nsc'], writes=[BKK])
                    if tc % 2 == 0:
                        S.add('act', lambda e, bk=bk, tc=tc, g=g: e.copy(out=fmT[g][:, tc * 512:(tc + 1) * 512], in_=bk), reads=[BKK], writes=[('fmT', g)])
                    else:
                        S.add('dve', lambda e, bk=bk, tc=tc, g=g: e.tensor_copy(out=fmT[g][:, tc * 512:(tc + 1) * 512], in_=bk), reads=[BKK], writes=[('fmT', g)])
        if 'p3_s2' in dbg:
            S.barrier()
            return
        Dc = A.tile([128, 2, 512], BF16, 'Dc')
        Pc = A.tile([128, 2, TC], BF16, 'Pc')
        S.add('sp', lambda e: [e.dma_start(out=Dc, in_=f_tm[TX:T, :].rearrange("(c p) f -> p c f", p=128))], writes=['Dc'], ndma=1)
        for g in range(4):
            for ri, cn in enumerate(('c256', 's256')):
                bk = bank(ri)
                for ch in range(2):
                    S.add('pe', lambda e, bk=bk, ch=ch, g=g, cn=cn: e.matmul(bk[:, 0:TC], lhsT=Dc[:, ch, g * 128:(g + 1) * 128], rhs=C[cn][:, ch, :],
                                                                            start=(ch == 0), stop=(ch == 1)),
                          reads=['Dc', cn], writes=[('bk', ri)])
                S.add('act', lambda e, bk=bk, ri=ri: e.copy(out=Pc[:, ri, :], in_=bk[:, 0:TC]), reads=[('bk', ri)], writes=[('Pc', ri)])
            bk = bank(2)
            S.add('pe', lambda e, bk=bk: e.matmul(bk[:, 0:TC], lhsT=C['cc'], rhs=Pc[:, 0, :], start=True, stop=False), reads=[('Pc', 0), 'cc'], writes=['bk2'])
            S.add('pe', lambda e, bk=bk: e.matmul(bk[:, 0:TC], lhsT=C['nsc'], rhs=Pc[:, 1, :], start=False, stop=True), reads=[('Pc', 1), 'nsc'], writes=['bk2'])
            S.add('dve', lambda e, bk=bk, g=g: e.tensor_copy(out=fmT[g][:, TX:T], in_=bk[:, 0:TC]), reads=['bk2'], writes=[('fmT', g)])
        S.barrier()

        if 'stop_P3' in dbg:
            return
        A.reset(MK3)
        wfo = A.tile([128, 4, D], BF16, 'wfo')
        S.add('pool', lambda e: [e.dma_start(out=wfo, in_=w_fo[l].rearrange("(g p) n -> p g n", p=128))], writes=['wfo'], ndma=1)
        wou = A.tile([128, 8, D], BF16, 'wou')
        S.add('pool', lambda e: [e.dma_start(out=wou, in_=w_out[l].rearrange("(k p) n -> p k n", p=128))], writes=['wou'], ndma=1)
        sgf = [A.tile([128, 4, 8, 128], BF16, 'sgf') for _ in range(2)]
        t2s = [A.tile([128, 4, 8, 128], BF16, 't2s')] * 2
        yT = [A.tile([128, 8, 512], BF16, 'yT')] * 2
        ut = A.tile([128, 512], F32, 'ut')
        xt_p4 = [A.tile([128, D], F32, 'xt4') for _ in range(2)]
        xo_p4 = [A.tile([128, D], F32, 'xo4')] * 2
        it = 0
        for st in range(nst):
            t0 = st * 512
            ntok = min(512, T - t0)
            ntile = ntok // 128
            isx = t0 < TX
            b = st % 2
            S.add('sp', lambda e, b=b, st=st, ntile=ntile: [e.dma_start(
                out=sgf[b][:, 0:ntile, :, :].rearrange("p a m t -> p a (m t)"),
                in_=g_blk[st * 4:st * 4 + ntile, :, 0:8, :].rearrange("b p m t -> p b (m t)"))], writes=[('sgf', b)], ndma=1)
            S.add('act', lambda e, b=b, st=st, ntile=ntile: [e.dma_start(
                out=t2s[b][:, 0:ntile, :, :].rearrange("p a m t -> p a (m t)"),
                in_=t2_blk[st * 4:st * 4 + ntile].rearrange("b p m t -> p b (m t)"))], writes=[('t2s', 0)], ndma=1)
            for m in range(8):
                bk = bank(m % 2)
                BKK = ('bk', m % 2)
                for g in range(4):
                    if isx:
                        rhs = fmT[g][:, 0:TX].rearrange("c (k1 k2) -> c k2 k1", k2=64)[:, st * 4:st * 4 + 4, :]
                    else:
                        rhs = fmT[g][:, TX:T].rearrange("c (a t) -> c a t", t=128)
                    S.add('pe', lambda e, bk=bk, g=g, m=m, rhs=rhs, ntok=ntok: e.matmul(
                        bk[:, 0:ntok].rearrange("p (a t) -> p a t", t=128), lhsT=wfo[:, g, m * 128:(m + 1) * 128], rhs=rhs, start=(g == 0), stop=(g == 3)),
                        reads=['wfo', ('fmT', g)], writes=[BKK])
                S.add('dve', lambda e, bk=bk, b=b, m=m, ntok=ntok, ntile=ntile: e.tensor_tensor(
                    out=ut[:, 0:ntok].rearrange("p (a t) -> p a t", t=128), in0=bk[:, 0:ntok].rearrange("p (a t) -> p a t", t=128),
                    in1=sgf[b][:, 0:ntile, m, :], op=ALU.mult), reads=[BKK, ('sgf', b)], writes=['ut'])
                S.add('pool', lambda e, b=b, m=m, ntok=ntok, ntile=ntile: e.tensor_tensor(
                    out=yT[b][:, m, 0:ntok].rearrange("p (a t) -> p a t", t=128), in0=ut[:, 0:ntok].rearrange("p (a t) -> p a t", t=128),
                    in1=t2s[b][:, 0:ntile, m, :], op=ALU.add), reads=['ut', ('t2s', 0)], writes=[('yT', 0)])
            for ti in range(ntile):
                xb = it % 2
                it += 1
                r0 = t0 + ti * 128
                S.add(dmaq(), lambda e, xb=xb, r0=r0: [e.dma_start(out=xt_p4[xb], in_=xres[r0:r0 + 128, :])], writes=[('xt4', xb)], ndma=1)
                gb = BT['g1x'] if isx else BT['g1c']
                for n in range(2):
                    bk = bank(2 + n)
                    BKK = ('bk', 2 + n)
                    for kc in range(8):
                        S.add('pe', lambda e, bk=bk, b=b, kc=kc, ti=ti, n=n: e.matmul(
                            bk, lhsT=yT[b][:, kc, ti * 128:(ti + 1) * 128], rhs=wou[:, kc, n * 512:(n + 1) * 512], start=(kc == 0), stop=(kc == 7)),
                            reads=[('yT', 0), 'wou'], writes=[BKK])
                    S.add('act', lambda e, bk=bk, xb=xb, n=n: e.copy(out=xo_p4[xb][:, n * 512:(n + 1) * 512], in_=bk),
                          reads=[BKK], writes=[('xo4', 0, n)])
                    S.add('dve', lambda e, xb=xb, n=n, gb=gb: e.tensor_tensor(
                        out=xo_p4[xb][:, n * 512:(n + 1) * 512], in0=xo_p4[xb][:, n * 512:(n + 1) * 512], in1=gb[:, n * 512:(n + 1) * 512], op=ALU.mult),
                        reads=[('xo4', 0, n), ('btile', id(gb))], writes=[('xo4', 0, n)])
                    S.add('pool', lambda e, xb=xb, n=n: e.tensor_tensor(
                        out=xo_p4[xb][:, n * 512:(n + 1) * 512], in0=xo_p4[xb][:, n * 512:(n + 1) * 512], in1=xt_p4[xb][:, n * 512:(n + 1) * 512], op=ALU.add),
                        reads=[('xo4', 0, n), ('xt4', xb)], writes=[('xo4', 0, n)])
                S.add(dmaq(), lambda e, xb=xb, r0=r0: [e.dma_start(out=xres[r0:r0 + 128, :], in_=xo_p4[xb])],
                      reads=[('xo4', 0, 0), ('xo4', 0, 1)], writes=[('xres', r0)], ndma=1)
        S.barrier()

        if 'stop_P4' in dbg:
            return
        if 'skip_moe' in dbg:
            return
        A.reset(PERSIST)
        LG = A.tile([128, NT, NE], F32, 'LG')
        V8 = A.tile([128, NT, 8], F32, 'V8')
        MK = A.tile([128, NT, NE], BF16, 'MK')
        G4 = A.tile([128, NT, 4], F32, 'G4')
        DESTi = A.tile([128, NT, 4], I32, 'DESTi')
        IDXW = A.tile([128, NB], I32, 'IDXW')
        IDXE = A.tile([2, NB], I32, 'IDXE')
        MK5 = A.mark()
        xt_p5 = [A.tile([128, D], F32, 'xt5') for _ in range(2)]
        xnf = [A.tile([128, D], F32, 'xnf') for _ in range(2)]
        xTf = [A.tile([128, 8, 128], F32, 'xTf') for _ in range(2)]
        h2a = [A.tile([128, D], F32, 'h2a') for _ in range(2)]
        h2b = [A.tile([128, D], BF16, 'h2b') for _ in range(2)]
        junk_p5 = A.tile([128, D], BF16, 'junk5')
        ss_p5 = [A.tile([128, 1], F32, 'ss5') for _ in range(2)]
        for tj in range(NT):
            b = tj % 2
            r0 = tj * 128
            isx = tj < NTX
            j = 0 if isx else 1
            S.add(dmaq(), lambda e, b=b, r0=r0: [e.dma_start(out=xt_p5[b], in_=xres[r0:r0 + 128, :])], writes=[('xt5', b)], ndma=1)
            S.add('act', lambda e, b=b: e.activation(out=junk_p5, in_=xt_p5[b], func=AF.Square, accum_out=ss_p5[b]),
                  reads=[('xt5', b)], writes=[('ss5', b), 'junk5'])
            S.add('dve', lambda e, b=b: e.tensor_scalar(out=ss_p5[b], in0=ss_p5[b], scalar1=1.0 / D, scalar2=EPS, op0=ALU.mult, op1=ALU.add),
                  reads=[('ss5', b)], writes=[('ss5', b)])
            S.add('act', lambda e, b=b: e.sqrt(out=ss_p5[b], in_=ss_p5[b]), reads=[('ss5', b)], writes=[('ss5', b)])
            S.add('dve', lambda e, b=b: e.reciprocal(out=ss_p5[b], in_=ss_p5[b]), reads=[('ss5', b)], writes=[('ss5', b)])
            S.add('act', lambda e, b=b: e.activation(out=xnf[b], in_=xt_p5[b], func=AF.Copy, scale=ss_p5[b][:, 0:1]),
                  reads=[('xt5', b), ('ss5', b)], writes=[('xnf', b)])
            sb_ = BT['s2x'] if isx else BT['s2c']
            hb_ = BT['h2x'] if isx else BT['h2c']
            S.add('dve', lambda e, b=b, sb_=sb_: e.tensor_tensor(out=h2a[b], in0=xnf[b], in1=sb_, op=ALU.mult),
                  reads=[('xnf', b), ('btile', id(sb_))], writes=[('h2a', b)])
            S.add('pool', lambda e, b=b, hb_=hb_: e.tensor_tensor(out=h2b[b], in0=h2a[b], in1=hb_, op=ALU.add),
                  reads=[('h2a', b), ('btile', id(hb_))], writes=[('h2b', b)])
            S.add(dmaq(), lambda e, b=b, r0=r0: [e.dma_start(out=h2_tm[r0:r0 + 128, :], in_=h2b[b])], reads=[('h2b', b)],
                  writes=[('h2_tm', tj)], ndma=1)
            pz = pq[b]
            PZK = ('pq', b)
            for kc in range(8):
                S.add('pe', lambda e, pz=pz, b=b, kc=kc: e.transpose(out=pz[:, kc * 128:(kc + 1) * 128], in_=xnf[b][:, kc * 128:(kc + 1) * 128],
                                                                    identity=C['ident_f']), reads=[('xnf', b), 'ident_f'], writes=[PZK])
            S.add('act', lambda e, pz=pz, b=b: e.copy(out=xTf[b].rearrange("p k t -> p (k t)")[:, 0:512], in_=pz[:, 0:512]), reads=[PZK], writes=[('xTf', b, 0)])
            S.add('dve', lambda e, pz=pz, b=b: e.tensor_copy(out=xTf[b].rearrange("p k t -> p (k t)")[:, 512:1024], in_=pz[:, 512:1024]), reads=[PZK], writes=[('xTf', b, 1)])
            lb = bank(4)
            for kc in range(8):
                S.add('pe', lambda e, lb=lb, b=b, kc=kc, j=j: e.matmul(lb[:, 0:NE], lhsT=xTf[b][:, kc, :], rhs=rwx[:, kc, j, :], start=(kc == 0), stop=False),
                      reads=[('xTf', b, 0), ('xTf', b, 1), ('rwx', j)], writes=['bk4'])
            S.add('pe', lambda e, lb=lb, j=j: e.matmul(lb[:, 0:NE], lhsT=ones_f[0:1, :], rhs=rcst[:, j, :], start=False, stop=True),
                  reads=['ones_f', ('rcst', j)], writes=['bk4'])
            S.add('dve', lambda e, lb=lb, tj=tj: e.tensor_copy(out=LG[:, tj, :], in_=lb[:, 0:NE]), reads=['bk4'], writes=[('LG', tj)])
            S.add('dve', lambda e, tj=tj: e.max(out=V8[:, tj, :], in_=LG[:, tj, :]), reads=[('LG', tj)], writes=[('V8', tj)])
            S.add('dve', lambda e, tj=tj: e.tensor_scalar(out=MK[:, tj, :], in0=LG[:, tj, :], scalar1=V8[:, tj, 3:4], scalar2=None, op0=ALU.is_ge),
                  reads=[('LG', tj), ('V8', tj)], writes=[('MK', tj)])
        S.barrier()
        A.reset(MK5)
        NC_ = NT * NE
        POS = A.tile([128, NT, NE], F32, 'POS')
        CNT = A.tile([128, NT, NE], F32, 'CNT')
        TB = A.tile([128, NT, NE], F32, 'TB')
        EQ = A.tile([128, NT, NE], F32, 'EQ')
        TOT = A.tile([128, NE], F32, 'TOT')
        PAD = A.tile([128, NE], F32, 'PAD')
        PADi = A.tile([128, NE], I32, 'PADi')
        PS_ = A.tile([128, NE], F32, 'PS')
        PEND = A.tile([128, NE], F32, 'PEND')
        pendc = A.tile([32, 1], F32, 'pendc')
        cmpt = A.tile([32, NB], BF16, 'cmpt')
        EB = A.tile([128, NB], F32, 'EB')
        EB2 = A.tile([128, NB], F32, 'EB2')
        DESTf = A.tile([128, NT, 4], F32, 'DESTf')
        gs = A.tile([128, NT], F32, 'gs')
        MKf = MK.rearrange("p t e -> p (t e)")
        for c0 in range(0, NC_, 512):
            w = min(512, NC_ - c0)
            S.add('pe', lambda e, c0=c0, w=w: e.matmul(bank(0)[:, 0:w], lhsT=C['ustrict'], rhs=MKf[:, c0:c0 + w], start=True, stop=True),
                  reads=['MKall', 'ustrict'], writes=['bk0'])
            S.add('pe', lambda e, c0=c0, w=w: e.matmul(bank(1)[:, 0:w], lhsT=ones_bf, rhs=MKf[:, c0:c0 + w], start=True, stop=True),
                  reads=['MKall', 'ones_bf'], writes=['bk1'])
            S.add('act', lambda e, c0=c0, w=w: e.copy(out=POS.rearrange("p t e -> p (t e)")[:, c0:c0 + w], in_=bank(0)[:, 0:w]), reads=['bk0'], writes=['POS'])
            S.add('dve', lambda e, c0=c0, w=w: e.tensor_copy(out=CNT.rearrange("p t e -> p (t e)")[:, c0:c0 + w], in_=bank(1)[:, 0:w]), reads=['bk1'], writes=['CNT'])
        S.add('pool', lambda e: e.memset(TB[:, 0, :], 0.0), writes=['TB'])
        for tj in range(1, NT):
            S.add('dve', lambda e, tj=tj: e.tensor_tensor(out=TB[:, tj, :], in0=TB[:, tj - 1, :], in1=CNT[:, tj - 1, :], op=ALU.add),
                  reads=['TB', 'CNT'], writes=['TB'])
        S.add('dve', lambda e: e.tensor_tensor(out=TOT, in0=TB[:, NT - 1, :], in1=CNT[:, NT - 1, :], op=ALU.add), reads=['TB', 'CNT'], writes=['TOT'])
        S.add('dve', lambda e: e.tensor_scalar(out=PADi, in0=TOT, scalar1=127.0, scalar2=None, op0=ALU.add), reads=['TOT'], writes=['PADi'])
        S.add('dve', lambda e: e.tensor_scalar(out=PADi, in0=PADi, scalar1=7, scalar2=7, op0=ALU.arith_shift_right, op1=ALU.logical_shift_left),
              reads=['PADi'], writes=['PADi'])
        S.add('dve', lambda e: e.tensor_copy(out=PAD, in_=PADi), reads=['PADi'], writes=['PAD'])
        S.add('pool', lambda e: e.memset(PS_[:, 0:1], 0.0), writes=['PS'])
        for ex in range(1, NE):
            S.add('dve', lambda e, ex=ex: e.tensor_tensor(out=PS_[:, ex:ex + 1], in0=PS_[:, ex - 1:ex], in1=PAD[:, ex - 1:ex], op=ALU.add),
                  reads=['PS', 'PAD'], writes=['PS'])
        S.add('dve', lambda e: e.tensor_tensor(out=PEND, in0=PS_, in1=PAD, op=ALU.add), reads=['PS', 'PAD'], writes=['PEND'])
        S.add('dve', lambda e: e.tensor_tensor(out=POS, in0=POS, in1=TB, op=ALU.add), reads=['POS', 'TB'], writes=['POS'])
        S.add('dve', lambda e: e.tensor_tensor(out=POS, in0=POS, in1=PS_.unsqueeze(1).to_broadcast([128, NT, NE]), op=ALU.add),
              reads=['POS', 'PS'], writes=['POS'])
        for k in range(4):
            S.add('dve', lambda e, k=k: e.tensor_tensor(out=EQ, in0=LG, in1=V8[:, :, k:k + 1].to_broadcast([128, NT, NE]), op=ALU.is_equal),
                  reads=['LGall', 'V8all'], writes=['EQ'])
            S.add('dve', lambda e: e.tensor_tensor(out=EQ, in0=EQ, in1=POS, op=ALU.mult), reads=['EQ', 'POS'], writes=['EQ'])
            S.add('dve', lambda e, k=k: e.tensor_reduce(out=DESTf[:, :, k], in_=EQ, axis=AX.X, op=ALU.add), reads=['EQ'], writes=['DESTf'])
        S.add('dve', lambda e: e.tensor_copy(out=DESTi, in_=DESTf), reads=['DESTf'], writes=['DESTi'])
        S.add('dve', lambda e: e.tensor_tensor(out=G4, in0=V8[:, :, 0:4], in1=V8[:, :, 0:1].to_broadcast([128, NT, 4]), op=ALU.subtract),
              reads=['V8all'], writes=['G4'])
        S.add('act', lambda e: e.activation(out=G4, in_=G4, func=AF.Exp), reads=['G4'], writes=['G4'])
        S.add('dve', lambda e: e.tensor_reduce(out=gs, in_=G4, axis=AX.X, op=ALU.add), reads=['G4'], writes=['gs'])
        S.add('dve', lambda e: e.reciprocal(out=gs, in_=gs), reads=['gs'], writes=['gs'])
        S.add('dve', lambda e: e.tensor_tensor(out=G4, in0=G4, in1=gs.unsqueeze(2).to_broadcast([128, NT, 4]), op=ALU.mult),
              reads=['G4', 'gs'], writes=['G4'])
        S.add('pe', lambda e: e.transpose(out=bank(2)[0:32, 0:128], in_=PEND, identity=C['ident_f']), reads=['PEND', 'ident_f'], writes=['bk2'])
        S.add('dve', lambda e: e.tensor_copy(out=pendc, in_=bank(2)[0:32, 0:1]), reads=['bk2'], writes=['pendc'])
        S.add('dve', lambda e: e.tensor_scalar(out=cmpt, in0=C['blk128'], scalar1=pendc[:, 0:1], scalar2=None, op0=ALU.is_ge),
              reads=['pendc', 'blk128'], writes=['cmpt'])
        S.add('pe', lambda e: e.matmul(bank(3)[:, 0:NB], lhsT=ones_bf[0:32, :], rhs=cmpt, start=True, stop=True), reads=['cmpt', 'ones_bf'], writes=['bk3'])
        S.add('dve', lambda e: e.tensor_scalar(out=EB, in0=bank(3)[:, 0:NB], scalar1=float(NE - 1), scalar2=None, op0=ALU.min), reads=['bk3'], writes=['EB'])
        S.add('dve', lambda e: e.tensor_scalar(out=IDXE, in0=EB[0:2, :], scalar1=float(l * NE), scalar2=None, op0=ALU.add), reads=['EB'], writes=['IDXE'])
        S.add('dve', lambda e: e.tensor_scalar(out=EB2, in0=EB, scalar1=128.0, scalar2=C['pcol'][:, 0:1], op0=ALU.mult, op1=ALU.add),
              reads=['EB', 'pcol'], writes=['EB2'])
        S.add('dve', lambda e: e.tensor_tensor(out=EQ.rearrange("p t e -> p (t e)")[:, 0:NB - 2], in0=EB[:, 2:NB], in1=EB[:, 0:NB - 2], op=ALU.is_equal),
              reads=['EB', 'EQ'], writes=['EQ'])
        S.add('dve', lambda e: e.scalar_tensor_tensor(out=EB2[:, 2:NB], in0=EQ.rearrange("p t e -> p (t e)")[:, 0:NB - 2], scalar=float(OOB),
                                                      in1=EB2[:, 2:NB], op0=ALU.mult, op1=ALU.add), reads=['EQ', 'EB2'], writes=['EB2'])
        S.add('dve', lambda e: e.tensor_scalar(out=IDXW, in0=EB2, scalar1=float(l * NE * 128), scalar2=None, op0=ALU.add), reads=['EB2'], writes=['IDXW'])
        if 'moe_dbg' in dbg:
            S.barrier()
            d_lg = nc.dram_tensor('d_lg', [128, NT, NE], F32, kind="ExternalOutput").ap()
            d_v8 = nc.dram_tensor('d_v8', [128, NT, 8], F32, kind="ExternalOutput").ap()
            d_dest = nc.dram_tensor('d_dest', [128, NT, 4], I32, kind="ExternalOutput").ap()
            d_g4 = nc.dram_tensor('d_g4', [128, NT, 4], F32, kind="ExternalOutput").ap()
            d_eb = nc.dram_tensor('d_eb', [128, NB], F32, kind="ExternalOutput").ap()
            d_idxw = nc.dram_tensor('d_idxw', [128, NB], I32, kind="ExternalOutput").ap()
            d_pend = nc.dram_tensor('d_pend', [128, NE], F32, kind="ExternalOutput").ap()
            for dd, tt_ in ((d_lg, LG), (d_v8, V8), (d_dest, DESTi), (d_g4, G4), (d_eb, EB), (d_idxw, IDXW), (d_pend, PEND)):
                S.add('sp', lambda e, dd=dd, tt_=tt_: [e.dma_start(out=dd, in_=tt_)], writes=[('dbgout', id(dd))], ndma=1)
        S.barrier()
        A.reset(MK5)
        hs = [A.tile([128, D], BF16, 'hs') for _ in range(3)]
        for tj in range(NT):
            b = tj % 3
            r0 = tj * 128
            S.add('sp', lambda e, b=b, r0=r0: [e.dma_start(out=hs[b], in_=h2_tm[r0:r0 + 128, :])], writes=[('hs', b)], ndma=1)
            for k in range(4):
                S.add('pool', lambda e, b=b, tj=tj, k=k: [e.indirect_dma_start(
                    out=Xs, out_offset=bass.IndirectOffsetOnAxis(ap=DESTi[:, tj, k:k + 1], axis=0), in_=hs[b], in_offset=None)],
                    reads=[('hs', b)], writes=[('Xs', tj, k)], ndma=1)
        S.barrier()
        A.reset(MK5)
        WG = [A.tile([128, 8, 2048], BF16, 'WG') for _ in range(2)]
        WD = [A.tile([128, 8, 1024], BF16, 'WD') for _ in range(2)]
        BG = [A.tile([2, 2048], BF16, 'BG') for _ in range(2)]
        BD = [A.tile([2, 1024], BF16, 'BD') for _ in range(2)]
        xb_ = [A.tile([128, D], BF16, 'xb') for _ in range(2)]
        xT_ = [A.tile([128, 8, 128], BF16, 'xT') for _ in range(2)]
        am = [A.tile([128, 512], F32, 'am') for _ in range(2)]
        sg = [A.tile([128, 512], F32, 'sg')] * 2
        uc = [A.tile([128, 512], F32, 'uc') for _ in range(2)]
        yb = [A.tile([128, D], BF16, 'yb') for _ in range(2)]
        yT_ = A.tile([128, 8, 128], BF16, 'yT8')
        ob_ = [A.tile([128, D], F32, 'ob') for _ in range(2)]
        wgu_rows = wgu.rearrange("l r c -> (l r) c")
        wdn_rows = wdn.rearrange("l r c -> (l r) c")
        bgu_rows = bgu.rearrange("l r c -> (l r) c")
        bdn_rows = bdn.rearrange("l r c -> (l r) c")

        def bcreg(e):
            if 'r' not in BCREG:
                BCREG['r'] = e.alloc_register('bcreg')
                e.reg_mov(BCREG['r'], DEPTH * NE * 128 - 1)
            return BCREG['r']
        def stage_a(bi):
            b = bi % 2
            S.add('pool', lambda e, b=b, bi=bi: [e.indirect_dma_start(
                out=WG[b].rearrange("p k n -> p (k n)"), out_offset=None, in_=wgu_rows,
                in_offset=bass.IndirectOffsetOnAxis(ap=IDXW[:, bi:bi + 1], axis=0), bounds_check=bcreg(e), oob_is_err=False)],
                writes=[('WG', b)], ndma=1)
            S.add('pool', lambda e, b=b, bi=bi: [e.indirect_dma_start(
                out=WD[b].rearrange("p k n -> p (k n)"), out_offset=None, in_=wdn_rows,
                in_offset=bass.IndirectOffsetOnAxis(ap=IDXW[:, bi:bi + 1], axis=0), bounds_check=bcreg(e), oob_is_err=False)],
                writes=[('WD', b)], ndma=1)
            S.add('pool', lambda e, b=b, bi=bi: [e.indirect_dma_start(
                out=BG[b], out_offset=None, in_=bgu_rows, in_offset=bass.IndirectOffsetOnAxis(ap=IDXE[0:2, bi:bi + 1], axis=0))],
                writes=[('BG', b)], ndma=1)
            S.add('pool', lambda e, b=b, bi=bi: [e.indirect_dma_start(
                out=BD[b], out_offset=None, in_=bdn_rows, in_offset=bass.IndirectOffsetOnAxis(ap=IDXE[0:2, bi:bi + 1], axis=0))],
                writes=[('BD', b)], ndma=1)
            S.add('sp', lambda e, b=b, bi=bi: [e.dma_start(out=xb_[b], in_=Xs[bi * 128:(bi + 1) * 128, :])], writes=[('xb', b)], ndma=1)
            pb = pbf[0]
            for kc in range(8):
                S.add('pe', lambda e, b=b, kc=kc, pb=pb: e.transpose(out=pb[:, kc * 128:(kc + 1) * 128], in_=xb_[b][:, kc * 128:(kc + 1) * 128],
                                                                    identity=C['ident_bf']), reads=[('xb', b), 'ident_bf'], writes=[('pbf', 0)])
            S.add('act', lambda e, b=b, pb=pb: e.copy(out=xT_[b].rearrange("p k t -> p (k t)"), in_=pb), reads=[('pbf', 0)], writes=[('xT', b)])
            for hf in range(2):
                for n in (hf, 2 + hf):
                    bk = bank(n)
                    BKK = ('bk', n)
                    for kc in range(8):
                        S.add('pe', lambda e, b=b, kc=kc, n=n, bk=bk: e.matmul(bk, lhsT=xT_[b][:, kc, :], rhs=WG[b][:, kc, n * 512:(n + 1) * 512],
                                                                              start=(kc == 0), stop=False), reads=[('xT', b), ('WG', b)], writes=[BKK])
                    S.add('pe', lambda e, b=b, n=n, bk=bk: e.matmul(bk, lhsT=ones_bf[0:1, :], rhs=BG[b][0:1, n * 512:(n + 1) * 512], start=False, stop=True),
                          reads=['ones_bf', ('BG', b)], writes=[BKK])
                ab = bank(hf)
                ub = bank(2 + hf)
                a_ = am[hf]
                s_ = sg[hf]
                u_ = uc[hf]
                S.add('act', lambda e, ab=ab, a_=a_: e.copy(out=a_, in_=ab), reads=[('bk', hf)], writes=[('am', hf)])
                S.add('act', lambda e, ub=ub, u_=u_: e.activation(out=u_, in_=ub, func=AF.Identity, bias=onec[:, 0:1]), reads=[('bk', 2 + hf), 'onec'], writes=[('uc', hf)])
                S.add('dve', lambda e, a_=a_: e.tensor_scalar(out=a_, in0=a_, scalar1=7.0, scalar2=None, op0=ALU.min), reads=[('am', hf)], writes=[('am', hf)])
                S.add('act', lambda e, a_=a_, s_=s_: e.activation(out=s_, in_=a_, func=AF.Sigmoid, scale=1.702), reads=[('am', hf)], writes=[('sg', 0)])
                S.add('dve', lambda e, u_=u_: e.tensor_scalar(out=u_, in0=u_, scalar1=-6.0, scalar2=8.0, op0=ALU.max, op1=ALU.min),
                      reads=[('uc', hf)], writes=[('uc', hf)])
                S.add('pool', lambda e, a_=a_, s_=s_: e.tensor_tensor(out=a_, in0=a_, in1=s_, op=ALU.mult), reads=[('am', hf), ('sg', 0)], writes=[('am', hf)])
                S.add('pool', lambda e, hf=hf, b=b, a_=a_, u_=u_: e.tensor_tensor(out=yb[b][:, hf * 512:(hf + 1) * 512], in0=u_, in1=a_, op=ALU.mult),
                      reads=[('uc', hf), ('am', hf)], writes=[('yb', b, hf)])

        def stage_b(bi):
            b = bi % 2
            pb = pbf[1]
            for kc in range(8):
                S.add('pe', lambda e, kc=kc, pb=pb, b=b: e.transpose(out=pb[:, kc * 128:(kc + 1) * 128], in_=yb[b][:, kc * 128:(kc + 1) * 128], identity=C['ident_bf']),
                      reads=[('yb', b, kc // 4), 'ident_bf'], writes=[('pbf', 1)])
            S.add('dve', lambda e, pb=pb: e.tensor_copy(out=yT_.rearrange("p k t -> p (k t)"), in_=pb), reads=[('pbf', 1)], writes=['yT8'])
            for n in range(2):
                bk = bank(4 + n)
                BKK = ('bk', 4 + n)
                for kc in range(8):
                    S.add('pe', lambda e, b=b, kc=kc, n=n, bk=bk: e.matmul(bk, lhsT=yT_[:, kc, :], rhs=WD[b][:, kc, n * 512:(n + 1) * 512],
                                                                          start=(kc == 0), stop=False), reads=['yT8', ('WD', b)], writes=[BKK])
                S.add('pe', lambda e, b=b, n=n, bk=bk: e.matmul(bk, lhsT=ones_bf[0:1, :], rhs=BD[b][0:1, n * 512:(n + 1) * 512], start=False, stop=True),
                      reads=['ones_bf', ('BD', b)], writes=[BKK])
                if n == 0:
                    S.add('act', lambda e, b=b, bk=bk: e.copy(out=ob_[b][:, 0:512], in_=bk), reads=[BKK], writes=[('ob', b, 0)])
                else:
                    S.add('dve', lambda e, b=b, bk=bk: e.tensor_copy(out=ob_[b][:, 512:1024], in_=bk), reads=[BKK], writes=[('ob', b, 1)])
            S.add('act', lambda e, b=b, bi=bi: [e.dma_start(out=Ys[bi * 128:(bi + 1) * 128, :], in_=ob_[b])],
                  reads=[('ob', b, 0), ('ob', b, 1)], writes=[('Ys', bi)], ndma=1)

        stage_a(0)
        for bi in range(NB):
            if bi + 1 < NB:
                stage_a(bi + 1)
            stage_b(bi)
        S.barrier()
        A.reset(MK5)
        Yk = [[A.tile([128, D], F32, 'Yk') for _ in range(4)] for _ in range(2)]
        xt_p9 = [A.tile([128, D], F32, 'xt9') for _ in range(2)]
        acc = [A.tile([128, D], F32, 'acc') for _ in range(2)]
        for tj in range(NT):
            b = tj % 2
            r0 = tj * 128
            isx = tj < NTX
            for k in range(4):
                S.add('pool', lambda e, b=b, tj=tj, k=k: [e.indirect_dma_start(
                    out=Yk[b][k], out_offset=None, in_=Ys, in_offset=bass.IndirectOffsetOnAxis(ap=DESTi[:, tj, k:k + 1], axis=0))],
                    writes=[('Yk', b, k)], ndma=1)
            S.add('sp', lambda e, b=b, r0=r0: [e.dma_start(out=xt_p9[b], in_=xres[r0:r0 + 128, :])], writes=[('xt9', b)], ndma=1)
            S.add('dve', lambda e, b=b, tj=tj: e.tensor_scalar(out=acc[b], in0=Yk[b][0], scalar1=G4[:, tj, 0:1], scalar2=None, op0=ALU.mult),
                  reads=[('Yk', b, 0)], writes=[('acc', b)])
            for k in range(1, 4):
                S.add('dve', lambda e, b=b, tj=tj, k=k: e.scalar_tensor_tensor(out=acc[b], in0=Yk[b][k], scalar=G4[:, tj, k:k + 1], in1=acc[b],
                                                                              op0=ALU.mult, op1=ALU.add), reads=[('Yk', b, k), ('acc', b)], writes=[('acc', b)])
            gb = BT['g2x'] if isx else BT['g2c']
            S.add('pool', lambda e, b=b, gb=gb: e.tensor_tensor(out=acc[b], in0=acc[b], in1=gb, op=ALU.mult), reads=[('acc', b), ('btile', id(gb))], writes=[('acc', b)])
            S.add('pool', lambda e, b=b: e.tensor_tensor(out=acc[b], in0=acc[b], in1=xt_p9[b], op=ALU.add), reads=[('acc', b), ('xt9', b)], writes=[('acc', b)])
            S.add('act', lambda e, b=b, r0=r0: [e.dma_start(out=xres[r0:r0 + 128, :], in_=acc[b])], reads=[('acc', b)], writes=[('xres', r0)], ndma=1)
        S.barrier()

    for _l in range(nl):
        _layer(_l)

    S.barrier()
    A.reset(PERSIST)
    fcol = A.tile([128, 8], F32, 'fcol')
    fb_pf = A.tile([128, D], F32, 'fb')
    S.add('sp', lambda e: [e.dma_start(out=fcol, in_=fng)], writes=['fcol'], ndma=1)
    if final_norm:
        bcast_tile(fb_pf, fcol, None, 'fcol')
    xt_pf = [A.tile([128, D], F32, 'xtf') for _ in range(2)]
    xo_pf = [A.tile([128, D], F32, 'xof') for _ in range(2)]
    junk_pf = A.tile([128, D], BF16, 'junkf')
    ss_pf = [A.tile([128, 1], F32, 'ssf') for _ in range(2)]
    for tj in range(NTX):
        b = tj % 2
        r0 = tj * 128
        S.add(dmaq(), lambda e, b=b, r0=r0: [e.dma_start(out=xt_pf[b], in_=xres[r0:r0 + 128, :])], writes=[('xtf', b)], ndma=1)
        if final_norm:
            S.add('act', lambda e, b=b: e.activation(out=junk_pf, in_=xt_pf[b], func=AF.Square, accum_out=ss_pf[b]), reads=[('xtf', b)], writes=[('ssf', b), 'junkf'])
            S.add('dve', lambda e, b=b: e.tensor_scalar(out=ss_pf[b], in0=ss_pf[b], scalar1=1.0 / D, scalar2=EPS, op0=ALU.mult, op1=ALU.add),
                  reads=[('ssf', b)], writes=[('ssf', b)])
            S.add('act', lambda e, b=b: e.sqrt(out=ss_pf[b], in_=ss_pf[b]), reads=[('ssf', b)], writes=[('ssf', b)])
            S.add('dve', lambda e, b=b: e.reciprocal(out=ss_pf[b], in_=ss_pf[b]), reads=[('ssf', b)], writes=[('ssf', b)])
            S.add('dve', lambda e, b=b: e.scalar_tensor_tensor(out=xo_pf[b], in0=xt_pf[b], scalar=ss_pf[b][:, 0:1], in1=fb_pf, op0=ALU.mult, op1=ALU.mult),
                  reads=[('xtf', b), ('ssf', b), ('btile', id(fb_pf))], writes=[('xof', b)])
        else:
            S.add('dve', lambda e, b=b: e.tensor_copy(out=xo_pf[b], in_=xt_pf[b]), reads=[('xtf', b)], writes=[('xof', b)])
        S.add(dmaq(), lambda e, b=b, r0=r0: [e.dma_start(out=yout[r0:r0 + 128, :], in_=xo_pf[b])], reads=[('xof', b)], writes=[('yout', tj)], ndma=1)
    S.barrier()
    S.emit()
    return nc, consts


def prep_shared(inp):
    f32 = lambda a: np.ascontiguousarray(np.asarray(a, dtype=np.float32))
    sh = {}
    sh['ada_w'] = f32(inp['ada_w'])
    sh['ada_bT'] = f32(np.asarray(inp['ada_b']).reshape(DEPTH, 48, 128).transpose(0, 2, 1))
    sh['n1g'] = f32(np.asarray(inp['norm1_g']).reshape(DEPTH, 8, 128).transpose(0, 2, 1))
    sh['n2g'] = f32(np.asarray(inp['norm2_g']).reshape(DEPTH, 8, 128).transpose(0, 2, 1))
    sh['fng'] = f32(np.asarray(inp['final_norm_g']).reshape(8, 128).T)
    sh['w_in'] = f32(inp['w_in'])
    sh['sink'] = f32(inp['attn_sink'])
    sh['w_fo'] = f32(inp['w_fourier_out'])
    sh['w_ao'] = f32(inp['w_attn_out'])
    sh['w_out'] = f32(inp['w_out'])
    sh['r_w'] = f32(np.asarray(inp['router_w']).reshape(DEPTH, 8, 128, NE).transpose(0, 2, 1, 3))
    sh['r_b'] = f32(np.asarray(inp['router_b']).reshape(DEPTH, 1, NE))
    sh['wgu'] = f32(np.asarray(inp['expert_w_gu']).reshape(DEPTH, NE, 8, 128, 2048).transpose(0, 1, 3, 2, 4).reshape(DEPTH, NE * 128, 8 * 2048))
    sh['wdn'] = f32(np.asarray(inp['expert_w_down']).reshape(DEPTH, NE, 8, 128, 1024).transpose(0, 1, 3, 2, 4).reshape(DEPTH, NE * 128, 8 * 1024))
    sh['bgu'] = f32(inp['expert_b_gu'])
    sh['bdn'] = f32(inp['expert_b_down'])
    return sh


def core_inputs(inp, sh, consts, b):
    m = dict(sh)
    m['xin'] = np.ascontiguousarray(np.asarray(inp['x'][b], dtype=np.float32))
    m['ctxin'] = np.ascontiguousarray(np.asarray(inp['ctx'][b], dtype=np.float32))
    cc = np.zeros((128, 8, 2), np.float32)
    cc[:, :, 0] = np.asarray(inp['c'][b]).reshape(8, 128).T
    cc[:, :, 1] = np.asarray(inp['c_ctx']).reshape(8, 128).T
    m['ccol'] = cc
    for k, v in consts.items():
        m['c_' + k] = v
    return m


def kernel(**inputs):
    nc, consts = build()
    sh = prep_shared(inputs)
    nb = inputs['x'].shape[0]
    in_maps = [core_inputs(inputs, sh, consts, i) for i in range(nb)]
    res = run_bass_kernel_spmd(nc, in_maps, core_ids=list(range(nb)))
    out = np.stack([np.asarray(res.results[i]['yout'], dtype=np.float32) for i in range(nb)], axis=0)
    return out
```

```python
import numpy as np
import ml_dtypes
import concourse.bass as bass
import concourse.mybir as mybir
from concourse.bass_utils import run_bass_kernel_spmd

F32 = mybir.dt.float32
BF16 = mybir.dt.bfloat16
I32 = mybir.dt.int32
ALU = mybir.AluOpType
AF = mybir.ActivationFunctionType
AX = mybir.AxisListType

D = 1024
TX = 8192
TC = 256
T = TX + TC
NT = T // 128
NTX = TX // 128
DEPTH = 4
NE = 32
NB = (T * 4 + NE * 127) // 128 + 1
NSLOT = NB * 128
EPS = 1e-5
OOB = 1 << 28
SAME_ENGINE_INORDER = False


class Sched:
    ENGS = ['pe', 'act', 'dve', 'pool', 'sp']
    EPOCH = 12000
    NPOOL = 6

    def __init__(self, nc):
        self.nc = nc
        self.ops = []
        self.last_w = {}
        self.readers = {}
        self.sig = []
        self.nsem = 0
        self.esem = {e: self._new_sem('e_' + e) for e in self.ENGS}
        self.ecnt = {e: 0 for e in self.ENGS}
        self.dpool = {e: [[self._new_sem('d_%s%d' % (e, i)), 0] for i in range(self.NPOOL)]
                      for e in self.ENGS}
        self.dnext = {e: 0 for e in self.ENGS}
        self.known = {e: {} for e in self.ENGS}

    def _new_sem(self, name):
        self.nsem += 1
        return self.nc.alloc_semaphore(name='%s_%d' % (name, self.nsem))

    def _prune(self, eng, waits):
        kn = self.known[eng]
        wl = []
        for key, (s, v) in waits.items():
            if kn.get(key, 0) >= v:
                continue
            kn[key] = v
            wl.append((s, v))
        return wl

    def add(self, eng, fn, reads=(), writes=(), ndma=0):
        opid = len(self.ops)
        deps = set()
        for k in list(reads) + list(writes):
            if k in self.last_w:
                deps.add(self.last_w[k])
        for k in writes:
            for r in self.readers.get(k, ()):
                deps.add(r)
        waits = {}
        for d in deps:
            deng = self.ops[d]['eng']
            if deng == 'pe' and eng == 'pe' and not self.ops[d]['ndma'] and not ndma:
                continue
            if SAME_ENGINE_INORDER and deng == eng and deng in ('act', 'dve', 'pool') and not self.ops[d]['ndma'] and not ndma:
                continue
            s, v = self.sig[d]
            key = id(s)
            if key not in waits or waits[key][1] < v:
                waits[key] = (s, v)
        if ndma:
            pool = self.dpool[eng]
            slot = pool[self.dnext[eng] % self.NPOOL]
            self.dnext[eng] += 1
            if slot[1] > 0:
                key = id(slot[0])
                if key not in waits or waits[key][1] < slot[1]:
                    waits[key] = (slot[0], slot[1])
            if slot[1] + 16 * ndma > self.EPOCH * 2:
                slot[0] = self._new_sem('d_' + eng)
                slot[1] = 0
            slot[1] += 16 * ndma
            sig = (slot[0], slot[1])
            inc = (slot[0], 16)
        else:
            if self.ecnt[eng] >= self.EPOCH:
                self.esem[eng] = self._new_sem('e_' + eng)
                self.ecnt[eng] = 0
            self.ecnt[eng] += 1
            sig = (self.esem[eng], self.ecnt[eng])
            inc = (self.esem[eng], 1)
        self.ops.append(dict(eng=eng, fn=fn, waits=self._prune(eng, waits), inc=inc, ndma=ndma))
        self.sig.append(sig)
        for k in reads:
            self.readers.setdefault(k, []).append(opid)
        for k in writes:
            self.last_w[k] = opid
            self.readers[k] = []
        return opid

    def barrier(self):
        sigs = {}
        for e in self.ENGS:
            if self.ecnt[e] > 0:
                sigs[id(self.esem[e])] = (self.esem[e], self.ecnt[e])
            for slot in self.dpool[e]:
                if slot[1] > 0:
                    sigs[id(slot[0])] = (slot[0], slot[1])
        for e in self.ENGS:
            self.ops.append(dict(eng=e, fn=None, waits=self._prune(e, dict(sigs)), inc=None, ndma=0))
            self.sig.append(None)
        self.last_w = {}
        self.readers = {}

    def emit(self):
        nc = self.nc
        with nc.Block() as block:
            def run(engname):
                def body(e):
                    for op in self.ops:
                        if op['eng'] != engname:
                            continue
                        for s, v in op['waits']:
                            e.wait_ge(s, v)
                        if op['fn'] is None:
                            continue
                        try:
                            r = op['fn'](e)
                        except Exception:
                            print('EMIT FAIL', engname, 'op#', self.ops.index(op), 'engine-op-count', self.ecnt, flush=True)
                            raise
                        if not isinstance(r, (list, tuple)):
                            r = [r]
                        if op['ndma']:
                            assert len(r) == op['ndma'], (len(r), op['ndma'])
                        else:
                            assert len(r) == 1
                        for ins in r:
                            ins.then_inc(op['inc'][0], op['inc'][1])
                return body
            block.tensor(run('pe'))
            block.scalar(run('act'))
            block.vector(run('dve'))
            block.gpsimd(run('pool'))
            block.sync(run('sp'))


_DTS = {F32: 4, BF16: 2, I32: 4}


class Arena:
    def __init__(self, nc, limit=229376):
        self.nc = nc
        self.off = 20480
        self.n = 0
        self.limit = limit

    def tile(self, shape, dtype, name='t'):
        nb = _DTS[dtype]
        for s in shape[1:]:
            nb *= s
        nb = (nb + 63) // 64 * 64
        self.n += 1
        h = self.nc.alloc_sbuf_tensor_at('%s_%d' % (name, self.n), list(shape), dtype, offset=self.off)
        self.off += nb
        assert self.off <= self.limit, ('SBUF overflow', name, self.off)
        return h.ap()

    def mark(self):
        return self.off

    def reset(self, m):
        self.off = m


def host_consts():
    c = {}
    c['ident_bf'] = np.eye(128, dtype=np.float32).astype(ml_dtypes.bfloat16)
    c['ident_f'] = np.eye(128, dtype=np.float32)
    k = np.arange(128)[:, None]
    q = np.arange(128)[None, :]
    c['mask_prev'] = (k >= q).astype(np.float32).astype(ml_dtypes.bfloat16)
    c['mask_next'] = (k <= q).astype(np.float32).astype(ml_dtypes.bfloat16)
    c['ustrict'] = (k < q).astype(np.float32).astype(ml_dtypes.bfloat16)
    n = np.arange(TX)
    row = (n // 64).astype(np.float64)
    col = (n % 64).astype(np.float64)
    inv = 10000.0 ** (-np.arange(16, dtype=np.float64) / 16)
    cosT = np.zeros((64, TX), np.float64)
    sinT = np.zeros((64, TX), np.float64)
    for ax, pos in enumerate((row, col)):
        ang = (pos[None, :].astype(np.float32) * inv[:, None].astype(np.float32)).astype(np.float32)
        for pr in range(2):
            cosT[ax * 32 + pr * 16: ax * 32 + pr * 16 + 16] = np.cos(ang)
            sinT[ax * 32 + pr * 16: ax * 32 + pr * 16 + 16] = np.sin(ang)
    c['cosT'] = cosT.astype(np.float32)
    c['sinT'] = sinT.astype(np.float32)
    rm = np.zeros((128, 128), np.float32)
    for ax in range(2):
        for f in range(16):
            d0 = ax * 32 + f
            d1 = ax * 32 + 16 + f
            rm[d1, d0] = -1.0
            rm[d0, d1] = 1.0
    c['rotm'] = rm.astype(ml_dtypes.bfloat16)
    a = np.arange(128, dtype=np.float64)
    th = 2 * np.pi * np.outer(a, a) / 128
    c['c128'] = np.cos(th).astype(np.float32).astype(ml_dtypes.bfloat16)
    c['s128'] = np.sin(th).astype(np.float32).astype(ml_dtypes.bfloat16)
    c['cc'] = np.cos(th).astype(np.float32).astype(ml_dtypes.bfloat16)
    c['nsc'] = (-np.sin(th)).astype(np.float32).astype(ml_dtypes.bfloat16)
    n2 = np.arange(64, dtype=np.float64)
    tw = 2 * np.pi * np.outer(a, n2) / 8192
    c['twr'] = np.cos(tw).astype(np.float32)
    c['twi'] = np.sin(tw).astype(np.float32)
    c['ntwi'] = (-np.sin(tw)).astype(np.float32)
    th64 = 2 * np.pi * np.outer(n2, n2) / 64
    c64 = np.cos(th64) / 1024.0
    s64 = np.sin(th64) / 1024.0
    r2 = np.zeros((128, 128), np.float64)
    r2[0:64, 0:64] = c64
    r2[64:128, 0:64] = -s64
    r2[0:64, 64:128] = s64
    r2[64:128, 64:128] = c64
    c['r2'] = r2.astype(np.float32).astype(ml_dtypes.bfloat16)
    b = np.arange(256, dtype=np.float64)
    th256 = 2 * np.pi * np.outer(b, b) / 256
    sc = 1.0 / np.sqrt(256.0 * 128.0)
    c['c256'] = (np.cos(th256) * sc).astype(np.float32).astype(ml_dtypes.bfloat16).reshape(2, 128, 256).transpose(1, 0, 2).copy()
    c['s256'] = (np.sin(th256) * sc).astype(np.float32).astype(ml_dtypes.bfloat16).reshape(2, 128, 256).transpose(1, 0, 2).copy()
    c['blk128'] = np.broadcast_to((np.arange(NB, dtype=np.float32) * 128.0)[None, :], (32, NB)).copy()
    c['pcol'] = np.arange(128, dtype=np.float32).reshape(128, 1)
    c['u32'] = (np.arange(32)[:, None] < np.arange(32)[None, :]).astype(np.float32)
    return c


CONST_SPECS = None


def build(nl=DEPTH, final_norm=True, dbg=()):
    nc = bass.Bass("TRN2", target_bir_lowering=False)
    consts = host_consts()

    def din(name, shape, dt=F32):
        return nc.dram_tensor(name, list(shape), dt, kind="ExternalInput").ap()

    def dscr(name, shape, dt):
        kind = "ExternalOutput" if name in dbg else "Internal"
        return nc.dram_tensor(name, list(shape), dt, kind=kind).ap()

    xin = din('xin', [TX, D])
    ctxin = din('ctxin', [TC, D])
    ccol = din('ccol', [128, 8, 2])
    ada_w = din('ada_w', [DEPTH, D, 6 * D])
    ada_bT = din('ada_bT', [DEPTH, 128, 48])
    n1g = din('n1g', [DEPTH, 128, 8])
    n2g = din('n2g', [DEPTH, 128, 8])
    fng = din('fng', [128, 8])
    w_in = din('w_in', [DEPTH, D, 3328])
    sink = din('sink', [DEPTH, 8])
    w_fo = din('w_fo', [DEPTH, 512, D])
    w_ao = din('w_ao', [DEPTH, 512, D])
    w_out = din('w_out', [DEPTH, D, D])
    r_w = din('r_w', [DEPTH, 128, 8, NE])
    r_b = din('r_b', [DEPTH, 1, NE])
    wgu = din('wgu', [DEPTH, NE * 128, 8 * 2048])
    wdn = din('wdn', [DEPTH, NE * 128, 8 * 1024])
    bgu = din('bgu', [DEPTH, NE, 2048])
    bdn = din('bdn', [DEPTH, NE, 1024])
    cd = {}
    for k, v in consts.items():
        dt = BF16 if v.dtype == ml_dtypes.bfloat16 else F32
        cd[k] = din('c_' + k, v.shape, dt)
    yout = nc.dram_tensor('yout', [TX, D], F32, kind="ExternalOutput").ap()

    xres = dscr('xres', [T, D], F32)
    f_tm = dscr('f_tm', [T, 512], BF16)
    q_blk = dscr('q_blk', [NT, 64, 8, 128], BF16)
    kT_all = dscr('kT_all', [64, 2, T], BF16)
    v_blk = dscr('v_blk', [128, NT, 128], BF16)
    g_blk = dscr('g_blk', [NT, 128, 16, 128], BF16)
    t2_blk = dscr('t2_blk', [NT, 128, 8, 128], BF16)
    Zd = dscr('Zd', [2, 128, 64, 512], BF16)
    h2_tm = dscr('h2_tm', [T, D], BF16)
    Xs = dscr('Xs', [NSLOT, D], BF16)
    Ys = dscr('Ys', [NSLOT, D], F32)

    S = Sched(nc)
    A = Arena(nc)
    BCREG = {}
    pq = [nc.alloc_psum_tensor('pq%d' % i, [128, 1024], F32).ap() for i in range(3)]
    pbf = [nc.alloc_psum_tensor('pbf%d' % i, [128, 1024], BF16).ap() for i in range(2)]

    def bank(i):
        return pq[i // 2][:, (i % 2) * 512:(i % 2) * 512 + 512]

    C = {}
    qi = [0]

    def dmaq():
        qi[0] += 1
        return 'sp' if qi[0] % 2 else 'act'

    def load_const(name, shape, dt):
        t = A.tile(shape, dt, name)
        S.add('sp', lambda e: [e.dma_start(out=t, in_=cd[name])], writes=[name], ndma=1)
        C[name] = t
        return t

    for nm in ('ident_bf', 'mask_prev', 'mask_next', 'ustrict', 'c128', 's128', 'cc', 'nsc', 'r2'):
        load_const(nm, [128, 128], BF16)
    load_const('ident_f', [128, 128], F32)
    load_const('rotm', [128, 128], BF16)
    load_const('twr', [128, 64], F32)
    load_const('twi', [128, 64], F32)
    load_const('ntwi', [128, 64], F32)
    load_const('c256', [128, 2, 256], BF16)
    load_const('s256', [128, 2, 256], BF16)
    load_const('blk128', [32, NB], F32)
    load_const('pcol', [128, 1], F32)
    load_const('u32', [32, 32], F32)
    ones_bf = A.tile([128, 128], BF16, 'ones_bf')
    S.add('pool', lambda e: e.memset(ones_bf, 1.0), writes=['ones_bf'])
    ones_f = A.tile([128, 128], F32, 'ones_f')
    S.add('pool', lambda e: e.memset(ones_f, 1.0), writes=['ones_f'])
    onec = A.tile([128, 1], F32, 'onec')
    S.add('pool', lambda e: e.memset(onec, 1.0), writes=['onec'])
    epsc = A.tile([128, 1], F32, 'epsc')
    S.add('pool', lambda e: e.memset(epsc, EPS), writes=['epsc'])
    scc = A.tile([128, 8, 2], F32, 'scc')
    S.add('sp', lambda e: [e.dma_start(out=scc, in_=ccol)], writes=['scc'], ndma=1)
    S.add('act', lambda e: e.activation(out=scc, in_=scc, func=AF.Silu), reads=['scc'], writes=['scc'])
    modT = A.tile([128, 48, 2], F32, 'modT')
    s1 = A.tile([128, 8, 2], F32, 's1')
    s2 = A.tile([128, 8, 2], F32, 's2')
    gcol = A.tile([128, 8], F32, 'gcol')
    BT = {k: A.tile([128, D], F32, 'bt_' + k) for k in
          ('g1x', 'g1c', 'g2x', 'g2c', 's2x', 's2c', 'h2x', 'h2c')}
    rwp = A.tile([128, 8, NE], F32, 'rwp')
    rwx = A.tile([128, 8, 2, NE], F32, 'rwx')
    rcst = A.tile([1, 2, NE], F32, 'rcst')
    rbt = A.tile([1, NE], F32, 'rbt')
    PERSIST = A.mark()

    for ci in range(16):
        S.add(dmaq(), lambda e, ci=ci: [e.dma_start(out=xres[ci * 512:(ci + 1) * 512, :], in_=xin[ci * 512:(ci + 1) * 512, :])],
              writes=[('xres0', ci)], ndma=1)
    S.add('act', lambda e: [e.dma_start(out=xres[TX:T, :], in_=ctxin)], writes=['xres2'], ndma=1)
    _mz = A.mark()
    zt = A.tile([128, 8 * D], BF16, 'zt')
    A.reset(_mz)
    S.add('pool', lambda e: e.memset(zt, 0.0), writes=['zt'])
    Xz = Xs.rearrange("(p r) d -> p (r d)", p=128)
    for c0 in range(0, NB * D, 8 * D):
        w_ = min(8 * D, NB * D - c0)
        S.add(dmaq(), lambda e, c0=c0, w_=w_: [e.dma_start(out=Xz[:, c0:c0 + w_], in_=zt[:, 0:w_])], reads=['zt'], writes=[('Xz', c0)], ndma=1)
    S.barrier()

    def bcast_tile(dst, colsrc, j, key):
        for kc in range(8):
            src = colsrc[:, kc, j:j + 1] if j is not None else colsrc[:, kc:kc + 1]
            tmp = BCT[kc % 2]
            S.add('dve', lambda e, src=src, tmp=tmp: e.tensor_copy(out=tmp, in_=src.to_broadcast([128, 128])),
                  reads=[key], writes=[('bct', kc % 2)])
            pb = bank(kc % 2)
            S.add('pe', lambda e, tmp=tmp, pb=pb: e.matmul(pb[:, 0:128], lhsT=tmp, rhs=C['ident_f'], start=True, stop=True),
                  reads=[('bct', kc % 2)], writes=[('bk', kc % 2)])
            S.add('act', lambda e, pb=pb, kc=kc: e.copy(out=dst[:, kc * 128:(kc + 1) * 128], in_=pb[:, 0:128]),
                  reads=[('bk', kc % 2)], writes=[('btile', id(dst))])

    BCT = [A.tile([128, 128], F32, 'bct') for _ in range(2)]
    PERSIST = A.mark()

    def _layer(l):
        A.reset(PERSIST)
        aw = [A.tile([128, 8, 768], F32, 'aw') for _ in range(2)]
        adb = A.tile([128, 48], F32, 'adb')
        S.add('act', lambda e: [e.dma_start(out=adb, in_=ada_bT[l])], writes=['adb'], ndma=1)
        for cch in range(8):
            buf = aw[cch % 2]
            S.add(dmaq(), lambda e, buf=buf, cch=cch: [e.dma_start(
                out=buf, in_=ada_w[l, :, cch * 768:(cch + 1) * 768].rearrange("(kc p) n -> p kc n", p=128))],
                writes=[('aw', cch % 2)], ndma=1)
            for mi in range(6):
                mm = cch * 6 + mi
                for kc in range(8):
                    S.add('pe', lambda e, buf=buf, kc=kc, mm=mm, mi=mi: e.matmul(
                        bank(0)[:, mm * 2:mm * 2 + 2], lhsT=buf[:, kc, mi * 128:(mi + 1) * 128], rhs=scc[:, kc, :],
                        start=(kc == 0), stop=(kc == 7)), reads=[('aw', cch % 2), 'scc'], writes=['bk0'])
        S.add('dve', lambda e: e.tensor_tensor(out=modT, in0=bank(0)[:, 0:96].rearrange("p (m j) -> p m j", j=2),
                                               in1=adb.unsqueeze(2).to_broadcast([128, 48, 2]), op=ALU.add),
              reads=['bk0', 'adb'], writes=['modT'])
        for (sdst, gsrc, moff, nm) in ((s1, n1g, 8, 's1'), (s2, n2g, 32, 's2')):
            S.add('sp', lambda e, gsrc=gsrc: [e.dma_start(out=gcol, in_=gsrc[l])], writes=['gcol'], ndma=1)
            S.add('dve', lambda e, sdst=sdst, moff=moff: e.tensor_scalar(
                out=sdst, in0=modT[:, moff:moff + 8, :], scalar1=1.0, scalar2=None, op0=ALU.add),
                reads=['modT'], writes=[nm])
            S.add('dve', lambda e, sdst=sdst: e.tensor_tensor(
                out=sdst, in0=sdst, in1=gcol.unsqueeze(2).to_broadcast([128, 8, 2]), op=ALU.mult),
                reads=[nm, 'gcol'], writes=[nm])
        bcast_tile(BT['g1x'], modT[:, 16:24, :], 0, 'modT')
        bcast_tile(BT['g1c'], modT[:, 16:24, :], 1, 'modT')
        bcast_tile(BT['g2x'], modT[:, 40:48, :], 0, 'modT')
        bcast_tile(BT['g2c'], modT[:, 40:48, :], 1, 'modT')
        bcast_tile(BT['s2x'], s2, 0, 's2')
        bcast_tile(BT['s2c'], s2, 1, 's2')
        bcast_tile(BT['h2x'], modT[:, 24:32, :], 0, 'modT')
        bcast_tile(BT['h2c'], modT[:, 24:32, :], 1, 'modT')
        S.add('sp', lambda e: [e.dma_start(out=rwp, in_=r_w[l])], writes=['rwp'], ndma=1)
        S.add('sp', lambda e: [e.dma_start(out=rbt, in_=r_b[l])], writes=['rbt'], ndma=1)
        for j in range(2):
            S.add('dve', lambda e, j=j: e.tensor_tensor(out=rwx[:, :, j, :], in0=rwp,
                                                        in1=s2[:, :, j:j + 1].to_broadcast([128, 8, NE]), op=ALU.mult),
                  reads=['rwp', 's2'], writes=[('rwx', j)])
            for kc in range(8):
                S.add('pe', lambda e, j=j, kc=kc: e.matmul(bank(2)[0:1, j * NE:(j + 1) * NE], lhsT=modT[:, 24 + kc, j:j + 1],
                                                          rhs=rwp[:, kc, :], start=(kc == 0), stop=(kc == 7)),
                      reads=['modT', 'rwp'], writes=['bk2'])
            S.add('dve', lambda e, j=j: e.tensor_tensor(out=rcst[:, j, :], in0=bank(2)[0:1, j * NE:(j + 1) * NE], in1=rbt, op=ALU.add),
                  reads=['bk2', 'rbt'], writes=[('rcst', j)])
        S.barrier()

        if 'stop_P0' in dbg:
            return
        A.reset(PERSIST)
        win = A.tile([128, 8, 3328], BF16, 'win')
        for kc in range(8):
            S.add('pool', lambda e, kc=kc: [e.dma_start(out=win[:, kc, :], in_=w_in[l, kc * 128:(kc + 1) * 128, :])],
                  writes=[('win', kc)], ndma=1)
        WIN = [('win', kc) for kc in range(8)]
        xt_p1 = [A.tile([128, D], F32, 'xt') for _ in range(2)]
        xn = [A.tile([128, D], BF16, 'xn') for _ in range(2)]
        junk_p1 = A.tile([128, D], BF16, 'junk')
        ss_p1 = [A.tile([128, 1], F32, 'ss') for _ in range(2)]
        hT = [A.tile([128, 8, 512], BF16, 'hT') for _ in range(2)]
        fsb = [A.tile([128, 512], BF16, 'fsb') for _ in range(2)]
        vst = [A.tile([128, 4, 128], BF16, 'vst') for _ in range(2)]
        qst = [A.tile([64, 4, 8, 128], BF16, 'qst') for _ in range(2)]
        gst = [A.tile([128, 4, 16, 128], BF16, 'gst') for _ in range(2)]
        qraw = [A.tile([128, 512], BF16, 'qraw') for _ in range(2)]
        for b_ in range(2):
            S.add('pool', lambda e, b_=b_: e.memset(qraw[b_], 0.0), writes=[('qraw', b_)])
        qro = [A.tile([64, 512], BF16, 'qro') for _ in range(2)]
        rt1 = [A.tile([64, 512], F32, 'rt1') for _ in range(2)]
        rt2 = [A.tile([64, 512], F32, 'rt2') for _ in range(2)]
        cst = A.tile([64, 512], F32, 'cst')
        snt = A.tile([64, 512], F32, 'snt')
        nst = (T + 511) // 512
        it = 0
        ih = 0
        for st in range(nst):
            if 'one_st' in dbg and st > 0:
                break
            t0 = st * 512
            ntok = min(512, T - t0)
            ntile = ntok // 128
            isx = t0 < TX
            j = 0 if isx else 1
            hb = hT[st % 2]
            HK = ('hT', st % 2)
            for ti in range(ntile):
                b = it % 2
                it += 1
                r0 = t0 + ti * 128
                S.add(dmaq(), lambda e, b=b, r0=r0: [e.dma_start(out=xt_p1[b], in_=xres[r0:r0 + 128, :])],
                      writes=[('xt', b)], ndma=1)
                S.add('act', lambda e, b=b: e.activation(out=junk_p1, in_=xt_p1[b], func=AF.Square, accum_out=ss_p1[b]),
                      reads=[('xt', b)], writes=[('ss', b), 'junk'])
                S.add('dve', lambda e, b=b: e.tensor_scalar(out=ss_p1[b], in0=ss_p1[b], scalar1=1.0 / D, scalar2=EPS,
                                                            op0=ALU.mult, op1=ALU.add), reads=[('ss', b)], writes=[('ss', b)])
                S.add('act', lambda e, b=b: e.sqrt(out=ss_p1[b], in_=ss_p1[b]), reads=[('ss', b)], writes=[('ss', b)])
                S.add('dve', lambda e, b=b: e.reciprocal(out=ss_p1[b], in_=ss_p1[b]), reads=[('ss', b)], writes=[('ss', b)])
                S.add('act', lambda e, b=b: e.activation(out=xn[b], in_=xt_p1[b], func=AF.Copy, scale=ss_p1[b][:, 0:1]),
                      reads=[('xt', b), ('ss', b)], writes=[('xn', b)])
                pb = pbf[b]
                for kc in range(8):
                    S.add('pe', lambda e, b=b, kc=kc, pb=pb: e.transpose(out=pb[:, kc * 128:(kc + 1) * 128],
                                                                        in_=xn[b][:, kc * 128:(kc + 1) * 128], identity=C['ident_bf']),
                          reads=[('xn', b), 'ident_bf'], writes=[('pbf', b)])
                S.add('dve', lambda e, pb=pb, j=j, hb=hb, ti=ti: e.tensor_tensor(
                    out=hb[:, :, ti * 128:(ti + 1) * 128], in0=pb.rearrange("p (k t) -> p k t", t=128),
                    in1=s1[:, :, j:j + 1].to_broadcast([128, 8, 128]), op=ALU.mult),
                    reads=[('pbf', b), 's1'], writes=[HK])
                S.add('pool', lambda e, j=j, hb=hb, ti=ti: e.tensor_tensor(
                    out=hb[:, :, ti * 128:(ti + 1) * 128], in0=hb[:, :, ti * 128:(ti + 1) * 128],
                    in1=modT[:, 0:8, j:j + 1].to_broadcast([128, 8, 128]), op=ALU.add),
                    reads=[HK, 'modT'], writes=[HK])
                if 'p1_norm' in dbg:
                    continue
                bk = bank(ti % 2)
                for kc in range(8):
                    S.add('pe', lambda e, hb=hb, ti=ti, kc=kc, bk=bk: e.matmul(
                        bk, lhsT=hb[:, kc, ti * 128:(ti + 1) * 128], rhs=win[:, kc, 0:512], start=(kc == 0), stop=(kc == 7)),
                        reads=[HK] + WIN, writes=[('bk', ti % 2)])
                fb = fsb[ti % 2]
                S.add('act', lambda e, fb=fb, bk=bk: e.copy(out=fb, in_=bk), reads=[('bk', ti % 2)], writes=[('fsb', ti % 2)])
                S.add('sp', lambda e, fb=fb, r0=r0: [e.dma_start(out=f_tm[r0:r0 + 128, :], in_=fb)],
                      reads=[('fsb', ti % 2)], writes=[('f_tm', r0)], ndma=1)
                bk = bank(2 + ti % 2)
                for kc in range(8):
                    S.add('pe', lambda e, hb=hb, ti=ti, kc=kc, bk=bk: e.matmul(
                        bk[:, 0:128], lhsT=hb[:, kc, ti * 128:(ti + 1) * 128], rhs=win[:, kc, 1152:1280], start=(kc == 0), stop=(kc == 7)),
                        reads=[HK] + WIN, writes=[('bk', 2 + ti % 2)])
                S.add('dve', lambda e, bk=bk, ti=ti, st=st: e.tensor_copy(out=vst[st % 2][:, ti, :], in_=bk[:, 0:128]),
                      reads=[('bk', 2 + ti % 2)], writes=[('vst', st % 2)])
            if 'p1_norm' not in dbg:
                S.add('sp', lambda e, st=st, ntile=ntile: [e.dma_start(out=v_blk[:, st * 4:st * 4 + ntile, :], in_=vst[st % 2][:, 0:ntile, :])],
                      reads=[('vst', st % 2)], writes=[('v_blk', st)], ndma=1)
            if 'p1_norm' in dbg or 'p1_fv' in dbg:
                continue
            if isx:
                S.add('sp', lambda e, t0=t0: [e.dma_start(out=cst, in_=cd['cosT'][:, t0:t0 + 512])], writes=['cst'], ndma=1)
                S.add('act', lambda e, t0=t0: [e.dma_start(out=snt, in_=cd['sinT'][:, t0:t0 + 512])], writes=['snt'], ndma=1)
            for hh in range(10):
                col0 = 512 + hh * 64
                b = ih % 2
                ih += 1
                bk = bank(4)
                for kc in range(8):
                    S.add('pe', lambda e, hb=hb, kc=kc, col0=col0, bk=bk, ntok=ntok: e.matmul(
                        bk[0:64, 0:ntok], lhsT=win[:, kc, col0:col0 + 64], rhs=hb[:, kc, 0:ntok], start=(kc == 0), stop=(kc == 7)),
                        reads=[HK] + WIN, writes=['bk4'])
                QSK = ('qst', st % 2)
                if hh < 8:
                    fin = qst[st % 2][:, 0:ntile, hh, :]
                    fink = [QSK]
                else:
                    fin = qro[b][:, 0:ntok].rearrange("p (a t) -> p a t", t=128)
                    fink = [('qro', b)]
                if isx:
                    S.add('act', lambda e, b=b, bk=bk: e.copy(out=qraw[b][0:64, :], in_=bk[0:64, :]), reads=['bk4'], writes=[('qraw', b)])
                    bk5 = bank(5)
                    S.add('pe', lambda e, b=b, bk5=bk5: e.matmul(bk5, lhsT=C['rotm'], rhs=qraw[b], start=True, stop=True),
                          reads=[('qraw', b), 'rotm'], writes=['bk5'])
                    S.add('act', lambda e, b=b, bk=bk: e.copy(out=rt1[b], in_=bk[0:64, :]), reads=['bk4'], writes=[('rt1', b)])
                    S.add('act', lambda e, b=b, bk5=bk5: e.copy(out=rt2[b], in_=bk5[0:64, :]), reads=['bk5'], writes=[('rt2', b)])
                    S.add('pool', lambda e, b=b: e.tensor_tensor(out=rt1[b], in0=rt1[b], in1=cst, op=ALU.mult),
                          reads=[('rt1', b), 'cst'], writes=[('rt1', b)])
                    S.add('pool', lambda e, b=b: e.tensor_tensor(out=rt2[b], in0=rt2[b], in1=snt, op=ALU.mult),
                          reads=[('rt2', b), 'snt'], writes=[('rt2', b)])
                    S.add('pool', lambda e, b=b, fin=fin: e.tensor_tensor(
                        out=fin, in0=rt1[b].rearrange("p (a t) -> p a t", t=128), in1=rt2[b].rearrange("p (a t) -> p a t", t=128), op=ALU.add),
                        reads=[('rt1', b), ('rt2', b)], writes=fink)
                else:
                    S.add('act', lambda e, bk=bk, ntok=ntok, fin=fin: e.copy(out=fin, in_=bk[0:64, 0:ntok].rearrange("p (a t) -> p a t", t=128)),
                          reads=['bk4'], writes=fink)
                if hh >= 8:
                    S.add(dmaq(), lambda e, b=b, hh=hh, t0=t0, ntok=ntok: [e.dma_start(out=kT_all[:, hh - 8, t0:t0 + ntok], in_=qro[b][:, 0:ntok])],
                          reads=[('qro', b)], writes=[('kT_all', hh, st)], ndma=1)
                elif hh == 7:
                    S.add(dmaq(), lambda e, st=st, ntile=ntile: [e.dma_start(
                        out=q_blk[st * 4:st * 4 + ntile].rearrange("b d h t -> d b (h t)"),
                        in_=qst[st % 2][:, 0:ntile, :, :].rearrange("p a h t -> p a (h t)"))],
                        reads=[QSK], writes=[('q_blk', st)], ndma=1)
            if 'p1_qk' in dbg:
                continue
            for m in range(16):
                col0 = 1280 + m * 128
                bk = bank(m % 2)
                for kc in range(8):
                    S.add('pe', lambda e, hb=hb, kc=kc, col0=col0, bk=bk, ntok=ntok: e.matmul(
                        bk[:, 0:ntok], lhsT=win[:, kc, col0:col0 + 128], rhs=hb[:, kc, 0:ntok], start=(kc == 0), stop=(kc == 7)),
                        reads=[HK] + WIN, writes=[('bk', m % 2)])
                S.add('act', lambda e, bk=bk, ntok=ntok, ntile=ntile, m=m, st=st: e.activation(
                    out=gst[st % 2][:, 0:ntile, m, :], in_=bk[:, 0:ntok].rearrange("p (a t) -> p a t", t=128), func=AF.Sigmoid),
                    reads=[('bk', m % 2)], writes=[('gst', st % 2)])
            S.add(dmaq(), lambda e, st=st, ntile=ntile: [e.dma_start(
                out=g_blk[st * 4:st * 4 + ntile].rearrange("b p m t -> p b (m t)"),
                in_=gst[st % 2][:, 0:ntile, :, :].rearrange("p a m t -> p a (m t)"))],
                reads=[('gst', st % 2)], writes=[('g_blk', st)], ndma=1)
        S.barrier()

        if 'stop_P1' in dbg:
            return
        A.reset(PERSIST)
        wao = A.tile([64, 8, D], BF16, 'wao')
        S.add('pool', lambda e: [e.dma_start(out=wao, in_=w_ao[l].rearrange("(h d) n -> d h n", d=64))], writes=['wao'], ndma=1)
        kall = A.tile([64, 2, T], BF16, 'kall')
        S.add('sp', lambda e: [e.dma_start(out=kall, in_=kT_all)], writes=['kall'], ndma=1)
        vall = A.tile([128, NT, 128], BF16, 'vall')
        S.add('act', lambda e: [e.dma_start(out=vall, in_=v_blk)], writes=['vall'], ndma=1)
        skr = A.tile([64, 8], F32, 'skr')
        S.add('sp', lambda e: [e.dma_start(out=skr, in_=sink[l:l + 1, :].to_broadcast([64, 8]))], writes=['skr'], ndma=1)
        S.add('act', lambda e: e.activation(out=skr, in_=skr, func=AF.Exp), reads=['skr'], writes=['skr'])
        esk = A.tile([64, 8, 128], F32, 'esk')
        S.add('dve', lambda e: e.tensor_copy(out=esk, in_=skr.unsqueeze(2).to_broadcast([64, 8, 128])), reads=['skr'], writes=['esk'])
        qb_sb = [A.tile([64, 8, 128], BF16, 'qb') for _ in range(2)]
        pT = [A.tile([128, 512], BF16, 'pT') for _ in range(3)]
        rec = A.tile([64, 512], F32, 'rec')
        pos_ = A.tile([64, 512], F32, 'pos_')
        atT = [A.tile([64, 8, 128], BF16, 'atT') for _ in range(2)]
        gab = [A.tile([128, 8, 128], BF16, 'gab') for _ in range(2)]
        t2b = [A.tile([128, 8, 128], BF16, 't2b') for _ in range(2)]
        ip = 0
        for qb in range(NT):
            b = qb % 2
            r0 = qb * 128
            isx = qb < NTX
            S.add('sp', lambda e, b=b, qb=qb: [e.dma_start(out=qb_sb[b], in_=q_blk[qb])], writes=[('qb', b)], ndma=1)
            S.add('act', lambda e, b=b, qb=qb: [e.dma_start(out=gab[b], in_=g_blk[qb][:, 8:16, :])], writes=[('gab', b)], ndma=1)
            chunks = []
            if isx:
                lo = max(qb - 1, 0)
                hi = min(qb + 1, NTX - 1)
                for kbk in range(lo, hi + 1):
                    ci = kbk
                    mk = None if kbk == qb else ('mask_prev' if kbk < qb else 'mask_next')
                    chunks.append(('loc', ci, mk))
            chunks.append(('loc', NTX, None))
            chunks.append(('loc', NTX + 1, None))
            for kv in range(2):
                po = bank(2 + kv * 2)
                pd = bank(3 + kv * 2)
                POK = ('bk', 2 + kv * 2)
                PDK = ('bk', 3 + kv * 2)
                nch = len(chunks)
                pts = []
                for cidx in range(nch):
                    pts.append((pT[ip % 3], ('pT', ip % 3)))
                    ip += 1

                def st_mm(cidx):
                    ci = chunks[cidx][1]
                    sb = bank(cidx % 2)
                    kl = kall[:, kv, ci * 128:(ci + 1) * 128]
                    S.add('pe', lambda e, sb=sb, kl=kl, b=b, kv=kv: e.matmul(
                        sb.rearrange("p (h t) -> p h t", t=128), lhsT=kl, rhs=qb_sb[b][:, kv * 4:(kv + 1) * 4, :], start=True, stop=True),
                        reads=['kall', ('qb', b)], writes=[('bk', cidx % 2)])

                st_mm(0)
                for cidx, (kind, ci, mk) in enumerate(chunks):
                    sb = bank(cidx % 2)
                    SBK = ('bk', cidx % 2)
                    vl = vall[:, ci, kv * 64:(kv + 1) * 64]
                    pt, PTK = pts[cidx]
                    S.add('act', lambda e, pt=pt, sb=sb: e.activation(out=pt, in_=sb, func=AF.Exp, scale=0.125),
                          reads=[SBK], writes=[PTK])
                    if cidx + 1 < nch:
                        st_mm(cidx + 1)
                    if mk is not None:
                        S.add('pool', lambda e, pt=pt, mk=mk: e.tensor_tensor(
                            out=pt.rearrange("p (h t) -> p h t", t=128), in0=pt.rearrange("p (h t) -> p h t", t=128),
                            in1=C[mk].unsqueeze(1).to_broadcast([128, 4, 128]), op=ALU.mult),
                            reads=[PTK, mk], writes=[PTK])
                    first = cidx == 0
                    last = cidx == nch - 1
                    S.add('pe', lambda e, po=po, vl=vl, pt=pt, first=first, last=last: e.matmul(
                        po[0:64, :], lhsT=vl, rhs=pt, start=first, stop=last), reads=['vall', PTK], writes=[POK])
                    S.add('pe', lambda e, pd=pd, pt=pt, first=first, last=last: e.matmul(
                        pd[0:64, :], lhsT=ones_bf[:, 0:64], rhs=pt, start=first, stop=last), reads=['ones_bf', PTK], writes=[PDK])
                S.add('act', lambda e, pd=pd: e.copy(out=rec, in_=pd[0:64, :]), reads=[PDK], writes=['rec'])
                S.add('act', lambda e, po=po: e.copy(out=pos_, in_=po[0:64, :]), reads=[POK], writes=['pos_'])
                S.add('dve', lambda e, kv=kv: e.tensor_tensor(
                    out=rec, in0=rec, in1=esk[:, kv * 4:(kv + 1) * 4, :].rearrange("p h t -> p (h t)"), op=ALU.add),
                    reads=['rec', 'esk'], writes=['rec'])
                S.add('dve', lambda e: e.reciprocal(out=rec, in_=rec), reads=['rec'], writes=['rec'])
                S.add('pool', lambda e, kv=kv, b=b: e.tensor_tensor(
                    out=atT[b][:, kv * 4:(kv + 1) * 4, :].rearrange("p h t -> p (h t)"), in0=pos_, in1=rec, op=ALU.mult),
                    reads=['pos_', 'rec'], writes=[('atT', b)])
            for m in range(8):
                ob = bank(m % 2)
                OBK = ('bk', m % 2)
                for h in range(8):
                    S.add('pe', lambda e, ob=ob, h=h, m=m, b=b: e.matmul(
                        ob[:, 0:128], lhsT=wao[:, h, m * 128:(m + 1) * 128], rhs=atT[b][:, h, :], start=(h == 0), stop=(h == 7)),
                        reads=['wao', ('atT', b)], writes=[OBK])
                S.add('dve', lambda e, ob=ob, m=m, b=b: e.tensor_tensor(
                    out=t2b[b][:, m, :], in0=ob[:, 0:128], in1=gab[b][:, m, :], op=ALU.mult),
                    reads=[OBK, ('gab', b)], writes=[('t2b', b)])
            S.add('sp', lambda e, b=b, qb=qb: [e.dma_start(out=t2_blk[qb], in_=t2b[b])],
                  reads=[('t2b', b)], writes=[('t2_blk', qb)], ndma=1)
        S.barrier()

        if 'stop_P2' in dbg:
            return
        A.reset(PERSIST)
        fmT = [A.tile([128, T], BF16, 'fmT') for _ in range(4)]
        MK3 = A.mark()
        Dt = [A.tile([128, 8, 512], BF16, 'Dt') for _ in range(2)]
        Zc = [A.tile([128, 2, 8, 512], BF16, 'Zc') for _ in range(2)]
        tt1 = [A.tile([128, 512], F32, 'tt1') for _ in range(2)]
        tt2 = [A.tile([128, 512], F32, 'tt2') for _ in range(2)]
        tt3 = [A.tile([128, 512], F32, 'tt3') for _ in range(2)]
        tt4 = [A.tile([128, 512], F32, 'tt4') for _ in range(2)]
        fview = f_tm[0:TX, :].rearrange("(n1 n2) c -> n1 n2 c", n2=64)
        for n2c in range(8):
            b = n2c % 2
            S.add(dmaq(), lambda e, b=b, n2c=n2c: [e.dma_start(out=Dt[b], in_=fview[:, n2c * 8:(n2c + 1) * 8, :])],
                  writes=[('Dt', b)], ndma=1)
            for jj in range(8):
                n2 = n2c * 8 + jj
                bb = jj % 2
                pr = bank(bb * 2)
                ps = bank(bb * 2 + 1)
                S.add('pe', lambda e, pr=pr, b=b, jj=jj: e.matmul(pr, lhsT=C['c128'], rhs=Dt[b][:, jj, :], start=True, stop=True),
                      reads=[('Dt', b), 'c128'], writes=[('bk', bb * 2)])
                S.add('pe', lambda e, ps=ps, b=b, jj=jj: e.matmul(ps, lhsT=C['s128'], rhs=Dt[b][:, jj, :], start=True, stop=True),
                      reads=[('Dt', b), 's128'], writes=[('bk', bb * 2 + 1)])
                S.add('act', lambda e, pr=pr, bb=bb, n2=n2: e.activation(out=tt1[bb], in_=pr, func=AF.Copy, scale=C['twr'][:, n2:n2 + 1]),
                      reads=[('bk', bb * 2), 'twr'], writes=[('tt1', bb)])
                S.add('act', lambda e, pr=pr, bb=bb, n2=n2: e.activation(out=tt2[bb], in_=pr, func=AF.Copy, scale=C['twi'][:, n2:n2 + 1]),
                      reads=[('bk', bb * 2), 'twi'], writes=[('tt2', bb)])
                S.add('act', lambda e, ps=ps, bb=bb, n2=n2: e.activation(out=tt3[bb], in_=ps, func=AF.Copy, scale=C['ntwi'][:, n2:n2 + 1]),
                      reads=[('bk', bb * 2 + 1), 'ntwi'], writes=[('tt3', bb)])
                S.add('act', lambda e, ps=ps, bb=bb, n2=n2: e.activation(out=tt4[bb], in_=ps, func=AF.Copy, scale=C['twr'][:, n2:n2 + 1]),
                      reads=[('bk', bb * 2 + 1), 'twr'], writes=[('tt4', bb)])
                S.add('dve', lambda e, bb=bb, b=b, jj=jj: e.tensor_tensor(out=Zc[b][:, 0, jj, :], in0=tt1[bb], in1=tt3[bb], op=ALU.add),
                      reads=[('tt1', bb), ('tt3', bb)], writes=[('Zc', b)])
                S.add('pool', lambda e, bb=bb, b=b, jj=jj: e.tensor_tensor(out=Zc[b][:, 1, jj, :], in0=tt2[bb], in1=tt4[bb], op=ALU.add),
                      reads=[('tt2', bb), ('tt4', bb)], writes=[('Zc', b)])
            for ri in range(2):
                S.add(dmaq(), lambda e, b=b, ri=ri, n2c=n2c: [e.dma_start(out=Zd[ri, :, n2c * 8:(n2c + 1) * 8, :], in_=Zc[b][:, ri, :, :])],
                      reads=[('Zc', b)], writes=[('Zd', ri, n2c)], ndma=1)
        S.barrier()
        if 'p3_s1' in dbg:
            return
        A.reset(MK3)
        Pg = A.tile([128, 2, 2, TX], BF16, 'Pg')
        Zs = [A.tile([128, 8, 256], BF16, 'Zs') for _ in range(2)]
        iz = 0
        for gp in range(2):
            for k1c in range(16):
                b = iz % 2
                iz += 1
                for ri in range(2):
                    S.add(dmaq(), lambda e, b=b, ri=ri, k1c=k1c, gp=gp: [e.dma_start(
                        out=Zs[b][ri * 64:(ri + 1) * 64, :, :],
                        in_=Zd[ri, k1c * 8:(k1c + 1) * 8, :, gp * 256:(gp + 1) * 256].rearrange("k n c -> n k c"))],
                        writes=[('Zs', b, ri)], ndma=1)
                for gi in range(2):
                    pz = pq[gi]
                    PZK = ('pq', gi)
                    for kj in range(8):
                        S.add('pe', lambda e, pz=pz, b=b, kj=kj, gi=gi: e.matmul(pz[:, kj * 128:(kj + 1) * 128], lhsT=Zs[b][:, kj, gi * 128:(gi + 1) * 128],
                                                                                rhs=C['r2'], start=True, stop=True),
                              reads=[('Zs', b, 0), ('Zs', b, 1), 'r2'], writes=[PZK])
                    for ri in range(2):
                        for hf in range(2):
                            outv = Pg[:, gi, ri, :].rearrange("c (k1 k2) -> c k1 k2", k2=64)[:, k1c * 8 + hf * 4:k1c * 8 + hf * 4 + 4, :]
                            inv = pz[:, hf * 512:(hf + 1) * 512].rearrange("c (k1 r k2) -> c k1 r k2", r=2, k2=64)[:, :, ri, :]
                            if (ri + hf) % 2 == 0:
                                S.add('act', lambda e, outv=outv, inv=inv: e.copy(out=outv, in_=inv), reads=[PZK], writes=[('Pg', gi, ri)])
                            else:
                                S.add('dve', lambda e, outv=outv, inv=inv: e.tensor_copy(out=outv, in_=inv), reads=[PZK], writes=[('Pg', gi, ri)])
            for gi in range(2):
                g = gp * 2 + gi
                for tc in range(16):
                    bk = bank(4 + tc % 2)
                    BKK = ('bk', 4 + tc % 2)
                    S.add('pe', lambda e, bk=bk, tc=tc, gi=gi: e.matmul(bk, lhsT=C['cc'], rhs=Pg[:, gi, 0, tc * 512:(tc + 1) * 512], start=True, stop=False),
                          reads=[('Pg', gi, 0), 'cc'], writes=[BKK])
                    S.add('pe', lambda e, bk=bk, tc=tc, gi=gi: e.matmul(bk, lhsT=C['nsc'], rhs=Pg[:, gi, 1, tc * 512:(tc + 1) * 512], start=False, stop=True),
                          reads=[('Pg', gi, 1), 'nsc'], writes=[BKK])
                    if tc % 2 == 0:
                        S.add('act', lambda e, bk=bk, tc=tc, g=g: e.copy(out=fmT[g][:, tc * 512:(tc + 1) * 512], in_=bk), reads=[BKK], writes=[('fmT', g)])
                    else:
                        S.add('dve', lambda e, bk=bk, tc=tc, g=g: e.tensor_copy(out=fmT[g][:, tc * 512:(tc + 1) * 512], in_=bk), reads=[BKK], writes=[('fmT', g)])
        if 'p3_s2' in dbg:
            S.barrier()
            return
        Dc = A.tile([128, 2, 512], BF16, 'Dc')
        Pc = A.tile([128, 2, TC], BF16, 'Pc')
        S.add('sp', lambda e: [e.dma_start(out=Dc, in_=f_tm[TX:T, :].rearrange("(c p) f -> p c f", p=128))], writes=['Dc'], ndma=1)
        for g in range(4):
            for ri, cn in enumerate(('c256', 's256')):
                bk = bank(ri)
                for ch in range(2):
                    S.add('pe', lambda e, bk=bk, ch=ch, g=g, cn=cn: e.matmul(bk[:, 0:TC], lhsT=Dc[:, ch, g * 128:(g + 1) * 128], rhs=C[cn][:, ch, :],
                                                                            start=(ch == 0), stop=(ch == 1)),
                          reads=['Dc', cn], writes=[('bk', ri)])
                S.add('act', lambda e, bk=bk, ri=ri: e.copy(out=Pc[:, ri, :], in_=bk[:, 0:TC]), reads=[('bk', ri)], writes=[('Pc', ri)])
            bk = bank(2)
            S.add('pe', lambda e, bk=bk: e.matmul(bk[:, 0:TC], lhsT=C['cc'], rhs=Pc[:, 0, :], start=True, stop=False), reads=[('Pc', 0), 'cc'], writes=['bk2'])
            S.add('pe', lambda e, bk=bk: e.matmul(bk[:, 0:TC], lhsT=C['nsc'], rhs=Pc[:, 1, :], start=False, stop=True), reads=[('Pc', 1), 'nsc'], writes=['bk2'])
            S.add('dve', lambda e, bk=bk, g=g: e.tensor_copy(out=fmT[g][:, TX:T], in_=bk[:, 0:TC]), reads=['bk2'], writes=[('fmT', g)])
        S.barrier()

        if 'stop_P3' in dbg:
            return
        A.reset(MK3)
        wfo = A.tile([128, 4, D], BF16, 'wfo')
        S.add('pool', lambda e: [e.dma_start(out=wfo, in_=w_fo[l].rearrange("(g p) n -> p g n", p=128))], writes=['wfo'], ndma=1)
        wou = A.tile([128, 8, D], BF16, 'wou')
        S.add('pool', lambda e: [e.dma_start(out=wou, in_=w_out[l].rearrange("(k p) n -> p k n", p=128))], writes=['wou'], ndma=1)
        sgf = [A.tile([128, 4, 8, 128], BF16, 'sgf') for _ in range(2)]
        t2s = [A.tile([128, 4, 8, 128], BF16, 't2s')] * 2
        yT = [A.tile([128, 8, 512], BF16, 'yT')] * 2
        ut = A.tile([128, 512], F32, 'ut')
        xt_p4 = [A.tile([128, D], F32, 'xt4') for _ in range(2)]
        xo_p4 = [A.tile([128, D], F32, 'xo4')] * 2
        it = 0
        for st in range(nst):
            t0 = st * 512
            ntok = min(512, T - t0)
            ntile = ntok // 128
            isx = t0 < TX
            b = st % 2
            S.add('sp', lambda e, b=b, st=st, ntile=ntile: [e.dma_start(
                out=sgf[b][:, 0:ntile, :, :].rearrange("p a m t -> p a (m t)"),
                in_=g_blk[st * 4:st * 4 + ntile, :, 0:8, :].rearrange("b p m t -> p b (m t)"))], writes=[('sgf', b)], ndma=1)
            S.add('act', lambda e, b=b, st=st, ntile=ntile: [e.dma_start(
                out=t2s[b][:, 0:ntile, :, :].rearrange("p a m t -> p a (m t)"),
                in_=t2_blk[st * 4:st * 4 + ntile].rearrange("b p m t -> p b (m t)"))], writes=[('t2s', 0)], ndma=1)
            for m in range(8):
                bk = bank(m % 2)
                BKK = ('bk', m % 2)
                for g in range(4):
                    if isx:
                        rhs = fmT[g][:, 0:TX].rearrange("c (k1 k2) -> c k2 k1", k2=64)[:, st * 4:st * 4 + 4, :]
                    else:
                        rhs = fmT[g][:, TX:T].rearrange("c (a t) -> c a t", t=128)
                    S.add('pe', lambda e, bk=bk, g=g, m=m, rhs=rhs, ntok=ntok: e.matmul(
                        bk[:, 0:ntok].rearrange("p (a t) -> p a t", t=128), lhsT=wfo[:, g, m * 128:(m + 1) * 128], rhs=rhs, start=(g == 0), stop=(g == 3)),
                        reads=['wfo', ('fmT', g)], writes=[BKK])
                S.add('dve', lambda e, bk=bk, b=b, m=m, ntok=ntok, ntile=ntile: e.tensor_tensor(
                    out=ut[:, 0:ntok].rearrange("p (a t) -> p a t", t=128), in0=bk[:, 0:ntok].rearrange("p (a t) -> p a t", t=128),
                    in1=sgf[b][:, 0:ntile, m, :], op=ALU.mult), reads=[BKK, ('sgf', b)], writes=['ut'])
                S.add('pool', lambda e, b=b, m=m, ntok=ntok, ntile=ntile: e.tensor_tensor(
                    out=yT[b][:, m, 0:ntok].rearrange("p (a t) -> p a t", t=128), in0=ut[:, 0:ntok].rearrange("p (a t) -> p a t", t=128),
                    in1=t2s[b][:, 0:ntile, m, :], op=ALU.add), reads=['ut', ('t2s', 0)], writes=[('yT', 0)])
            for ti in range(ntile):
                xb = it % 2
                it += 1
                r0 = t0 + ti * 128
                S.add(dmaq(), lambda e, xb=xb, r0=r0: [e.dma_start(out=xt_p4[xb], in_=xres[r0:r0 + 128, :])], writes=[('xt4', xb)], ndma=1)
                gb = BT['g1x'] if isx else BT['g1c']
                for n in range(2):
                    bk = bank(2 + n)
                    BKK = ('bk', 2 + n)
                    for kc in range(8):
                        S.add('pe', lambda e, bk=bk, b=b, kc=kc, ti=ti, n=n: e.matmul(
                            bk, lhsT=yT[b][:, kc, ti * 128:(ti + 1) * 128], rhs=wou[:, kc, n * 512:(n + 1) * 512], start=(kc == 0), stop=(kc == 7)),
                            reads=[('yT', 0), 'wou'], writes=[BKK])
                    S.add('act', lambda e, bk=bk, xb=xb, n=n: e.copy(out=xo_p4[xb][:, n * 512:(n + 1) * 512], in_=bk),
                          reads=[BKK], writes=[('xo4', 0, n)])
                    S.add('dve', lambda e, xb=xb, n=n, gb=gb: e.tensor_tensor(
                        out=xo_p4[xb][:, n * 512:(n + 1) * 512], in0=xo_p4[xb][:, n * 512:(n + 1) * 512], in1=gb[:, n * 512:(n + 1) * 512], op=ALU.mult),
                        reads=[('xo4', 0, n), ('btile', id(gb))], writes=[('xo4', 0, n)])
                    S.add('pool', lambda e, xb=xb, n=n: e.tensor_tensor(
                        out=xo_p4[xb][:, n * 512:(n + 1) * 512], in0=xo_p4[xb][:, n * 512:(n + 1) * 512], in1=xt_p4[xb][:, n * 512:(n + 1) * 512], op=ALU.add),
                        reads=[('xo4', 0, n), ('xt4', xb)], writes=[('xo4', 0, n)])
                S.add(dmaq(), lambda e, xb=xb, r0=r0: [e.dma_start(out=xres[r0:r0 + 128, :], in_=xo_p4[xb])],
                      reads=[('xo4', 0, 0), ('xo4', 0, 1)], writes=[('xres', r0)], ndma=1)
        S.barrier()

        if 'stop_P4' in dbg:
            return
        if 'skip_moe' in dbg:
            return
        A.reset(PERSIST)
        LG = A.tile([128, NT, NE], F32, 'LG')
        V8 = A.tile([128, NT, 8], F32, 'V8')
        MK = A.tile([128, NT, NE], BF16, 'MK')
        G4 = A.tile([128, NT, 4], F32, 'G4')
        DESTi = A.tile([128, NT, 4], I32, 'DESTi')
        IDXW = A.tile([128, NB], I32, 'IDXW')
        IDXE = A.tile([2, NB], I32, 'IDXE')
        MK5 = A.mark()
        xt_p5 = [A.tile([128, D], F32, 'xt5') for _ in range(2)]
        xnf = [A.tile([128, D], F32, 'xnf') for _ in range(2)]
        xTf = [A.tile([128, 8, 128], F32, 'xTf') for _ in range(2)]
        h2a = [A.tile([128, D], F32, 'h2a') for _ in range(2)]
        h2b = [A.tile([128, D], BF16, 'h2b') for _ in range(2)]
        junk_p5 = A.tile([128, D], BF16, 'junk5')
        ss_p5 = [A.tile([128, 1], F32, 'ss5') for _ in range(2)]
        for tj in range(NT):
            b = tj % 2
            r0 = tj * 128
            isx = tj < NTX
            j = 0 if isx else 1
            S.add(dmaq(), lambda e, b=b, r0=r0: [e.dma_start(out=xt_p5[b], in_=xres[r0:r0 + 128, :])], writes=[('xt5', b)], ndma=1)
            S.add('act', lambda e, b=b: e.activation(out=junk_p5, in_=xt_p5[b], func=AF.Square, accum_out=ss_p5[b]),
                  reads=[('xt5', b)], writes=[('ss5', b), 'junk5'])
            S.add('dve', lambda e, b=b: e.tensor_scalar(out=ss_p5[b], in0=ss_p5[b], scalar1=1.0 / D, scalar2=EPS, op0=ALU.mult, op1=ALU.add),
                  reads=[('ss5', b)], writes=[('ss5', b)])
            S.add('act', lambda e, b=b: e.sqrt(out=ss_p5[b], in_=ss_p5[b]), reads=[('ss5', b)], writes=[('ss5', b)])
            S.add('dve', lambda e, b=b: e.reciprocal(out=ss_p5[b], in_=ss_p5[b]), reads=[('ss5', b)], writes=[('ss5', b)])
            S.add('act', lambda e, b=b: e.activation(out=xnf[b], in_=xt_p5[b], func=AF.Copy, scale=ss_p5[b][:, 0:1]),
                  reads=[('xt5', b), ('ss5', b)], writes=[('xnf', b)])
            sb_ = BT['s2x'] if isx else BT['s2c']
            hb_ = BT['h2x'] if isx else BT['h2c']
            S.add('dve', lambda e, b=b, sb_=sb_: e.tensor_tensor(out=h2a[b], in0=xnf[b], in1=sb_, op=ALU.mult),
                  reads=[('xnf', b), ('btile', id(sb_))], writes=[('h2a', b)])
            S.add('pool', lambda e, b=b, hb_=hb_: e.tensor_tensor(out=h2b[b], in0=h2a[b], in1=hb_, op=ALU.add),
                  reads=[('h2a', b), ('btile', id(hb_))], writes=[('h2b', b)])
            S.add(dmaq(), lambda e, b=b, r0=r0: [e.dma_start(out=h2_tm[r0:r0 + 128, :], in_=h2b[b])], reads=[('h2b', b)],
                  writes=[('h2_tm', tj)], ndma=1)
            pz = pq[b]
            PZK = ('pq', b)
            for kc in range(8):
                S.add('pe', lambda e, pz=pz, b=b, kc=kc: e.transpose(out=pz[:, kc * 128:(kc + 1) * 128], in_=xnf[b][:, kc * 128:(kc + 1) * 128],
                                                                    identity=C['ident_f']), reads=[('xnf', b), 'ident_f'], writes=[PZK])
            S.add('act', lambda e, pz=pz, b=b: e.copy(out=xTf[b].rearrange("p k t -> p (k t)")[:, 0:512], in_=pz[:, 0:512]), reads=[PZK], writes=[('xTf', b, 0)])
            S.add('dve', lambda e, pz=pz, b=b: e.tensor_copy(out=xTf[b].rearrange("p k t -> p (k t)")[:, 512:1024], in_=pz[:, 512:1024]), reads=[PZK], writes=[('xTf', b, 1)])
            lb = bank(4)
            for kc in range(8):
                S.add('pe', lambda e, lb=lb, b=b, kc=kc, j=j: e.matmul(lb[:, 0:NE], lhsT=xTf[b][:, kc, :], rhs=rwx[:, kc, j, :], start=(kc == 0), stop=False),
                      reads=[('xTf', b, 0), ('xTf', b, 1), ('rwx', j)], writes=['bk4'])
            S.add('pe', lambda e, lb=lb, j=j: e.matmul(lb[:, 0:NE], lhsT=ones_f[0:1, :], rhs=rcst[:, j, :], start=False, stop=True),
                  reads=['ones_f', ('rcst', j)], writes=['bk4'])
            S.add('dve', lambda e, lb=lb, tj=tj: e.tensor_copy(out=LG[:, tj, :], in_=lb[:, 0:NE]), reads=['bk4'], writes=[('LG', tj)])
            S.add('dve', lambda e, tj=tj: e.max(out=V8[:, tj, :], in_=LG[:, tj, :]), reads=[('LG', tj)], writes=[('V8', tj)])
            S.add('dve', lambda e, tj=tj: e.tensor_scalar(out=MK[:, tj, :], in0=LG[:, tj, :], scalar1=V8[:, tj, 3:4], scalar2=None, op0=ALU.is_ge),
                  reads=[('LG', tj), ('V8', tj)], writes=[('MK', tj)])
        S.barrier()
        A.reset(MK5)
        NC_ = NT * NE
        POS = A.tile([128, NT, NE], F32, 'POS')
        CNT = A.tile([128, NT, NE], F32, 'CNT')
        TB = A.tile([128, NT, NE], F32, 'TB')
        EQ = A.tile([128, NT, NE], F32, 'EQ')
        TOT = A.tile([128, NE], F32, 'TOT')
        PAD = A.tile([128, NE], F32, 'PAD')
        PADi = A.tile([128, NE], I32, 'PADi')
        PS_ = A.tile([128, NE], F32, 'PS')
        PEND = A.tile([128, NE], F32, 'PEND')
        pendc = A.tile([32, 1], F32, 'pendc')
        cmpt = A.tile([32, NB], BF16, 'cmpt')
        EB = A.tile([128, NB], F32, 'EB')
        EB2 = A.tile([128, NB], F32, 'EB2')
        DESTf = A.tile([128, NT, 4], F32, 'DESTf')
        gs = A.tile([128, NT], F32, 'gs')
        MKf = MK.rearrange("p t e -> p (t e)")
        for c0 in range(0, NC_, 512):
            w = min(512, NC_ - c0)
            S.add('pe', lambda e, c0=c0, w=w: e.matmul(bank(0)[:, 0:w], lhsT=C['ustrict'], rhs=MKf[:, c0:c0 + w], start=True, stop=True),
                  reads=['MKall', 'ustrict'], writes=['bk0'])
            S.add('pe', lambda e, c0=c0, w=w: e.matmul(bank(1)[:, 0:w], lhsT=ones_bf, rhs=MKf[:, c0:c0 + w], start=True, stop=True),
                  reads=['MKall', 'ones_bf'], writes=['bk1'])
            S.add('act', lambda e, c0=c0, w=w: e.copy(out=POS.rearrange("p t e -> p (t e)")[:, c0:c0 + w], in_=bank(0)[:, 0:w]), reads=['bk0'], writes=['POS'])
            S.add('dve', lambda e, c0=c0, w=w: e.tensor_copy(out=CNT.rearrange("p t e -> p (t e)")[:, c0:c0 + w], in_=bank(1)[:, 0:w]), reads=['bk1'], writes=['CNT'])
        S.add('pool', lambda e: e.memset(TB[:, 0, :], 0.0), writes=['TB'])
        for tj in range(1, NT):
            S.add('dve', lambda e, tj=tj: e.tensor_tensor(out=TB[:, tj, :], in0=TB[:, tj - 1, :], in1=CNT[:, tj - 1, :], op=ALU.add),
                  reads=['TB', 'CNT'], writes=['TB'])
        S.add('dve', lambda e: e.tensor_tensor(out=TOT, in0=TB[:, NT - 1, :], in1=CNT[:, NT - 1, :], op=ALU.add), reads=['TB', 'CNT'], writes=['TOT'])
        S.add('dve', lambda e: e.tensor_scalar(out=PADi, in0=TOT, scalar1=127.0, scalar2=None, op0=ALU.add), reads=['TOT'], writes=['PADi'])
        S.add('dve', lambda e: e.tensor_scalar(out=PADi, in0=PADi, scalar1=7, scalar2=7, op0=ALU.arith_shift_right, op1=ALU.logical_shift_left),
              reads=['PADi'], writes=['PADi'])
        S.add('dve', lambda e: e.tensor_copy(out=PAD, in_=PADi), reads=['PADi'], writes=['PAD'])
        S.add('pool', lambda e: e.memset(PS_[:, 0:1], 0.0), writes=['PS'])
        for ex in range(1, NE):
            S.add('dve', lambda e, ex=ex: e.tensor_tensor(out=PS_[:, ex:ex + 1], in0=PS_[:, ex - 1:ex], in1=PAD[:, ex - 1:ex], op=ALU.add),
                  reads=['PS', 'PAD'], writes=['PS'])
        S.add('dve', lambda e: e.tensor_tensor(out=PEND, in0=PS_, in1=PAD, op=ALU.add), reads=['PS', 'PAD'], writes=['PEND'])
        S.add('dve', lambda e: e.tensor_tensor(out=POS, in0=POS, in1=TB, op=ALU.add), reads=['POS', 'TB'], writes=['POS'])
        S.add('dve', lambda e: e.tensor_tensor(out=POS, in0=POS, in1=PS_.unsqueeze(1).to_broadcast([128, NT, NE]), op=ALU.add),
              reads=['POS', 'PS'], writes=['POS'])
        for k in range(4):
            S.add('dve', lambda e, k=k: e.tensor_tensor(out=EQ, in0=LG, in1=V8[:, :, k:k + 1].to_broadcast([128, NT, NE]), op=ALU.is_equal),
                  reads=['LGall', 'V8all'], writes=['EQ'])
            S.add('dve', lambda e: e.tensor_tensor(out=EQ, in0=EQ, in1=POS, op=ALU.mult), reads=['EQ', 'POS'], writes=['EQ'])
            S.add('dve', lambda e, k=k: e.tensor_reduce(out=DESTf[:, :, k], in_=EQ, axis=AX.X, op=ALU.add), reads=['EQ'], writes=['DESTf'])
        S.add('dve', lambda e: e.tensor_copy(out=DESTi, in_=DESTf), reads=['DESTf'], writes=['DESTi'])
        S.add('dve', lambda e: e.tensor_tensor(out=G4, in0=V8[:, :, 0:4], in1=V8[:, :, 0:1].to_broadcast([128, NT, 4]), op=ALU.subtract),
              reads=['V8all'], writes=['G4'])
        S.add('act', lambda e: e.activation(out=G4, in_=G4, func=AF.Exp), reads=['G4'], writes=['G4'])
        S.add('dve', lambda e: e.tensor_reduce(out=gs, in_=G4, axis=AX.X, op=ALU.add), reads=['G4'], writes=['gs'])
        S.add('dve', lambda e: e.reciprocal(out=gs, in_=gs), reads=['gs'], writes=['gs'])
        S.add('dve', lambda e: e.tensor_tensor(out=G4, in0=G4, in1=gs.unsqueeze(2).to_broadcast([128, NT, 4]), op=ALU.mult),
              reads=['G4', 'gs'], writes=['G4'])
        S.add('pe', lambda e: e.transpose(out=bank(2)[0:32, 0:128], in_=PEND, identity=C['ident_f']), reads=['PEND', 'ident_f'], writes=['bk2'])
        S.add('dve', lambda e: e.tensor_copy(out=pendc, in_=bank(2)[0:32, 0:1]), reads=['bk2'], writes=['pendc'])
        S.add('dve', lambda e: e.tensor_scalar(out=cmpt, in0=C['blk128'], scalar1=pendc[:, 0:1], scalar2=None, op0=ALU.is_ge),
              reads=['pendc', 'blk128'], writes=['cmpt'])
        S.add('pe', lambda e: e.matmul(bank(3)[:, 0:NB], lhsT=ones_bf[0:32, :], rhs=cmpt, start=True, stop=True), reads=['cmpt', 'ones_bf'], writes=['bk3'])
        S.add('dve', lambda e: e.tensor_scalar(out=EB, in0=bank(3)[:, 0:NB], scalar1=float(NE - 1), scalar2=None, op0=ALU.min), reads=['bk3'], writes=['EB'])
        S.add('dve', lambda e: e.tensor_scalar(out=IDXE, in0=EB[0:2, :], scalar1=float(l * NE), scalar2=None, op0=ALU.add), reads=['EB'], writes=['IDXE'])
        S.add('dve', lambda e: e.tensor_scalar(out=EB2, in0=EB, scalar1=128.0, scalar2=C['pcol'][:, 0:1], op0=ALU.mult, op1=ALU.add),
              reads=['EB', 'pcol'], writes=['EB2'])
        S.add('dve', lambda e: e.tensor_tensor(out=EQ.rearrange("p t e -> p (t e)")[:, 0:NB - 2], in0=EB[:, 2:NB], in1=EB[:, 0:NB - 2], op=ALU.is_equal),
              reads=['EB', 'EQ'], writes=['EQ'])
        S.add('dve', lambda e: e.scalar_tensor_tensor(out=EB2[:, 2:NB], in0=EQ.rearrange("p t e -> p (t e)")[:, 0:NB - 2], scalar=float(OOB),
                                                      in1=EB2[:, 2:NB], op0=ALU.mult, op1=ALU.add), reads=['EQ', 'EB2'], writes=['EB2'])
        S.add('dve', lambda e: e.tensor_scalar(out=IDXW, in0=EB2, scalar1=float(l * NE * 128), scalar2=None, op0=ALU.add), reads=['EB2'], writes=['IDXW'])
        if 'moe_dbg' in dbg:
            S.barrier()
            d_lg = nc.dram_tensor('d_lg', [128, NT, NE], F32, kind="ExternalOutput").ap()
            d_v8 = nc.dram_tensor('d_v8', [128, NT, 8], F32, kind="ExternalOutput").ap()
            d_dest = nc.dram_tensor('d_dest', [128, NT, 4], I32, kind="ExternalOutput").ap()
            d_g4 = nc.dram_tensor('d_g4', [128, NT, 4], F32, kind="ExternalOutput").ap()
            d_eb = nc.dram_tensor('d_eb', [128, NB], F32, kind="ExternalOutput").ap()
            d_idxw = nc.dram_tensor('d_idxw', [128, NB], I32, kind="ExternalOutput").ap()
            d_pend = nc.dram_tensor('d_pend', [128, NE], F32, kind="ExternalOutput").ap()
            for dd, tt_ in ((d_lg, LG), (d_v8, V8), (d_dest, DESTi), (d_g4, G4), (d_eb, EB), (d_idxw, IDXW), (d_pend, PEND)):
                S.add('sp', lambda e, dd=dd, tt_=tt_: [e.dma_start(out=dd, in_=tt_)], writes=[('dbgout', id(dd))], ndma=1)
        S.barrier()
        A.reset(MK5)
        hs = [A.tile([128, D], BF16, 'hs') for _ in range(3)]
        for tj in range(NT):
            b = tj % 3
            r0 = tj * 128
            S.add('sp', lambda e, b=b, r0=r0: [e.dma_start(out=hs[b], in_=h2_tm[r0:r0 + 128, :])], writes=[('hs', b)], ndma=1)
            for k in range(4):
                S.add('pool', lambda e, b=b, tj=tj, k=k: [e.indirect_dma_start(
                    out=Xs, out_offset=bass.IndirectOffsetOnAxis(ap=DESTi[:, tj, k:k + 1], axis=0), in_=hs[b], in_offset=None)],
                    reads=[('hs', b)], writes=[('Xs', tj, k)], ndma=1)
        S.barrier()
        A.reset(MK5)
        WG = [A.tile([128, 8, 2048], BF16, 'WG') for _ in range(2)]
        WD = [A.tile([128, 8, 1024], BF16, 'WD') for _ in range(2)]
        BG = [A.tile([2, 2048], BF16, 'BG') for _ in range(2)]
        BD = [A.tile([2, 1024], BF16, 'BD') for _ in range(2)]
        xb_ = [A.tile([128, D], BF16, 'xb') for _ in range(2)]
        xT_ = [A.tile([128, 8, 128], BF16, 'xT') for _ in range(2)]
        am = [A.tile([128, 512], F32, 'am') for _ in range(2)]
        sg = [A.tile([128, 512], F32, 'sg')] * 2
        uc = [A.tile([128, 512], F32, 'uc') for _ in range(2)]
        yb = [A.tile([128, D], BF16, 'yb') for _ in range(2)]
        yT_ = A.tile([128, 8, 128], BF16, 'yT8')
        ob_ = [A.tile([128, D], F32, 'ob') for _ in range(2)]
        wgu_rows = wgu.rearrange("l r c -> (l r) c")
        wdn_rows = wdn.rearrange("l r c -> (l r) c")
        bgu_rows = bgu.rearrange("l r c -> (l r) c")
        bdn_rows = bdn.rearrange("l r c -> (l r) c")

        def bcreg(e):
            if 'r' not in BCREG:
                BCREG['r'] = e.alloc_register('bcreg')
                e.reg_mov(BCREG['r'], DEPTH * NE * 128 - 1)
            return BCREG['r']
        def stage_a(bi):
            b = bi % 2
            S.add('pool', lambda e, b=b, bi=bi: [e.indirect_dma_start(
                out=WG[b].rearrange("p k n -> p (k n)"), out_offset=None, in_=wgu_rows,
                in_offset=bass.IndirectOffsetOnAxis(ap=IDXW[:, bi:bi + 1], axis=0), bounds_check=bcreg(e), oob_is_err=False)],
                writes=[('WG', b)], ndma=1)
            S.add('pool', lambda e, b=b, bi=bi: [e.indirect_dma_start(
                out=WD[b].rearrange("p k n -> p (k n)"), out_offset=None, in_=wdn_rows,
                in_offset=bass.IndirectOffsetOnAxis(ap=IDXW[:, bi:bi + 1], axis=0), bounds_check=bcreg(e), oob_is_err=False)],
                writes=[('WD', b)], ndma=1)
            S.add('pool', lambda e, b=b, bi=bi: [e.indirect_dma_start(
                out=BG[b], out_offset=None, in_=bgu_rows, in_offset=bass.IndirectOffsetOnAxis(ap=IDXE[0:2, bi:bi + 1], axis=0))],
                writes=[('BG', b)], ndma=1)
            S.add('pool', lambda e, b=b, bi=bi: [e.indirect_dma_start(
                out=BD[b], out_offset=None, in_=bdn_rows, in_offset=bass.IndirectOffsetOnAxis(ap=IDXE[0:2, bi:bi + 1], axis=0))],
                writes=[('BD', b)], ndma=1)
            S.add('sp', lambda e, b=b, bi=bi: [e.dma_start(out=xb_[b], in_=Xs[bi * 128:(bi + 1) * 128, :])], writes=[('xb', b)], ndma=1)
            pb = pbf[0]
            for kc in range(8):
                S.add('pe', lambda e, b=b, kc=kc, pb=pb: e.transpose(out=pb[:, kc * 128:(kc + 1) * 128], in_=xb_[b][:, kc * 128:(kc + 1) * 128],
                                                                    identity=C['ident_bf']), reads=[('xb', b), 'ident_bf'], writes=[('pbf', 0)])
            S.add('act', lambda e, b=b, pb=pb: e.copy(out=xT_[b].rearrange("p k t -> p (k t)"), in_=pb), reads=[('pbf', 0)], writes=[('xT', b)])
            for hf in range(2):
                for n in (hf, 2 + hf):
                    bk = bank(n)
                    BKK = ('bk', n)
                    for kc in range(8):
                        S.add('pe', lambda e, b=b, kc=kc, n=n, bk=bk: e.matmul(bk, lhsT=xT_[b][:, kc, :], rhs=WG[b][:, kc, n * 512:(n + 1) * 512],
                                                                              start=(kc == 0), stop=False), reads=[('xT', b), ('WG', b)], writes=[BKK])
                    S.add('pe', lambda e, b=b, n=n, bk=bk: e.matmul(bk, lhsT=ones_bf[0:1, :], rhs=BG[b][0:1, n * 512:(n + 1) * 512], start=False, stop=True),
                          reads=['ones_bf', ('BG', b)], writes=[BKK])
                ab = bank(hf)
                ub = bank(2 + hf)
                a_ = am[hf]
                s_ = sg[hf]
                u_ = uc[hf]
                S.add('act', lambda e, ab=ab, a_=a_: e.copy(out=a_, in_=ab), reads=[('bk', hf)], writes=[('am', hf)])
                S.add('act', lambda e, ub=ub, u_=u_: e.activation(out=u_, in_=ub, func=AF.Identity, bias=onec[:, 0:1]), reads=[('bk', 2 + hf), 'onec'], writes=[('uc', hf)])
                S.add('dve', lambda e, a_=a_: e.tensor_scalar(out=a_, in0=a_, scalar1=7.0, scalar2=None, op0=ALU.min), reads=[('am', hf)], writes=[('am', hf)])
                S.add('act', lambda e, a_=a_, s_=s_: e.activation(out=s_, in_=a_, func=AF.Sigmoid, scale=1.702), reads=[('am', hf)], writes=[('sg', 0)])
                S.add('dve', lambda e, u_=u_: e.tensor_scalar(out=u_, in0=u_, scalar1=-6.0, scalar2=8.0, op0=ALU.max, op1=ALU.min),
                      reads=[('uc', hf)], writes=[('uc', hf)])
                S.add('pool', lambda e, a_=a_, s_=s_: e.tensor_tensor(out=a_, in0=a_, in1=s_, op=ALU.mult), reads=[('am', hf), ('sg', 0)], writes=[('am', hf)])
                S.add('pool', lambda e, hf=hf, b=b, a_=a_, u_=u_: e.tensor_tensor(out=yb[b][:, hf * 512:(hf + 1) * 512], in0=u_, in1=a_, op=ALU.mult),
                      reads=[('uc', hf), ('am', hf)], writes=[('yb', b, hf)])

        def stage_b(bi):
            b = bi % 2
            pb = pbf[1]
            for kc in range(8):
                S.add('pe', lambda e, kc=kc, pb=pb, b=b: e.transpose(out=pb[:, kc * 128:(kc + 1) * 128], in_=yb[b][:, kc * 128:(kc + 1) * 128], identity=C['ident_bf']),
                      reads=[('yb', b, kc // 4), 'ident_bf'], writes=[('pbf', 1)])
            S.add('dve', lambda e, pb=pb: e.tensor_copy(out=yT_.rearrange("p k t -> p (k t)"), in_=pb), reads=[('pbf', 1)], writes=['yT8'])
            for n in range(2):
                bk = bank(4 + n)
                BKK = ('bk', 4 + n)
                for kc in range(8):
                    S.add('pe', lambda e, b=b, kc=kc, n=n, bk=bk: e.matmul(bk, lhsT=yT_[:, kc, :], rhs=WD[b][:, kc, n * 512:(n + 1) * 512],
                                                                          start=(kc == 0), stop=False), reads=['yT8', ('WD', b)], writes=[BKK])
                S.add('pe', lambda e, b=b, n=n, bk=bk: e.matmul(bk, lhsT=ones_bf[0:1, :], rhs=BD[b][0:1, n * 512:(n + 1) * 512], start=False, stop=True),
                      reads=['ones_bf', ('BD', b)], writes=[BKK])
                if n == 0:
                    S.add('act', lambda e, b=b, bk=bk: e.copy(out=ob_[b][:, 0:512], in_=bk), reads=[BKK], writes=[('ob', b, 0)])
                else:
                    S.add('dve', lambda e, b=b, bk=bk: e.tensor_copy(out=ob_[b][:, 512:1024], in_=bk), reads=[BKK], writes=[('ob', b, 1)])
            S.add('act', lambda e, b=b, bi=bi: [e.dma_start(out=Ys[bi * 128:(bi + 1) * 128, :], in_=ob_[b])],
                  reads=[('ob', b, 0), ('ob', b, 1)], writes=[('Ys', bi)], ndma=1)

        stage_a(0)
        for bi in range(NB):
            if bi + 1 < NB:
                stage_a(bi + 1)
            stage_b(bi)
        S.barrier()
        A.reset(MK5)
        Yk = [[A.tile([128, D], F32, 'Yk') for _ in range(4)] for _ in range(2)]
        xt_p9 = [A.tile([128, D], F32, 'xt9') for _ in range(2)]
        acc = [A.tile([128, D], F32, 'acc') for _ in range(2)]
        for tj in range(NT):
            b = tj % 2
            r0 = tj * 128
            isx = tj < NTX
            for k in range(4):
                S.add('pool', lambda e, b=b, tj=tj, k=k: [e.indirect_dma_start(
                    out=Yk[b][k], out_offset=None, in_=Ys, in_offset=bass.IndirectOffsetOnAxis(ap=DESTi[:, tj, k:k + 1], axis=0))],
                    writes=[('Yk', b, k)], ndma=1)
            S.add('sp', lambda e, b=b, r0=r0: [e.dma_start(out=xt_p9[b], in_=xres[r0:r0 + 128, :])], writes=[('xt9', b)], ndma=1)
            S.add('dve', lambda e, b=b, tj=tj: e.tensor_scalar(out=acc[b], in0=Yk[b][0], scalar1=G4[:, tj, 0:1], scalar2=None, op0=ALU.mult),
                  reads=[('Yk', b, 0)], writes=[('acc', b)])
            for k in range(1, 4):
                S.add('dve', lambda e, b=b, tj=tj, k=k: e.scalar_tensor_tensor(out=acc[b], in0=Yk[b][k], scalar=G4[:, tj, k:k + 1], in1=acc[b],
                                                                              op0=ALU.mult, op1=ALU.add), reads=[('Yk', b, k), ('acc', b)], writes=[('acc', b)])
            gb = BT['g2x'] if isx else BT['g2c']
            S.add('pool', lambda e, b=b, gb=gb: e.tensor_tensor(out=acc[b], in0=acc[b], in1=gb, op=ALU.mult), reads=[('acc', b), ('btile', id(gb))], writes=[('acc', b)])
            S.add('pool', lambda e, b=b: e.tensor_tensor(out=acc[b], in0=acc[b], in1=xt_p9[b], op=ALU.add), reads=[('acc', b), ('xt9', b)], writes=[('acc', b)])
            S.add('act', lambda e, b=b, r0=r0: [e.dma_start(out=xres[r0:r0 + 128, :], in_=acc[b])], reads=[('acc', b)], writes=[('xres', r0)], ndma=1)
        S.barrier()

    for _l in range(nl):
        _layer(_l)

    S.barrier()
    A.reset(PERSIST)
    fcol = A.tile([128, 8], F32, 'fcol')
    fb_pf = A.tile([128, D], F32, 'fb')
    S.add('sp', lambda e: [e.dma_start(out=fcol, in_=fng)], writes=['fcol'], ndma=1)
    if final_norm:
        bcast_tile(fb_pf, fcol, None, 'fcol')
    xt_pf = [A.tile([128, D], F32, 'xtf') for _ in range(2)]
    xo_pf = [A.tile([128, D], F32, 'xof') for _ in range(2)]
    junk_pf = A.tile([128, D], BF16, 'junkf')
    ss_pf = [A.tile([128, 1], F32, 'ssf') for _ in range(2)]
    for tj in range(NTX):
        b = tj % 2
        r0 = tj * 128
        S.add(dmaq(), lambda e, b=b, r0=r0: [e.dma_start(out=xt_pf[b], in_=xres[r0:r0 + 128, :])], writes=[('xtf', b)], ndma=1)
        if final_norm:
            S.add('act', lambda e, b=b: e.activation(out=junk_pf, in_=xt_pf[b], func=AF.Square, accum_out=ss_pf[b]), reads=[('xtf', b)], writes=[('ssf', b), 'junkf'])
            S.add('dve', lambda e, b=b: e.tensor_scalar(out=ss_pf[b], in0=ss_pf[b], scalar1=1.0 / D, scalar2=EPS, op0=ALU.mult, op1=ALU.add),
                  reads=[('ssf', b)], writes=[('ssf', b)])
            S.add('act', lambda e, b=b: e.sqrt(out=ss_pf[b], in_=ss_pf[b]), reads=[('ssf', b)], writes=[('ssf', b)])
            S.add('dve', lambda e, b=b: e.reciprocal(out=ss_pf[b], in_=ss_pf[b]), reads=[('ssf', b)], writes=[('ssf', b)])
            S.add('dve', lambda e, b=b: e.scalar_tensor_tensor(out=xo_pf[b], in0=xt_pf[b], scalar=ss_pf[b][:, 0:1], in1=fb_pf, op0=ALU.mult, op1=ALU.mult),
                  reads=[('xtf', b), ('ssf', b), ('btile', id(fb_pf))], writes=[('xof', b)])
        else:
            S.add('dve', lambda e, b=b: e.tensor_copy(out=xo_pf[b], in_=xt_pf[b]), reads=[('xtf', b)], writes=[('xof', b)])
        S.add(dmaq(), lambda e, b=b, r0=r0: [e.dma_start(out=yout[r0:r0 + 128, :], in_=xo_pf[b])], reads=[('xof', b)], writes=[('yout', tj)], ndma=1)
    S.barrier()
    S.emit()
    return nc, consts


def prep_shared(inp):
    f32 = lambda a: np.ascontiguousarray(np.asarray(a, dtype=np.float32))
    sh = {}
    sh['ada_w'] = f32(inp['ada_w'])
    sh['ada_bT'] = f32(np.asarray(inp['ada_b']).reshape(DEPTH, 48, 128).transpose(0, 2, 1))
    sh['n1g'] = f32(np.asarray(inp['norm1_g']).reshape(DEPTH, 8, 128).transpose(0, 2, 1))
    sh['n2g'] = f32(np.asarray(inp['norm2_g']).reshape(DEPTH, 8, 128).transpose(0, 2, 1))
    sh['fng'] = f32(np.asarray(inp['final_norm_g']).reshape(8, 128).T)
    sh['w_in'] = f32(inp['w_in'])
    sh['sink'] = f32(inp['attn_sink'])
    sh['w_fo'] = f32(inp['w_fourier_out'])
    sh['w_ao'] = f32(inp['w_attn_out'])
    sh['w_out'] = f32(inp['w_out'])
    sh['r_w'] = f32(np.asarray(inp['router_w']).reshape(DEPTH, 8, 128, NE).transpose(0, 2, 1, 3))
    sh['r_b'] = f32(np.asarray(inp['router_b']).reshape(DEPTH, 1, NE))
    sh['wgu'] = f32(np.asarray(inp['expert_w_gu']).reshape(DEPTH, NE, 8, 128, 2048).transpose(0, 1, 3, 2, 4).reshape(DEPTH, NE * 128, 8 * 2048))
    sh['wdn'] = f32(np.asarray(inp['expert_w_down']).reshape(DEPTH, NE, 8, 128, 1024).transpose(0, 1, 3, 2, 4).reshape(DEPTH, NE * 128, 8 * 1024))
    sh['bgu'] = f32(inp['expert_b_gu'])
    sh['bdn'] = f32(inp['expert_b_down'])
    return sh


def core_inputs(inp, sh, consts, b):
    m = dict(sh)
    m['xin'] = np.ascontiguousarray(np.asarray(inp['x'][b], dtype=np.float32))
    m['ctxin'] = np.ascontiguousarray(np.asarray(inp['ctx'][b], dtype=np.float32))
    cc = np.zeros((128, 8, 2), np.float32)
    cc[:, :, 0] = np.asarray(inp['c'][b]).reshape(8, 128).T
    cc[:, :, 1] = np.asarray(inp['c_ctx']).reshape(8, 128).T
    m['ccol'] = cc
    for k, v in consts.items():
        m['c_' + k] = v
    return m


def kernel(**inputs):
    nc, consts = build()
    sh = prep_shared(inputs)
    nb = inputs['x'].shape[0]
    in_maps = [core_inputs(inputs, sh, consts, i) for i in range(nb)]
    res = run_bass_kernel_spmd(nc, in_maps, core_ids=list(range(nb)))
    out = np.stack([np.asarray(res.results[i]['yout'], dtype=np.float32) for i in range(nb)], axis=0)
    return out
```
